# Optimizing a Trainium2 kernel written in Bass

```python
import math
import jax
import jax.numpy as jnp
from jax import lax
import numpy as np

D_MODEL = 1024
BATCH = 8
SEQ = 4096
DEPTH = 2

CTX_LEN = 256
GRID_W = 64
RMS_EPS = 1e-6

MLA_HEADS = 8
MLA_Q_LORA = 384
MLA_KV_LORA = 256
MLA_NOPE = 64
MLA_ROPE = 32
MLA_V = 64
MLA_QK = MLA_NOPE + MLA_ROPE
ROPE_PAIRS = MLA_ROPE // 4
ROPE_BASE = 10000.0
Q_BLOCK = 128

LRU_WIDTH = 512
LRU_BLOCKS = 8
LRU_BLOCK_DIM = LRU_WIDTH // LRU_BLOCKS
LRU_C = 8.0
LRU_CONV = 4

EVEN_SPLITS = (MLA_Q_LORA, MLA_Q_LORA + MLA_KV_LORA, MLA_Q_LORA + MLA_KV_LORA + MLA_ROPE, MLA_Q_LORA + MLA_KV_LORA + MLA_ROPE + LRU_WIDTH)
EVEN_IN = MLA_Q_LORA + MLA_KV_LORA + MLA_ROPE + 2 * LRU_WIDTH
EVEN_MIX = MLA_HEADS * MLA_V + LRU_WIDTH

HY_WIDTH = D_MODEL
HY_ORDER = 2
HY_CONV = 3
HY_BANDS = 16
HY_EMB = 2 * HY_BANDS + 1
HY_HIDDEN = 64

N_EXPERTS = 16
N_GROUPS = 4
EXPERTS_PER_GROUP = N_EXPERTS // N_GROUPS
TOP_K = 2
EXPERT_FF = 512

N_EVEN = (DEPTH + 1) // 2
N_ODD = DEPTH // 2

kernel_name = 'hybrid_mla_rglru_hyena_moe_dit'


def rms_norm(x, g):
    xf = x.astype(jnp.float32)
    y = xf * lax.rsqrt(jnp.mean(xf * xf, axis=-1, keepdims=True) + RMS_EPS)
    return (y * g.astype(jnp.float32)).astype(x.dtype)


def modulate(h, shift, scale):
    return h * (1.0 + scale) + shift


def grid_rope_tables(rows):
    row = jnp.repeat(jnp.arange(rows), GRID_W)
    col = jnp.tile(jnp.arange(GRID_W), rows)
    inv_freq = ROPE_BASE ** (-jnp.arange(ROPE_PAIRS, dtype=jnp.float32) / ROPE_PAIRS)
    ang = jnp.stack([row, col], axis=-1).astype(jnp.float32)[:, :, None] * inv_freq
    return jnp.cos(ang), jnp.sin(ang)


def apply_rope_2d(x, cos, sin):
    xr = x.astype(jnp.float32).reshape(*x.shape[:-1], 2, 2, ROPE_PAIRS)
    xa, xb = xr[..., 0, :], xr[..., 1, :]
    out = jnp.stack([xa * cos - xb * sin, xa * sin + xb * cos], axis=-2)
    return out.reshape(x.shape).astype(x.dtype)


def attend(q, k, v):
    s = jnp.einsum('bqhd,bkhd->bhqk', q, k).astype(jnp.float32) * (MLA_QK ** -0.5)
    p = jax.nn.softmax(s, axis=-1).astype(v.dtype)
    return jnp.einsum('bhqk,bkhd->bqhd', p, v)


def blocked_attend(q, k, v):
    bsz, n, heads, dk = q.shape
    nb = n // Q_BLOCK
    qb = q.reshape(bsz, nb, Q_BLOCK, heads, dk).transpose(1, 0, 2, 3, 4)
    ob = lax.map(lambda qi: attend(qi, k, v), qb)
    return ob.transpose(1, 0, 2, 3, 4).reshape(bsz, n, heads, v.shape[-1])


def depthwise_conv(x, w, b, pad_left, pad_right):
    y = lax.conv_general_dilated(x, w[:, None, :].astype(x.dtype), window_strides=(1,),
                                 padding=[(pad_left, pad_right)],
                                 dimension_numbers=('NWC', 'WIO', 'NWC'),
                                 feature_group_count=x.shape[-1])
    return y + b.astype(x.dtype)


def linear_scan(a, b, h0):
    def combine(left, right):
        a_l, b_l = left
        a_r, b_r = right
        return a_l * a_r, a_r * b_l + b_r
    a_cum, b_cum = lax.associative_scan(combine, (a, b), axis=1)
    return a_cum * h0[:, None, :] + b_cum


def rglru_coeffs(u, w_a, b_a, w_x, b_x, lam):
    bsz, n, width = u.shape
    ub = u.reshape(bsz, n, LRU_BLOCKS, LRU_BLOCK_DIM)
    gate_a = jnp.einsum('bnhi,hij->bnhj', ub, w_a).reshape(bsz, n, width) + b_a
    gate_x = jnp.einsum('bnhi,hij->bnhj', ub, w_x).reshape(bsz, n, width) + b_x
    r = jax.nn.sigmoid(gate_a.astype(jnp.float32))
    i = jax.nn.sigmoid(gate_x.astype(jnp.float32))
    log_a = -LRU_C * r * jax.nn.softplus(-lam.astype(jnp.float32))
    a = jnp.exp(log_a)
    b = jnp.sqrt(-jnp.expm1(2.0 * log_a)) * (i * u.astype(jnp.float32))
    return a, b


def maybe_flip(t, reverse):
    return jnp.flip(t, axis=1) if reverse else t


def rglru_bidirectional(u_ctx, u_lat, w_a, b_a, w_x, b_x, lam):
    zero = jnp.zeros((u_lat.shape[0], u_lat.shape[-1]), jnp.float32)
    outs = []
    for d in range(2):
        rev = d == 1
        a_c, b_c = rglru_coeffs(maybe_flip(u_ctx, rev), w_a[d], b_a[d], w_x[d], b_x[d], lam[d])
        h_c = linear_scan(a_c, b_c, zero)
        a_l, b_l = rglru_coeffs(maybe_flip(u_lat, rev), w_a[d], b_a[d], w_x[d], b_x[d], lam[d])
        h_l = linear_scan(a_l, b_l, h_c[:, -1])
        outs.append((maybe_flip(h_c, rev), maybe_flip(h_l, rev)))
    return outs[0][0] + outs[1][0], outs[0][1] + outs[1][1]


def mla_up(cq, ckv, q_norm_g, kv_norm_g, w_uq, w_ukv):
    lead = cq.shape[:-1]
    q = (rms_norm(cq, q_norm_g) @ w_uq).reshape(*lead, MLA_HEADS, MLA_QK)
    kv = (rms_norm(ckv, kv_norm_g) @ w_ukv).reshape(*lead, MLA_HEADS, MLA_NOPE + MLA_V)
    return q, kv[..., :MLA_NOPE], kv[..., MLA_NOPE:]


def mla_rglru_mixer(h_ctx, h_lat, cos, sin, w_in, q_norm_g, kv_norm_g, w_uq, w_ukv,
                    conv_w, conv_b, w_a, b_a, w_x, b_x, lam, w_out, need_ctx):
    bsz, n_lat, _ = h_lat.shape
    n_ctx = h_ctx.shape[1]
    cq_c, ckv_c, kr_c, ux_c, ug_c = jnp.split(h_ctx @ w_in, EVEN_SPLITS, axis=-1)
    cq_l, ckv_l, kr_l, ux_l, ug_l = jnp.split(h_lat @ w_in, EVEN_SPLITS, axis=-1)

    q_c, kn_c, v_c = mla_up(cq_c, ckv_c, q_norm_g, kv_norm_g, w_uq, w_ukv)
    q_l, kn_l, v_l = mla_up(cq_l, ckv_l, q_norm_g, kv_norm_g, w_uq, w_ukv)
    k_c = jnp.concatenate([kn_c, jnp.broadcast_to(kr_c[:, :, None, :], (bsz, n_ctx, MLA_HEADS, MLA_ROPE))], axis=-1)
    kr_l = apply_rope_2d(kr_l, cos, sin)
    k_l = jnp.concatenate([kn_l, jnp.broadcast_to(kr_l[:, :, None, :], (bsz, n_lat, MLA_HEADS, MLA_ROPE))], axis=-1)
    q_l = jnp.concatenate([q_l[..., :MLA_NOPE], apply_rope_2d(q_l[..., MLA_NOPE:], cos[:, None], sin[:, None])], axis=-1)
    k_all = jnp.concatenate([k_c, k_l], axis=1)
    v_all = jnp.concatenate([v_c, v_l], axis=1)
    att_l = blocked_attend(q_l, k_all, v_all).reshape(bsz, n_lat, MLA_HEADS * MLA_V)

    u_c = depthwise_conv(ux_c, conv_w, conv_b, 2, 1)
    u_l = depthwise_conv(ux_l, conv_w, conv_b, 2, 1)
    y_c, y_l = rglru_bidirectional(u_c, u_l, w_a, b_a, w_x, b_x, lam)
    lru_l = (y_l * jax.nn.gelu(ug_l.astype(jnp.float32))).astype(h_lat.dtype)
    out_l = jnp.concatenate([att_l, lru_l], axis=-1) @ w_out
    if not need_ctx:
        return None, out_l
    att_c = attend(q_c, k_c, v_c).reshape(bsz, n_ctx, MLA_HEADS * MLA_V)
    lru_c = (y_c * jax.nn.gelu(ug_c.astype(jnp.float32))).astype(h_ctx.dtype)
    out_c = jnp.concatenate([att_c, lru_c], axis=-1) @ w_out
    return out_c, out_l


def hyena_filters(n, w1, b1, f1, w2, b2, f2, w3, decay):
    t = jnp.linspace(0.0, 1.0, n, dtype=jnp.float32)[:, None]
    w = (2.0 * math.pi / n) * jnp.arange(n, dtype=jnp.float32)[:, None]
    f = jnp.linspace(1e-4, HY_BANDS - 1, HY_BANDS, dtype=jnp.float32)[None, :]
    z = jnp.concatenate([t, jnp.cos(f * w), -jnp.sin(f * w)], axis=-1)
    a = jnp.sin(f1.astype(jnp.float32) * (z @ w1.astype(jnp.float32) + b1.astype(jnp.float32)))
    a = jnp.sin(f2.astype(jnp.float32) * (a @ w2.astype(jnp.float32) + b2.astype(jnp.float32)))
    h = (a @ w3.astype(jnp.float32)).reshape(n, 2 * HY_ORDER, HY_WIDTH)
    h = h * jnp.exp(-t[:, :, None] * jnp.abs(decay.astype(jnp.float32)))
    h = h.reshape(n, HY_ORDER, 2, HY_WIDTH)
    return h / (jnp.sum(jnp.abs(h), axis=(0, 2), keepdims=True) + 1e-6)


def two_sided_fftconv(u, h_fwd, h_bwd, skip):
    n = u.shape[1]
    u32 = u.astype(jnp.float32)
    spec_u = jnp.fft.rfft(u32, n=2 * n, axis=1)
    spec_h = jnp.fft.rfft(h_fwd, n=2 * n, axis=0) + jnp.conj(jnp.fft.rfft(h_bwd, n=2 * n, axis=0))
    y = jnp.fft.irfft(spec_u * spec_h[None], n=2 * n, axis=1)[:, :n]
    return (y + u32 * skip.astype(jnp.float32)).astype(u.dtype)


def hyena_operator(h, w_in, conv_w, conv_b, w1, b1, f1, w2, b2, f2, w3, decay, skip, w_out):
    n = h.shape[1]
    p = depthwise_conv(h @ w_in, conv_w, conv_b, 1, 1)
    v, x1, x2 = jnp.split(p, 3, axis=-1)
    filt = hyena_filters(n, w1, b1, f1, w2, b2, f2, w3, decay)
    z = v
    for order, gate in enumerate((x1, x2)):
        z = gate * two_sided_fftconv(z, filt[:, order, 0], filt[:, order, 1], skip[order])
    return z @ w_out


def grouped_moe(h, router_w, router_b, w1, w3, w2):
    s = jax.nn.sigmoid(jnp.einsum('bld,de->ble', h, router_w).astype(jnp.float32))
    sel = s + router_b.astype(jnp.float32)
    sel_g = sel.reshape(*sel.shape[:-1], N_GROUPS, EXPERTS_PER_GROUP)
    group_score = jnp.sum(lax.top_k(sel_g, 2)[0], axis=-1)
    g_idx = jnp.argmax(group_score, axis=-1)
    in_group = (jnp.arange(N_EXPERTS) // EXPERTS_PER_GROUP) == g_idx[..., None]
    _, e_idx = lax.top_k(jnp.where(in_group, sel, -jnp.inf), TOP_K)
    w_sel = jnp.take_along_axis(s, e_idx, axis=-1)
    w_sel = w_sel / jnp.sum(w_sel, axis=-1, keepdims=True)
    gates = jnp.sum(jax.nn.one_hot(e_idx, N_EXPERTS, dtype=jnp.float32) * w_sel[..., None], axis=-2).astype(h.dtype)
    y = jnp.zeros_like(h)
    for e in range(N_EXPERTS):
        act = jax.nn.silu(h @ w1[e]) * (h @ w3[e])
        y = y + gates[..., e:e + 1] * (act @ w2[e])
    return y


def setup_inputs(seed: int = 0) -> dict:
    key = jax.random.key(seed)
    keys = iter(jax.random.split(key, 64))

    def normal(shape, scale):
        return jax.random.normal(next(keys), shape, jnp.float32) * scale

    def gain(shape):
        return 1.0 + normal(shape, 0.02)

    d = D_MODEL
    u = jax.random.uniform(next(keys), (N_EVEN, 2, LRU_WIDTH), jnp.float32, 0.9, 0.999)
    a_base = u ** (1.0 / LRU_C)
    lru_lambda = jnp.log(a_base) - jnp.log1p(-a_base)
    decay_base = jnp.linspace(math.log(100.0) / 1.5, math.log(100.0) / 0.3, HY_WIDTH, dtype=jnp.float32)
    hy_decay = decay_base * (1.0 + normal((N_ODD, 2 * HY_ORDER, HY_WIDTH), 0.05))
    return {
        'x': normal((BATCH, SEQ, d), 1.0),
        'c': normal((BATCH, d), 1.0),
        'ctx': normal((BATCH, CTX_LEN, d), 1.0),
        'c_ctx': normal((d,), 1.0),
        'ada_w': normal((DEPTH, d, 6 * d), 0.5 * d ** -0.5),
        'ada_b': normal((DEPTH, 6 * d), 0.02),
        'norm1_g': gain((DEPTH, d)),
        'norm2_g': gain((DEPTH, d)),
        'final_g': gain((d,)),
        'ev_w_in': normal((N_EVEN, d, EVEN_IN), d ** -0.5),
        'mla_q_norm_g': gain((N_EVEN, MLA_Q_LORA)),
        'mla_kv_norm_g': gain((N_EVEN, MLA_KV_LORA)),
        'mla_w_uq': normal((N_EVEN, MLA_Q_LORA, MLA_HEADS * MLA_QK), MLA_Q_LORA ** -0.5),
        'mla_w_ukv': normal((N_EVEN, MLA_KV_LORA, MLA_HEADS * (MLA_NOPE + MLA_V)), MLA_KV_LORA ** -0.5),
        'lru_conv_w': normal((N_EVEN, LRU_CONV, LRU_WIDTH), LRU_CONV ** -0.5),
        'lru_conv_b': normal((N_EVEN, LRU_WIDTH), 0.02),
        'lru_w_a': normal((N_EVEN, 2, LRU_BLOCKS, LRU_BLOCK_DIM, LRU_BLOCK_DIM), LRU_BLOCK_DIM ** -0.5),
        'lru_b_a': normal((N_EVEN, 2, LRU_WIDTH), 0.02),
        'lru_w_x': normal((N_EVEN, 2, LRU_BLOCKS, LRU_BLOCK_DIM, LRU_BLOCK_DIM), LRU_BLOCK_DIM ** -0.5),
        'lru_b_x': normal((N_EVEN, 2, LRU_WIDTH), 0.02),
        'lru_lambda': lru_lambda,
        'ev_w_out': normal((N_EVEN, EVEN_MIX, d), EVEN_MIX ** -0.5),
        'od_w_in': normal((N_ODD, d, 3 * HY_WIDTH), d ** -0.5),
        'hy_conv_w': normal((N_ODD, HY_CONV, 3 * HY_WIDTH), HY_CONV ** -0.5),
        'hy_conv_b': normal((N_ODD, 3 * HY_WIDTH), 0.02),
        'hy_w1': normal((N_ODD, HY_EMB, HY_HIDDEN), HY_EMB ** -0.5),
        'hy_b1': normal((N_ODD, HY_HIDDEN), 0.1),
        'hy_freq1': 1.0 + normal((N_ODD, HY_HIDDEN), 0.05),
        'hy_w2': normal((N_ODD, HY_HIDDEN, HY_HIDDEN), HY_HIDDEN ** -0.5),
        'hy_b2': normal((N_ODD, HY_HIDDEN), 0.1),
        'hy_freq2': 1.0 + normal((N_ODD, HY_HIDDEN), 0.05),
        'hy_w3': normal((N_ODD, HY_HIDDEN, 2 * HY_ORDER * HY_WIDTH), HY_HIDDEN ** -0.5),
        'hy_decay': hy_decay,
        'hy_skip': normal((N_ODD, HY_ORDER, HY_WIDTH), 1.0),
        'od_w_out': normal((N_ODD, HY_WIDTH, d), HY_WIDTH ** -0.5),
        'router_w': normal((d, N_EXPERTS), d ** -0.5),
        'router_b': normal((N_EXPERTS,), 0.01),
        'moe_w1': normal((DEPTH, N_EXPERTS, d, EXPERT_FF), d ** -0.5),
        'moe_w3': normal((DEPTH, N_EXPERTS, d, EXPERT_FF), d ** -0.5),
        'moe_w2': normal((DEPTH, N_EXPERTS, EXPERT_FF, d), EXPERT_FF ** -0.5),
    }


def reference(x, c, ctx, c_ctx, ada_w, ada_b, norm1_g, norm2_g, final_g,
              ev_w_in, mla_q_norm_g, mla_kv_norm_g, mla_w_uq, mla_w_ukv,
              lru_conv_w, lru_conv_b, lru_w_a, lru_b_a, lru_w_x, lru_b_x, lru_lambda, ev_w_out,
              od_w_in, hy_conv_w, hy_conv_b, hy_w1, hy_b1, hy_freq1, hy_w2, hy_b2, hy_freq2,
              hy_w3, hy_decay, hy_skip, od_w_out,
              router_w, router_b, moe_w1, moe_w3, moe_w2):
    n_lat = x.shape[1]
    n_ctx = ctx.shape[1]
    rows = n_lat // GRID_W
    cos, sin = grid_rope_tables(rows)
    silu_c = jax.nn.silu(c)
    silu_cc = jax.nn.silu(c_ctx)
    xc = ctx
    for layer in range(DEPTH):
        need_ctx = layer < DEPTH - 1
        is_even = layer % 2 == 0
        idx = layer // 2
        mod = (silu_c @ ada_w[layer] + ada_b[layer])[:, None, :]
        sh1, sc1, g1, sh2, sc2, g2 = jnp.split(mod, 6, axis=-1)
        h_lat = modulate(rms_norm(x, norm1_g[layer]), sh1, sc1)
        h_ctx = None
        if need_ctx or is_even:
            mod_c = silu_cc @ ada_w[layer] + ada_b[layer]
            csh1, csc1, cg1, csh2, csc2, cg2 = jnp.split(mod_c, 6, axis=-1)
            h_ctx = modulate(rms_norm(xc, norm1_g[layer]), csh1, csc1)
        if is_even:
            o_ctx, o_lat = mla_rglru_mixer(h_ctx, h_lat, cos, sin, ev_w_in[idx], mla_q_norm_g[idx],
                                           mla_kv_norm_g[idx], mla_w_uq[idx], mla_w_ukv[idx],
                                           lru_conv_w[idx], lru_conv_b[idx], lru_w_a[idx], lru_b_a[idx],
                                           lru_w_x[idx], lru_b_x[idx], lru_lambda[idx], ev_w_out[idx],
                                           need_ctx)
        else:
            hy_params = (od_w_in[idx], hy_conv_w[idx], hy_conv_b[idx], hy_w1[idx], hy_b1[idx],
                         hy_freq1[idx], hy_w2[idx], hy_b2[idx], hy_freq2[idx], hy_w3[idx],
                         hy_decay[idx], hy_skip[idx], od_w_out[idx])
            o_lat = hyena_operator(h_lat, *hy_params)
            o_ctx = hyena_operator(h_ctx, *hy_params) if need_ctx else None
        x = x + g1 * o_lat
        h_lat = modulate(rms_norm(x, norm2_g[layer]), sh2, sc2)
        if need_ctx:
            xc = xc + cg1 * o_ctx
            h_ctx = modulate(rms_norm(xc, norm2_g[layer]), csh2, csc2)
            m = grouped_moe(jnp.concatenate([h_ctx, h_lat], axis=1), router_w, router_b,
                            moe_w1[layer], moe_w3[layer], moe_w2[layer])
            xc = xc + cg2 * m[:, :n_ctx]
            m_lat = m[:, n_ctx:]
        else:
            m_lat = grouped_moe(h_lat, router_w, router_b, moe_w1[layer], moe_w3[layer], moe_w2[layer])
        x = x + g2 * m_lat
    return rms_norm(x, final_g)
```

```python
import numpy as np
import concourse.bass as bass
import concourse.mybir as mybir

F32 = mybir.dt.float32
BF16 = mybir.dt.bfloat16
I32 = mybir.dt.int32
ALU = mybir.AluOpType
AF = mybir.ActivationFunctionType
AX = mybir.AxisListType

SEM_ROLL = 30000


class Sem:
    def __init__(self, fw, name, is_dma):
        self.h = fw.nc.alloc_semaphore(name)
        self.total = 0
        self.is_dma = is_dma
        fw.all_sems.append(self)


class Buf:
    __slots__ = ("name", "writers", "pwriters", "readers", "dsem", "excl")

    def __init__(self, name="", excl=False):
        self.name = name
        self.excl = excl
        self.writers = {}
        self.pwriters = {}
        self.readers = {}
        self.dsem = None


class EngW:
    def __init__(self, fw, eng, name, is_pe=False):
        self.fw = fw
        self.eng = eng
        self.name = name
        self.is_pe = is_pe
        self.sem = Sem(fw, "e_" + name + "0", False)
        self.gen = 0
        self.waited = {}
        self.pending = False

    def wait_tok(self, sem, val):
        if sem.is_dma:
            val = sem.total
        if val <= 0:
            return
        if self.waited.get(sem, 0) >= val:
            return
        self.eng.wait_ge(sem.h, val)
        self.waited[sem] = val

    def roll(self):
        if self.sem.total >= SEM_ROLL and not self.pending:
            self.gen += 1
            self.sem = Sem(self.fw, "e_%s%d" % (self.name, self.gen), False)


class FW:
    def __init__(self, nc):
        self.nc = nc
        self.all_sems = []
        self.pe = EngW(self, nc.tensor, "pe", True)
        self.act = EngW(self, nc.scalar, "act")
        self.dve = EngW(self, nc.vector, "dve")
        self.pool = EngW(self, nc.gpsimd, "pool")
        self.sp = EngW(self, nc.sync, "sp")
        self.engs = [self.pe, self.act, self.dve, self.pool, self.sp]
        self.n_ins = 0
        self.free_dma = {False: [], True: []}

    def _deps(self, E, reads, writes, partial):
        def w(d, skip_same=False):
            for s, v in d.items():
                if E.is_pe and s is E.sem:
                    continue
                if skip_same and s is E.sem:
                    continue
                E.wait_tok(s, v)
        for b in reads:
            w(b.writers)
            w(b.pwriters)
            if b.excl:
                w(b.readers, skip_same=True)
        for b in writes:
            w(b.readers)
            w(b.writers)
            if not partial:
                w(b.pwriters)

    def _record(self, sem, val, reads, writes, partial):
        for b in writes:
            if not partial:
                b.writers = {sem: val}
                b.pwriters = {}
                b.readers = {}
            else:
                b.pwriters[sem] = val
        for b in reads:
            b.readers[sem] = val

    def op(self, E, fn, reads=(), writes=(), signal=True, partial=False):
        self._deps(E, reads, writes, partial)
        ins = fn()
        self.n_ins += 1
        if signal:
            ins.then_inc(E.sem.h, 1)
            E.sem.total += 1
            E.pending = False
            tokv = E.sem.total
        else:
            E.pending = True
            tokv = E.sem.total + 1
        self._record(E.sem, tokv, reads, writes, partial)
        if signal:
            E.roll()
        return ins

    def dma(self, Q, out, in_, reads=(), writes=(), primary=None, partial=False, **kw):
        self._deps(Q, reads, writes, partial)
        if primary is None:
            primary = writes[0] if writes else reads[0]
        if primary.dsem is None:
            primary.dsem = {}
        sw = Q is self.pool
        if sw not in primary.dsem or primary.dsem[sw].total >= SEM_ROLL:
            fl = self.free_dma[sw]
            while fl and fl[-1].total >= SEM_ROLL - 4000:
                fl.pop()
            if fl:
                primary.dsem[sw] = fl.pop()
            else:
                sm = Sem(self, "d%d" % len(self.all_sems), True)
                sm.sw = sw
                primary.dsem[sw] = sm
        ds = primary.dsem[sw]
        ins = Q.eng.dma_start(out=out, in_=in_, **kw)
        ins.then_inc(ds.h, 16)
        ds.total += 16
        self.n_ins += 1
        self._record(ds, ds.total, reads, writes, partial)
        return ins

    def barrier(self):
        for E in self.engs:
            for s in self.all_sems:
                if s.total > 0:
                    E.wait_tok(s, s.total)
        for s in self.all_sems:
            if s.is_dma and s.total < SEM_ROLL - 4000 and s not in self.free_dma[s.sw]:
                self.free_dma[s.sw].append(s)

    def final_wait(self, E):
        for s in self.all_sems:
            if s.total > 0:
                E.wait_tok(s, s.total)


import numpy as np
from contextlib import ExitStack
import concourse.bass as bass
import concourse.mybir as mybir
from concourse.bass_utils import run_bass_kernel_spmd

NT0 = 34
T0 = 4352
NCTX = 256
NLAT = 4096
D = 1024
EPS = 1e-6


def host_prep(inputs, b):
    f = lambda a: np.ascontiguousarray(a, dtype=np.float32)
    m = {}
    m["x"] = f(inputs["x"][b])
    m["ctx"] = f(inputs["ctx"][b])
    cc = np.stack([inputs["c"][b], inputs["c_ctx"]], axis=-1)
    m["ccT"] = f(cc.reshape(8, 128, 2).transpose(1, 0, 2))
    m["ada_w"] = f(inputs["ada_w"])
    m["ada_bT"] = f(inputs["ada_b"].reshape(2, 48, 128).transpose(0, 2, 1))
    m["n1gT"] = f(inputs["norm1_g"].reshape(2, 8, 128).transpose(0, 2, 1))
    m["n2gT"] = f(inputs["norm2_g"].reshape(2, 8, 128).transpose(0, 2, 1))
    m["final_g"] = f(inputs["final_g"].reshape(1, 1024))
    w_in = inputs["ev_w_in"][0]
    cq, ckv, kr, ux, ug = np.split(w_in, [384, 640, 672, 1184], axis=1)
    m["w_in"] = f(np.concatenate([ux, ug, cq, ckv, kr], axis=1))
    return m


class K:
    pass


class NCProxy:
    def __init__(self, nc):
        object.__setattr__(self, "_nc", nc)
        object.__setattr__(self, "_cnt", [0])

    def __getattr__(self, name):
        return getattr(self._nc, name)

    def sbuf_tensor(self, name, *a, **kw):
        self._cnt[0] += 1
        return self._nc.sbuf_tensor("%s_u%d" % (name, self._cnt[0]), *a, **kw)

    def psum_tensor(self, name, *a, **kw):
        self._cnt[0] += 1
        return self._nc.psum_tensor("%s_u%d" % (name, self._cnt[0]), *a, **kw)


def build(nc, dbg=None):
    nc = NCProxy(nc)
    fw = FW(nc)
    k = K()
    k.fw = fw
    k.nc = nc
    dbg = dbg or {}
    dram_in = lambda name, shape: nc.dram_tensor(name, list(shape), F32, kind="ExternalInput").ap()
    k.x = dram_in("x", [NLAT, D])
    k.ctx = dram_in("ctx", [NCTX, D])
    k.ccT = dram_in("ccT", [128, 8, 2])
    k.ada_w = dram_in("ada_w", [2, D, 6 * D])
    k.ada_bT = dram_in("ada_bT", [2, 128, 48])
    k.n1gT = dram_in("n1gT", [2, 128, 8])
    k.n2gT = dram_in("n2gT", [2, 128, 8])
    k.final_g = dram_in("final_g", [1, D])
    k.w_in = dram_in("w_in", [D, 1728])
    k.out = nc.dram_tensor("out", [NLAT, D], F32, kind="ExternalOutput").ap()
    k.dbg = {}
    for name, shape in dbg.items():
        k.dbg[name] = nc.dram_tensor("dbg_" + name, list(shape), F32, kind="ExternalOutput").ap()

    k.ident_bf = nc.alloc_sbuf_tensor("ident_bf", [128, 128], BF16)
    k.ident_f = nc.alloc_sbuf_tensor("ident_f", [128, 128], F32)
    k.ones_bf = nc.alloc_sbuf_tensor("ones_bf", [128, 128], BF16)
    k.ones_f = nc.alloc_sbuf_tensor("ones_f", [128, 128], F32)
    k.modT = nc.alloc_sbuf_tensor("modT", [128, 2, 48, 2], F32)
    k.gsT = nc.alloc_sbuf_tensor("gsT", [128, 2, 2, 8, 2], F32)
    k.nexp = nc.alloc_sbuf_tensor("nexp", [128, 1], F32)
    k.cbuf = Buf("consts")
    fw.op(fw.pool, lambda: nc.gpsimd.memset(k.ident_f[:], 0.0), writes=[k.cbuf])
    k.iota_p = nc.alloc_sbuf_tensor("iota_p", [128, 1], F32)
    k.iota_f = nc.alloc_sbuf_tensor("iota_f", [128, 128], F32)
    fw.op(fw.pool, lambda: nc.gpsimd.iota(k.iota_p[:], pattern=[[0, 1]], base=0, channel_multiplier=1,
                                          allow_small_or_imprecise_dtypes=True), writes=[k.cbuf], partial=True)
    fw.op(fw.pool, lambda: nc.gpsimd.iota(k.iota_f[:], pattern=[[1, 128]], base=0, channel_multiplier=0,
                                          allow_small_or_imprecise_dtypes=True), writes=[k.cbuf], partial=True)
    fw.op(fw.dve, lambda: nc.vector.tensor_scalar(out=k.ident_f[:], in0=k.iota_f[:], scalar1=k.iota_p[:, 0:1],
                                                  scalar2=None, op0=ALU.is_equal), reads=[k.cbuf], writes=[k.cbuf])
    fw.op(fw.dve, lambda: nc.vector.tensor_copy(out=k.ident_bf[:], in_=k.ident_f[:]), reads=[k.cbuf], writes=[k.cbuf], partial=True)
    fw.op(fw.dve, lambda: nc.vector.memset(k.ones_bf[:], 1.0), writes=[k.cbuf], partial=True)
    fw.op(fw.dve, lambda: nc.vector.memset(k.ones_f[:], 1.0), writes=[k.cbuf], partial=True)
    fw.op(fw.dve, lambda: nc.vector.memset(k.nexp[:], -0.5), writes=[k.cbuf], partial=True)
    return k


def stage_mod(k):
    nc, fw = k.nc, k.fw
    with ExitStack() as es:
        cc = es.enter_context(nc.sbuf_tensor("m_cc", [128, 8, 2], F32))
        sc = es.enter_context(nc.sbuf_tensor("m_sc", [128, 8, 2], F32))
        abT = es.enter_context(nc.sbuf_tensor("m_abT", [128, 2, 48], F32))
        gT = es.enter_context(nc.sbuf_tensor("m_gT", [128, 2, 2, 8], F32))
        NW = 768
        wst = [es.enter_context(nc.sbuf_tensor("m_w%d" % i, [128, 8, NW], F32)) for i in range(2)]
        ps = es.enter_context(nc.psum_tensor("m_ps", [128, 512], F32))
        b_cc, b_ab, b_g, b_ps = Buf("cc"), Buf("ab"), Buf("g"), Buf("mps", excl=True)
        b_w = [Buf("mw0"), Buf("mw1")]
        fw.dma(fw.sp, cc[:], k.ccT, writes=[b_cc])
        for l in range(2):
            fw.dma(fw.sp, abT[:, l, :], k.ada_bT[l], writes=[b_ab], partial=True)
            fw.dma(fw.sp, gT[:, l, 0, :], k.n1gT[l], writes=[b_g], partial=True)
            fw.dma(fw.sp, gT[:, l, 1, :], k.n2gT[l], writes=[b_g], partial=True)
        fw.op(fw.act, lambda: nc.scalar.activation(out=sc[:], in_=cc[:], func=AF.Silu), reads=[b_cc], writes=[b_cc])
        it = 0
        for l in range(2):
            for cg in range(8):
                slot = it % 2
                it += 1
                src = k.ada_w[l].rearrange("(kc p) n -> p kc n", p=128)[:, :, cg * NW:(cg + 1) * NW]
                fw.dma(fw.sp, wst[slot][:], src, writes=[b_w[slot]])
                for mi in range(NW // 128):
                    mc = cg * (NW // 128) + mi
                    for kc in range(8):
                        last = (kc == 7)
                        fw.op(fw.pe, lambda: nc.tensor.matmul(ps[:, mc * 2:mc * 2 + 2],
                                                              lhsT=wst[slot][:, kc, mi * 128:(mi + 1) * 128],
                                                              rhs=sc[:, kc, :], start=(kc == 0), stop=last),
                              reads=[b_w[slot], b_cc], writes=[b_ps], signal=last, partial=True)
            fw.op(fw.dve, lambda: nc.vector.tensor_tensor(out=k.modT[:, l, :, :],
                                                          in0=ps[:, 0:96].rearrange("p (c j) -> p c j", j=2),
                                                          in1=abT[:, l, :].unsqueeze(2).to_broadcast([128, 48, 2]),
                                                          op=ALU.add),
                  reads=[b_ps, b_ab], writes=[k.cbuf], partial=True)
            b_ps.readers[fw.dve.sem] = fw.dve.sem.total
            for w, c0 in ((0, 8), (1, 32)):
                fw.op(fw.dve, lambda: nc.vector.scalar_tensor_tensor(
                    out=k.gsT[:, l, w, :, :], in0=k.modT[:, l, c0:c0 + 8, :], scalar=1.0,
                    in1=gT[:, l, w, :].unsqueeze(2).to_broadcast([128, 8, 2]), op0=ALU.add, op1=ALU.mult),
                    reads=[k.cbuf, b_g], writes=[k.cbuf], partial=True)
        fw.barrier()


def norm_stats_xn(k, es_bufs, xt, b_x, xn, b_xn, tagbufs):
    nc, fw = k.nc, k.fw
    junk, ss, b_t = tagbufs
    fw.op(fw.act, lambda: nc.scalar.activation(out=junk[:], in_=xt, func=AF.Square, accum_out=ss[:, 0:1]),
          reads=[b_x], writes=[b_t])
    fw.op(fw.dve, lambda: nc.vector.tensor_scalar(out=ss[:, 1:2], in0=ss[:, 0:1], scalar1=1.0 / D, scalar2=EPS,
                                                  op0=ALU.mult, op1=ALU.add), reads=[b_t], writes=[b_t])
    fw.op(fw.pool, lambda: nc.gpsimd.tensor_tensor(out=ss[:, 2:3], in0=ss[:, 1:2], in1=k.nexp[:, 0:1], op=ALU.pow),
          reads=[b_t, k.cbuf], writes=[b_t])
    fw.op(fw.dve, lambda: nc.vector.tensor_scalar(out=xn, in0=xt, scalar1=ss[:, 2:3], scalar2=None, op0=ALU.mult),
          reads=[b_x, b_t], writes=[b_xn])


TBLK = [(0, 256)] + [(256 + 512 * i, 512) for i in range(8)]


def stage_l0_norm1(k, es):
    nc, fw = k.nc, k.fw
    k.hT = es.enter_context(nc.sbuf_tensor("hT", [128, 8, T0], BF16))
    k.b_hT = [Buf("hT%d" % i) for i in range(NT0)]
    with ExitStack() as es2:
        norm_transpose_pass(k, es2, 0, 0, NT0,
                            lambda i: (k.ctx[i * 128:(i + 1) * 128, :] if i < 2 else k.x[(i - 2) * 128:(i - 1) * 128, :]),
                            lambda i: 1 if i < 2 else 0, k.hT, k.b_hT, None)
        fw.barrier()


def norm_transpose_pass(k, es, layer, which, ntiles, src_fn, j_fn, hT, b_hT, src_buf):
    nc, fw = k.nc, k.fw
    NS = 4
    N2 = 4
    xts = [es.enter_context(nc.sbuf_tensor("nt_x%d" % i, [128, D], F32)) for i in range(NS)]
    xns = [es.enter_context(nc.sbuf_tensor("nt_xn%d" % i, [128, D], BF16)) for i in range(N2)]
    junks = [es.enter_context(nc.sbuf_tensor("nt_j%d" % i, [128, D], BF16)) for i in range(N2)]
    sss = [es.enter_context(nc.sbuf_tensor("nt_s%d" % i, [128, 4], F32)) for i in range(N2)]
    tps = [es.enter_context(nc.psum_tensor("nt_tp%d" % i, [128, D], BF16)) for i in range(N2)]
    b_x = [Buf() for _ in range(NS)]
    b_xn = [Buf() for _ in range(N2)]
    b_t = [Buf() for _ in range(N2)]
    b_tp = [Buf(excl=True) for _ in range(N2)]
    sh_c0 = 0 if which == 0 else 24
    for i in range(ntiles):
        s3, s2 = i % NS, i % N2
        j = j_fn(i)
        rd = [src_buf] if src_buf is not None else []
        fw.dma(fw.sp, xts[s3][:], src_fn(i), reads=rd, writes=[b_x[s3]], primary=b_x[s3])
        norm_stats_xn(k, None, xts[s3][:], b_x[s3], xns[s2][:], b_xn[s2], (junks[s2], sss[s2], b_t[s2]))
        for kc in range(8):
            fw.op(fw.pe, lambda: nc.tensor.transpose(out=tps[s2][:, kc * 128:(kc + 1) * 128],
                                                     in_=xns[s2][:, kc * 128:(kc + 1) * 128], identity=k.ident_bf[:]),
                  reads=[b_xn[s2], k.cbuf], writes=[b_tp[s2]], signal=(kc == 7), partial=(kc > 0))
        for kc in range(8):
            o = hT[:, kc, i * 128:(i + 1) * 128]
            src = tps[s2][:, kc * 128:(kc + 1) * 128]
            sc_ap = k.gsT[:, layer, which, kc, j:j + 1]
            bi_ap = k.modT[:, layer, sh_c0 + kc, j:j + 1]
            if i % 2 == 0:
                fw.op(fw.act, lambda: nc.scalar.activation(out=o, in_=src, func=AF.Identity, bias=bi_ap, scale=sc_ap),
                      reads=[b_tp[s2], k.cbuf], writes=[b_hT[i]], partial=(kc > 0))
            else:
                fw.op(fw.dve, lambda: nc.vector.tensor_scalar(out=o, in0=src, scalar1=sc_ap, scalar2=bi_ap,
                                                              op0=ALU.mult, op1=ALU.add),
                      reads=[b_tp[s2], k.cbuf], writes=[b_hT[i]], partial=(kc > 0))


def load_w_bf16(k, dst, src_ap, buf, nparts=1):
    k.fw.dma(k.fw.pool, dst, src_ap, writes=[buf], partial=True)


def stage_l0_inproj_test(k, es):
    nc, fw = k.nc, k.fw
    w = es.enter_context(nc.sbuf_tensor("s_w_in", [128, 8, 1696], BF16))
    b_w = Buf("w_in")
    for kc in range(8):
        load_w_bf16(k, w[:, kc, :], k.w_in[kc * 128:(kc + 1) * 128, :], b_w)
    pss = [es.enter_context(nc.psum_tensor("ip_ps%d" % i, [128, 512], F32)) for i in range(2)]
    b_ps = [Buf(excl=True), Buf(excl=True)]
    obs = [es.enter_context(nc.sbuf_tensor("ip_o%d" % i, [128, 512], F32)) for i in range(2)]
    b_o = [Buf(), Buf()]
    chunks = [(c * 128, 128) for c in range(13)] + [(1664, 32)]
    it = 0
    for (f0, fs) in chunks:
        for (t0, ts) in TBLK:
            s = it % 2
            it += 1
            tiles = range(t0 // 128, (t0 + ts) // 128)
            for kc in range(8):
                fw.op(fw.pe, lambda: nc.tensor.matmul(pss[s][:fs, :ts], lhsT=w[:, kc, f0:f0 + fs], rhs=k.hT[:, kc, t0:t0 + ts],
                                                      start=(kc == 0), stop=(kc == 7)),
                      reads=[b_w] + [k.b_hT[i] for i in tiles], writes=[b_ps[s]], signal=(kc == 7), partial=(kc > 0))
            fw.op(fw.act, lambda: nc.scalar.copy(out=obs[s][:fs, :ts], in_=pss[s][:fs, :ts]), reads=[b_ps[s]], writes=[b_o[s]])
            fw.dma(fw.sp, k.dbg["projT"][f0:f0 + fs, t0:t0 + ts], obs[s][:fs, :ts], reads=[b_o[s]])


def finish(k):
    k.fw.final_wait(k.fw.sp)


C1 = 0.7978845608028654
C2 = C1 * 0.044715


def host_prep2(inputs, b, m):
    f = lambda a: np.ascontiguousarray(a, dtype=np.float32)
    w_in = inputs["ev_w_in"][0]
    cq, ckv, kr, ux, ug = np.split(w_in, [384, 640, 672, 1184], axis=1)
    perm = np.arange(32) ^ 8
    m["w_in"] = f(np.concatenate([ux, ug, cq, ckv, kr, kr[:, perm]], axis=1))
    cw = inputs["lru_conv_w"][0]
    m["lru_cw"] = f(cw.reshape(4, 4, 128).transpose(2, 1, 0))
    m["lru_cb"] = f(inputs["lru_conv_b"][0].reshape(4, 128).T)
    wbd = np.zeros((2, 2, 4, 128, 128), np.float32)
    for wi, key in enumerate(("lru_w_a", "lru_w_x")):
        w = inputs[key][0]
        for d in range(2):
            for j in range(4):
                for hh in range(2):
                    wbd[wi, d, j, hh * 64:(hh + 1) * 64, hh * 64:(hh + 1) * 64] = w[d, 2 * j + hh]
    m["lru_wbd"] = f(wbd.transpose(3, 0, 1, 2, 4).reshape(128, 16, 128))
    vec = lambda a: f(a.reshape(2, 4, 128).transpose(2, 0, 1))
    m["lru_ba"] = vec(inputs["lru_b_a"][0])
    m["lru_bx"] = vec(inputs["lru_b_x"][0])
    m["lru_lam"] = vec(inputs["lru_lambda"][0])
    return m


def declare2(k):
    nc = k.nc
    din = lambda name, shape: nc.dram_tensor(name, list(shape), F32, kind="ExternalInput").ap()
    k.lru_cw = din("lru_cw", [128, 4, 4])
    k.lru_cb = din("lru_cb", [128, 4])
    k.lru_wbd = din("lru_wbd", [128, 16, 128])
    k.lru_ba = din("lru_ba", [128, 2, 4])
    k.lru_bx = din("lru_bx", [128, 2, 4])
    k.lru_lam = din("lru_lam", [128, 2, 4])
    k.mixT = nc.dram_tensor("mixT", [1024, T0], BF16, kind="Internal").ap()
    k.b_mixT = Buf("mixT")


def inproj_block(k, w, b_w, f0, fs, t0, ts, ps, b_ps):
    nc, fw = k.nc, k.fw
    tiles = range(t0 // 128, (t0 + ts) // 128)
    for kc in range(8):
        fw.op(fw.pe, lambda: nc.tensor.matmul(ps[:fs, :ts], lhsT=w[:, kc, f0:f0 + fs], rhs=k.hT[:, kc, t0:t0 + ts],
                                              start=(kc == 0), stop=(kc == 7)),
              reads=[b_w] + [k.b_hT[i] for i in tiles], writes=[b_ps], signal=(kc == 7), partial=(kc > 0))


def rev(t, t0, ts):
    return t[:, t0:t0 + ts][:, ::-1]


def ucol(t0):
    return 2 + t0 if t0 < NCTX else 261 + (t0 - NCTX)


def stage_l0_lru(k, es, w, b_w):
    nc, fw = k.nc, k.fw
    with ExitStack() as es2:
        sb = lambda name, shape, dt=F32: es2.enter_context(nc.sbuf_tensor(name, shape, dt))
        cw = sb("l_cw", [128, 4, 4]); cb = sb("l_cb", [128, 4])
        ba = sb("l_ba", [128, 2, 4]); bx = sb("l_bx", [128, 2, 4]); lam = sb("l_lam", [128, 2, 4])
        sp4 = sb("l_sp4", [128, 2, 4])
        wbd = sb("l_wbd", [128, 16, 128], BF16)
        phalf = sb("l_phalf", [128, 1])
        b_p = Buf("lru_params")
        for dst, src in ((cw, k.lru_cw), (cb, k.lru_cb), (ba, k.lru_ba), (bx, k.lru_bx), (lam, k.lru_lam)):
            fw.dma(fw.sp, dst[:], src, writes=[b_p], partial=True)
        fw.dma(fw.pool, wbd[:], k.lru_wbd, writes=[b_p], partial=True)
        fw.op(fw.dve, lambda: nc.vector.memset(phalf[:], 0.5), writes=[b_p], partial=True)
        fw.op(fw.dve, lambda: nc.vector.tensor_scalar(out=ba[:], in0=ba[:], scalar1=0.5, scalar2=None, op0=ALU.mult), reads=[b_p], writes=[b_p])
        fw.op(fw.dve, lambda: nc.vector.tensor_scalar(out=bx[:], in0=bx[:], scalar1=0.5, scalar2=None, op0=ALU.mult), reads=[b_p], writes=[b_p])
        fw.op(fw.act, lambda: nc.scalar.activation(out=sp4[:], in_=lam[:], func=AF.Exp, scale=-1.0), reads=[b_p], writes=[b_p])
        fw.op(fw.act, lambda: nc.scalar.activation(out=sp4[:], in_=sp4[:], func=AF.Ln, bias=1.0, scale=1.0), reads=[b_p], writes=[b_p])
        fw.op(fw.dve, lambda: nc.vector.tensor_scalar(out=sp4[:], in0=sp4[:], scalar1=-4.0, scalar2=None, op0=ALU.mult), reads=[b_p], writes=[b_p])
        UXW = 4358
        uxp = sb("l_uxp", [128, UXW]); b_uxp = Buf("uxp")
        u = sb("l_u", [128, T0]); b_u = Buf("u")
        ubf = sb("l_ubf", [128, T0], BF16); b_ubf = Buf("ubf")
        gg = sb("l_gg", [128, T0], BF16); b_gg = Buf("gg")
        NB = 2
        tmp = [[sb("l_t%d_%d" % (i, s), [128, 512]) for i in range(6)] for s in range(NB)]
        b_tmp = [[Buf() for i in range(6)] for s in range(NB)]
        lo = [sb("l_lo%d" % s, [128, 512], BF16) for s in range(NB)]
        b_lo = [Buf() for s in range(NB)]
        pss = [es2.enter_context(nc.psum_tensor("l_ps%d" % i, [128, 512], F32)) for i in range(4)]
        b_ps = [Buf(excl=True) for _ in range(4)]
        fw.op(fw.pool, lambda: nc.gpsimd.memset(uxp[:], 0.0), writes=[b_uxp])
        pi = 0
        it = 0
        for j in range(4):
            for (t0, ts) in TBLK:
                p = pi % 4; pi += 1
                inproj_block(k, w, b_w, j * 128, 128, t0, ts, pss[p], b_ps[p])
                c0 = ucol(t0)
                fw.op(fw.act, lambda: nc.scalar.copy(out=uxp[:, c0:c0 + ts], in_=pss[p][:, :ts]), reads=[b_ps[p]], writes=[b_uxp], partial=True)
            for (t0, ts) in TBLK:
                p = pi % 4; pi += 1
                s = it % NB; it += 1
                inproj_block(k, w, b_w, 512 + j * 128, 128, t0, ts, pss[p], b_ps[p])
                xg, sq, th = tmp[s][0], tmp[s][1], tmp[s][2]
                fw.op(fw.act, lambda: nc.scalar.copy(out=xg[:, :ts], in_=pss[p][:, :ts]), reads=[b_ps[p]], writes=[b_tmp[s][0]])
                fw.op(fw.dve, lambda: nc.vector.tensor_tensor(out=sq[:, :ts], in0=xg[:, :ts], in1=xg[:, :ts], op=ALU.mult), reads=[b_tmp[s][0]], writes=[b_tmp[s][1]])
                fw.op(fw.dve, lambda: nc.vector.tensor_scalar(out=sq[:, :ts], in0=sq[:, :ts], scalar1=C2, scalar2=C1, op0=ALU.mult, op1=ALU.add), reads=[b_tmp[s][1]], writes=[b_tmp[s][1]])
                fw.op(fw.dve, lambda: nc.vector.tensor_tensor(out=sq[:, :ts], in0=sq[:, :ts], in1=xg[:, :ts], op=ALU.mult), reads=[b_tmp[s][0], b_tmp[s][1]], writes=[b_tmp[s][1]])
                fw.op(fw.act, lambda: nc.scalar.activation(out=th[:, :ts], in_=sq[:, :ts], func=AF.Tanh), reads=[b_tmp[s][1]], writes=[b_tmp[s][2]])
                fw.op(fw.dve, lambda: nc.vector.scalar_tensor_tensor(out=gg[:, t0:t0 + ts], in0=th[:, :ts], scalar=1.0, in1=xg[:, :ts], op0=ALU.add, op1=ALU.mult),
                      reads=[b_tmp[s][2], b_tmp[s][0]], writes=[b_gg], partial=True)
            for (o0, n, base) in ((0, NCTX, 0), (NCTX, NLAT, 259)):
                fw.op(fw.act, lambda: nc.scalar.activation(out=u[:, o0:o0 + n], in_=uxp[:, base:base + n], func=AF.Identity,
                                                           bias=cb[:, j:j + 1], scale=cw[:, j, 0:1]),
                      reads=[b_uxp, b_p], writes=[b_u], partial=True)
                for tap in range(1, 4):
                    E = fw.dve if tap != 2 else fw.pool
                    eng = nc.vector if tap != 2 else nc.gpsimd
                    if tap != 2:
                        fw.op(fw.dve, lambda: nc.vector.scalar_tensor_tensor(out=u[:, o0:o0 + n], in0=uxp[:, base + tap:base + tap + n], scalar=cw[:, j, tap:tap + 1],
                                                                             in1=u[:, o0:o0 + n], op0=ALU.mult, op1=ALU.add),
                              reads=[b_uxp, b_p, b_u], writes=[b_u], partial=True)
                    else:
                        fw.op(fw.dve, lambda: nc.vector.scalar_tensor_tensor(out=u[:, o0:o0 + n], in0=uxp[:, base + tap:base + tap + n], scalar=cw[:, j, tap:tap + 1],
                                                                             in1=u[:, o0:o0 + n], op0=ALU.mult, op1=ALU.add),
                              reads=[b_uxp, b_p, b_u], writes=[b_u], partial=True)
            fw.op(fw.act, lambda: nc.scalar.copy(out=ubf[:], in_=u[:]), reads=[b_u], writes=[b_ubf])
            yb = uxp
            b_yb = b_uxp
            for d in (1, 0):
                order = [TBLK[0]] + (TBLK[1:] if d == 0 else TBLK[:0:-1])
                prev = None
                for bi, (t0, ts) in enumerate(order):
                    s = it % NB; it += 1
                    pa = pi % 4; pi += 1
                    px = pi % 4; pi += 1
                    ia = (0 * 2 + d) * 4 + j
                    ix = (1 * 2 + d) * 4 + j
                    fw.op(fw.pe, lambda: nc.tensor.matmul(pss[pa][:, :ts], lhsT=wbd[:, ia, :], rhs=ubf[:, t0:t0 + ts], start=True, stop=True),
                          reads=[b_p, b_ubf], writes=[b_ps[pa]])
                    fw.op(fw.pe, lambda: nc.tensor.matmul(pss[px][:, :ts], lhsT=wbd[:, ix, :], rhs=ubf[:, t0:t0 + ts], start=True, stop=True),
                          reads=[b_p, b_ubf], writes=[b_ps[px]])
                    tr, a, ti, om, iu, bb = tmp[s]
                    btr, bA, bti, bom, biu, bbb = b_tmp[s]
                    fw.op(fw.act, lambda: nc.scalar.activation(out=tr[:, :ts], in_=pss[pa][:, :ts], func=AF.Tanh, bias=ba[:, d, j:j + 1], scale=0.5), reads=[b_ps[pa], b_p], writes=[btr])
                    fw.op(fw.act, lambda: nc.scalar.activation(out=ti[:, :ts], in_=pss[px][:, :ts], func=AF.Tanh, bias=bx[:, d, j:j + 1], scale=0.5), reads=[b_ps[px], b_p], writes=[bti])
                    fw.op(fw.act, lambda: nc.scalar.activation(out=a[:, :ts], in_=tr[:, :ts], func=AF.Exp, bias=sp4[:, d, j:j + 1], scale=sp4[:, d, j:j + 1]), reads=[btr, b_p], writes=[bA])
                    fw.op(fw.dve, lambda: nc.vector.tensor_tensor(out=om[:, :ts], in0=a[:, :ts], in1=a[:, :ts], op=ALU.mult), reads=[bA], writes=[bom])
                    fw.op(fw.dve, lambda: nc.vector.tensor_scalar(out=om[:, :ts], in0=om[:, :ts], scalar1=-1.0, scalar2=1.0, op0=ALU.mult, op1=ALU.add), reads=[bom], writes=[bom])
                    fw.op(fw.dve, lambda: nc.vector.tensor_scalar_max(out=om[:, :ts], in0=om[:, :ts], scalar1=1e-30), reads=[bom], writes=[bom])
                    fw.op(fw.act, lambda: nc.scalar.activation(out=om[:, :ts], in_=om[:, :ts], func=AF.Sqrt), reads=[bom], writes=[bom])
                    fw.op(fw.dve, lambda: nc.vector.scalar_tensor_tensor(out=iu[:, :ts], in0=ti[:, :ts], scalar=1.0, in1=u[:, t0:t0 + ts], op0=ALU.add, op1=ALU.mult), reads=[bti, b_u], writes=[biu])
                    fw.op(fw.dve, lambda: nc.vector.scalar_tensor_tensor(out=bb[:, :ts], in0=om[:, :ts], scalar=0.5, in1=iu[:, :ts], op0=ALU.mult, op1=ALU.mult), reads=[bom, biu], writes=[bbb])
                    if d == 1:
                        c0 = t0
                        init = 0.0 if bi == 0 else yb[:, prev:prev + 1]
                        fw.op(fw.dve, lambda: nc.vector.tensor_tensor_scan(out=rev(yb, t0, ts), data0=rev(a, 0, ts), data1=rev(bb, 0, ts),
                                                                           initial=init, op0=ALU.mult, op1=ALU.add),
                              reads=[bA, bbb, b_yb], writes=[b_yb], partial=True)
                        prev = t0
                    else:
                        yf = tmp[s][0]
                        rd = [bA, bbb] + ([b_tmp[1 - s][0]] if bi > 0 else [])
                        init = 0.0 if bi == 0 else k._yf_last
                        fw.op(fw.dve, lambda: nc.vector.tensor_tensor_scan(out=yf[:, :ts], data0=a[:, :ts], data1=bb[:, :ts], initial=init, op0=ALU.mult, op1=ALU.add),
                              reads=rd, writes=[btr])
                        k._yf_last = yf[:, ts - 1:ts]
                        fw.op(fw.dve, lambda: nc.vector.tensor_tensor(out=om[:, :ts], in0=yf[:, :ts], in1=yb[:, t0:t0 + ts], op=ALU.add), reads=[btr, b_yb], writes=[bom])
                        fw.op(fw.dve, lambda: nc.vector.scalar_tensor_tensor(out=lo[s][:, :ts], in0=om[:, :ts], scalar=0.5, in1=gg[:, t0:t0 + ts], op0=ALU.mult, op1=ALU.mult),
                              reads=[bom, b_gg], writes=[b_lo[s]])
                        fw.dma(fw.sp, k.mixT[512 + j * 128:512 + (j + 1) * 128, t0:t0 + ts], lo[s][:, :ts], reads=[b_lo[s]], writes=[k.b_mixT], primary=b_lo[s], partial=True)
            if j < 3:
                fw.op(fw.pool, lambda: nc.gpsimd.memset(uxp[:], 0.0), writes=[b_uxp])
        fw.barrier()


import math

SM_SCALE = 96.0 ** -0.5


def host_prep3(inputs, b, m):
    f = lambda a: np.ascontiguousarray(a, dtype=np.float32)
    perm = np.arange(32) ^ 8
    m["gq"] = f(inputs["mla_q_norm_g"][0].reshape(3, 128).T)
    m["gkv"] = f(inputs["mla_kv_norm_g"][0].reshape(2, 128).T)
    wq = inputs["mla_w_uq"][0].reshape(384, 8, 96)
    A = wq
    Bm = np.concatenate([np.zeros((384, 8, 64), np.float32), wq[:, :, 64:96][:, :, perm]], axis=2)
    wqAB = np.concatenate([A.reshape(384, 768), Bm.reshape(384, 768)], axis=1)
    m["w_uq"] = f(wqAB.reshape(3, 128, 1536).transpose(1, 0, 2))
    wkv = inputs["mla_w_ukv"][0].reshape(256, 8, 128)
    wk = wkv[:, :, 0:64]
    wv = wkv[:, :, 64:128]
    wkv2 = np.concatenate([wk.reshape(256, 512), wv.reshape(256, 512)], axis=1)
    m["w_ukv"] = f(wkv2.reshape(2, 128, 1024).transpose(1, 0, 2))
    t = np.arange(NLAT)
    pos = np.stack([t // 64, t % 64], axis=0).astype(np.float32)
    inv = (10000.0 ** (-np.arange(8, dtype=np.float32) / 8)).astype(np.float32)
    C = np.ones((32, T0), np.float32); S = np.zeros((32, T0), np.float32)
    for axis in range(2):
        for ab in range(2):
            for p in range(8):
                r = axis * 16 + ab * 8 + p
                ang = (pos[axis] * inv[p]).astype(np.float32)
                C[r, NCTX:] = np.cos(ang)
                S[r, NCTX:] = np.sin(ang) * (-1.0 if ab == 0 else 1.0)
    m["ropeC"] = f(C); m["ropeS"] = f(S)
    return m


def declare3(k):
    nc = k.nc
    din = lambda name, shape: nc.dram_tensor(name, list(shape), F32, kind="ExternalInput").ap()
    k.gq = din("gq", [128, 3]); k.gkv = din("gkv", [128, 2])
    k.w_uq = din("w_uq", [128, 3, 1536]); k.w_ukv = din("w_ukv", [128, 2, 1024])
    k.ropeC = din("ropeC", [32, T0]); k.ropeS = din("ropeS", [32, T0])


def alloc_mla_lat(k, es):
    nc = k.nc
    k.cqTd = nc.dram_tensor("cqTd", [384, T0], BF16, kind="Internal").ap()
    k.ckvTd = nc.dram_tensor("ckvTd", [256, T0], BF16, kind="Internal").ap()
    k.krrd = nc.dram_tensor("krrd", [32, T0], BF16, kind="Internal").ap()
    k.b_latd = Buf("latd")


def load_rope(k, es):
    nc, fw = k.nc, k.fw
    sbp = lambda name, shape, dt=F32: es.enter_context(nc.sbuf_tensor(name, shape, dt))
    k.rC = sbp("ropeC_s", [96, T0], BF16); k.rS = sbp("ropeS_s", [96, T0], BF16); k.b_rope = Buf("rope")
    fw.dma(fw.pool, k.rC[64:96, :], k.ropeC, writes=[k.b_rope], partial=True)
    fw.dma(fw.pool, k.rS[64:96, :], k.ropeS, writes=[k.b_rope], partial=True)


def stage_l0_mla_lat(k, w, b_w):
    nc, fw = k.nc, k.fw
    with ExitStack() as es2:
        sb = lambda name, shape, dt=F32: es2.enter_context(nc.sbuf_tensor(name, shape, dt))
        load_rope(k, es2)
        cqb = [sb("p3_cq%d" % i, [128, 3, 512], BF16) for i in range(2)]; b_cqb = [Buf() for _ in range(2)]
        ckvb = [sb("p3_ckv%d" % i, [128, 2, 512], BF16) for i in range(2)]; b_ckvb = [Buf() for _ in range(2)]
        krb = [sb("p3_kr%d" % i, [96, 512], BF16) for i in range(2)]; b_krb = [Buf() for _ in range(2)]
        pss = [es2.enter_context(nc.psum_tensor("p3_ps%d" % i, [128, 512], F32)) for i in range(4)]
        b_ps = [Buf(excl=True) for _ in range(4)]
        pq = [es2.enter_context(nc.psum_tensor("p3_pq%d" % i, [128, 512], F32)) for i in range(2)]
        b_pq = [Buf(excl=True) for _ in range(2)]
        sqt = [sb("p3_sq%d" % i, [128, 512], BF16) for i in range(3)]; b_sq = [Buf() for _ in range(3)]
        rr = [sb("p3_rr%d" % i, [128, 512]) for i in range(2)]; b_rr = [Buf() for _ in range(2)]
        t1 = [sb("p3_t1_%d" % i, [96, 512]) for i in range(2)]; t2 = [sb("p3_t2_%d" % i, [96, 512]) for i in range(2)]
        b_t1 = [Buf() for _ in range(2)]; b_t2 = [Buf() for _ in range(2)]
        pi = 0; si = 0; qi = 0
        for bi, (t0, ts) in enumerate(TBLK):
            bs = bi % 2
            for (dstt, b_dst, f_base, nch, width, dd) in ((cqb[bs], b_cqb[bs], 1024, 3, 384.0, k.cqTd), (ckvb[bs], b_ckvb[bs], 1408, 2, 256.0, k.ckvTd)):
                q = qi % 2; qi += 1
                for c in range(nch):
                    p = pi % 4; pi += 1
                    s = si % 3; si += 1
                    inproj_block(k, w, b_w, f_base + c * 128, 128, t0, ts, pss[p], b_ps[p])
                    fw.op(fw.act, lambda: nc.scalar.copy(out=dstt[:, c, :ts], in_=pss[p][:, :ts]), reads=[b_ps[p]], writes=[b_dst], partial=(c > 0))
                    fw.op(fw.act, lambda: nc.scalar.activation(out=sqt[s][:, :ts], in_=pss[p][:, :ts], func=AF.Square), reads=[b_ps[p]], writes=[b_sq[s]])
                    fw.op(fw.pe, lambda: nc.tensor.matmul(pq[q][:, :ts], lhsT=k.ones_bf[:], rhs=sqt[s][:, :ts], start=(c == 0), stop=(c == nch - 1)),
                          reads=[b_sq[s], k.cbuf], writes=[b_pq[q]], signal=(c == nch - 1), partial=(c > 0))
                fw.op(fw.dve, lambda: nc.vector.tensor_scalar(out=rr[q][:, :ts], in0=pq[q][:, :ts], scalar1=1.0 / width, scalar2=EPS, op0=ALU.mult, op1=ALU.add),
                      reads=[b_pq[q]], writes=[b_rr[q]])
                fw.op(fw.act, lambda: nc.scalar.activation(out=rr[q][:, :ts], in_=rr[q][:, :ts], func=AF.Sqrt), reads=[b_rr[q]], writes=[b_rr[q]])
                fw.op(fw.dve, lambda: nc.vector.reciprocal(out=rr[q][:, :ts], in_=rr[q][:, :ts]), reads=[b_rr[q]], writes=[b_rr[q]])
                for c in range(nch):
                    fw.op(fw.dve, lambda: nc.vector.tensor_tensor(out=dstt[:, c, :ts], in0=dstt[:, c, :ts], in1=rr[q][:, :ts], op=ALU.mult),
                          reads=[b_rr[q], b_dst], writes=[b_dst], partial=True)
                fw.dma(fw.sp, dd[:, t0:t0 + ts].rearrange("(c p) t -> p c t", p=128), dstt[:, :, :ts], reads=[b_dst], writes=[k.b_latd], primary=b_dst, partial=True)
            pa = pi % 4; pi += 1
            pb = pi % 4; pi += 1
            q = qi % 2
            inproj_block(k, w, b_w, 1600, 96, t0, ts, pss[pa], b_ps[pa])
            inproj_block(k, w, b_w, 1632, 96, t0, ts, pss[pb], b_ps[pb])
            fw.op(fw.dve, lambda: nc.vector.tensor_tensor(out=t1[q][64:96, :ts], in0=pss[pa][64:96, :ts], in1=k.rC[64:96, t0:t0 + ts], op=ALU.mult), reads=[b_ps[pa], k.b_rope], writes=[b_t1[q]])
            fw.op(fw.dve, lambda: nc.vector.tensor_tensor(out=t2[q][64:96, :ts], in0=pss[pb][64:96, :ts], in1=k.rS[64:96, t0:t0 + ts], op=ALU.mult), reads=[b_ps[pb], k.b_rope], writes=[b_t2[q]])
            fw.op(fw.dve, lambda: nc.vector.tensor_tensor(out=krb[bs][64:96, :ts], in0=t1[q][64:96, :ts], in1=t2[q][64:96, :ts], op=ALU.add), reads=[b_t1[q], b_t2[q]], writes=[b_krb[bs]])
            fw.dma(fw.sp, k.krrd[:, t0:t0 + ts], krb[bs][64:96, :ts], reads=[b_krb[bs]], writes=[k.b_latd], primary=b_krb[bs], partial=True)
        fw.barrier()


def stage_l0_attn(k, es):
    nc, fw = k.nc, k.fw
    with ExitStack() as es2:
        sb = lambda name, shape, dt=F32: es2.enter_context(nc.sbuf_tensor(name, shape, dt))
        k.cqT = sb("cqT", [128, 3, T0], BF16); k.ckvT = sb("ckvT", [128, 2, T0], BF16); k.krr = sb("krr", [96, T0], BF16)
        b_lat = Buf("lat")
        k.b_cq = [b_lat for _ in TBLK]; k.b_ckv = [b_lat for _ in TBLK]; k.b_krr = b_lat
        fw.dma(fw.sp, k.cqT[:], k.cqTd.rearrange("(c p) t -> p c t", p=128), reads=[k.b_latd], writes=[b_lat], partial=True)
        fw.dma(fw.sp, k.ckvT[:], k.ckvTd.rearrange("(c p) t -> p c t", p=128), reads=[k.b_latd], writes=[b_lat], partial=True)
        fw.dma(fw.sp, k.krr[64:96, :], k.krrd, reads=[k.b_latd], writes=[b_lat], partial=True)
        load_rope(k, es2)
        gq = sb("a_gq", [128, 3]); gkv = sb("a_gkv", [128, 2])
        wq = sb("a_wq", [128, 3, 1536], BF16)
        wkv = sb("a_wkv", [128, 2, 1024], BF16)
        b_g, b_wst, b_wq, b_wkv = Buf(), Buf(), Buf(), Buf()
        fw.dma(fw.sp, gq[:], k.gq, writes=[b_g], partial=True)
        fw.dma(fw.sp, gkv[:], k.gkv, writes=[b_g], partial=True)
        with ExitStack() as es3:
            wst = es3.enter_context(nc.sbuf_tensor("a_wst", [128, 3, 1536], F32))
            fw.dma(fw.sp, wst[:, :, :], k.w_uq, writes=[b_wst])
            for c in range(3):
                fw.op(fw.dve, lambda: nc.vector.tensor_scalar(out=wq[:, c, :], in0=wst[:, c, :], scalar1=gq[:, c:c + 1], scalar2=SM_SCALE, op0=ALU.mult, op1=ALU.mult),
                      reads=[b_wst, b_g], writes=[b_wq], partial=True)
            fw.dma(fw.sp, wst[:, 0:2, 0:1024], k.w_ukv, reads=[], writes=[b_wst])
            for c in range(2):
                fw.op(fw.dve, lambda: nc.vector.tensor_scalar(out=wkv[:, c, :], in0=wst[:, c, 0:1024], scalar1=gkv[:, c:c + 1], scalar2=None, op0=ALU.mult),
                      reads=[b_wst, b_g], writes=[b_wkv], partial=True)
            fw.barrier()
        Vp = sb("a_Vp", [128, NT0, 4, 192], BF16); b_V = Buf("Vp")
        fw.op(fw.pool, lambda: nc.gpsimd.memset(Vp[:], 0.0), writes=[b_V])
        fw.op(fw.pool, lambda: nc.gpsimd.memset(Vp[:, :, :, 64:65], 1.0), writes=[b_V], partial=True)
        NPS = 3
        psS = [es2.enter_context(nc.psum_tensor("a_pS%d" % i, [128, 512], F32)) for i in range(NPS)]; b_pS = [Buf(excl=True) for _ in range(NPS)]
        psO = [es2.enter_context(nc.psum_tensor("a_pO%d" % i, [128, 512], F32)) for i in range(2)]; b_pO = [Buf(excl=True) for _ in range(2)]
        psP = [es2.enter_context(nc.psum_tensor("a_pP%d" % i, [128, 512], F32)) for i in range(2)]; b_pP = [Buf(excl=True) for _ in range(2)]
        psB = es2.enter_context(nc.psum_tensor("a_pB", [128, 512], F32)); b_pB = Buf(excl=True)
        ppi = 0
        for i in range(NT0):
            p = ppi % 2; ppi += 1
            blk = 0 if i < 2 else 1 + (i - 2) // 4
            for c in range(2):
                fw.op(fw.pe, lambda: nc.tensor.matmul(psP[p][:, :], lhsT=k.ckvT[:, c, i * 128:(i + 1) * 128], rhs=wkv[:, c, 512:1024], start=(c == 0), stop=(c == 1)),
                      reads=[k.b_ckv[blk], b_wkv], writes=[b_pP[p]], signal=(c == 1), partial=(c > 0))
            src = psP[p][:, :].rearrange("p (pr two d) -> p pr two d", two=2, d=64)
            fw.op(fw.dve, lambda: nc.vector.tensor_copy(out=Vp[:, i, :, 0:64], in_=src[:, :, 0, :]), reads=[b_pP[p]], writes=[b_V], partial=True)
            fw.op(fw.dve, lambda: nc.vector.tensor_copy(out=Vp[:, i, :, 128:192], in_=src[:, :, 1, :]), reads=[b_pP[p]], writes=[b_V], partial=True)
        qT = [sb("a_qT%d" % i, [96, T0], BF16) for i in range(2)]; b_qT = [Buf() for _ in range(2)]
        kT = [sb("a_kT%d" % i, [96, T0], BF16) for i in range(2)]; b_kT = [Buf() for _ in range(2)]
        NPT = 3
        PT = [sb("a_PT%d" % i, [128, 512], BF16) for i in range(NPT)]; b_PT = [Buf() for _ in range(NPT)]
        t1 = [sb("a_t1_0", [96, 512])] * 2; t2 = [sb("a_t2_0", [96, 512])] * 2
        b_t1 = [Buf()] * 2; b_t2 = [Buf()] * 2
        den = [sb("a_den0", [128, 512])] * 2; b_den = [Buf()] * 2
        bcs = [sb("a_bc0", [128, 512])] * 2; b_bc = [Buf()] * 2
        ao = [sb("a_ao%d" % i, [128, 512], BF16) for i in range(2)]; b_ao = [Buf() for _ in range(2)]
        ti = 0; si = 0; pti = 0; oi = 0

        def build_head(h):
            nonlocal ppi, ti
            hs = h % 2
            fw.op(fw.dve, lambda: nc.vector.tensor_copy(out=kT[hs][64:96, :], in_=k.krr[64:96, :]), reads=[k.b_krr], writes=[b_kT[hs]])
            for bi, (t0, ts) in enumerate(TBLK):
                p = ppi % 2; ppi += 1
                for c in range(2):
                    fw.op(fw.pe, lambda: nc.tensor.matmul(psP[p][:64, :ts], lhsT=wkv[:, c, h * 64:(h + 1) * 64], rhs=k.ckvT[:, c, t0:t0 + ts], start=(c == 0), stop=(c == 1)),
                          reads=[k.b_ckv[bi], b_wkv], writes=[b_pP[p]], signal=(c == 1), partial=(c > 0))
                fw.op(fw.dve, lambda: nc.vector.tensor_copy(out=kT[hs][0:64, t0:t0 + ts], in_=psP[p][0:64, :ts]), reads=[b_pP[p]], writes=[b_kT[hs]], partial=True)
            first = True
            for bi, (t0, ts) in enumerate(TBLK):
                pa = ppi % 2; ppi += 1
                t = ti % 2; ti += 1
                for c in range(3):
                    fw.op(fw.pe, lambda: nc.tensor.matmul(psP[pa][:96, :ts], lhsT=wq[:, c, h * 96:(h + 1) * 96], rhs=k.cqT[:, c, t0:t0 + ts], start=(c == 0), stop=(c == 2)),
                          reads=[k.b_cq[bi], b_wq], writes=[b_pP[pa]], signal=(c == 2), partial=(c > 0))
                for c in range(3):
                    fw.op(fw.pe, lambda: nc.tensor.matmul(psB[:96, :ts], lhsT=wq[:, c, 768 + h * 96:768 + (h + 1) * 96], rhs=k.cqT[:, c, t0:t0 + ts], start=(c == 0), stop=(c == 2)),
                          reads=[k.b_cq[bi], b_wq], writes=[b_pB], signal=(c == 2), partial=(c > 0))
                fw.op(fw.dve, lambda: nc.vector.tensor_copy(out=qT[hs][0:64, t0:t0 + ts], in_=psP[pa][0:64, :ts]), reads=[b_pP[pa]], writes=[b_qT[hs]], partial=not first)
                first = False
                fw.op(fw.dve, lambda: nc.vector.tensor_tensor(out=t1[t][64:96, :ts], in0=psP[pa][64:96, :ts], in1=k.rC[64:96, t0:t0 + ts], op=ALU.mult), reads=[b_pP[pa], k.b_rope], writes=[b_t1[t]])
                fw.op(fw.dve, lambda: nc.vector.tensor_tensor(out=t2[t][64:96, :ts], in0=psB[64:96, :ts], in1=k.rS[64:96, t0:t0 + ts], op=ALU.mult), reads=[b_pB, k.b_rope], writes=[b_t2[t]])
                fw.op(fw.dve, lambda: nc.vector.tensor_tensor(out=qT[hs][64:96, t0:t0 + ts], in0=t1[t][64:96, :ts], in1=t2[t][64:96, :ts], op=ALU.add), reads=[b_t1[t], b_t2[t]], writes=[b_qT[hs]], partial=True)

        def attend(h, q0, qn, ktiles):
            nonlocal si, pti, oi
            hs = h % 2; pr = h // 2; odd = h % 2
            o = oi % 2; oi += 1
            M = 128 if odd else 65
            c0 = 64 if odd else 0
            nk = len(ktiles)
            slots = {}

            def S_mm(idx):
                nonlocal si
                kt = ktiles[idx]
                s = si % NPS; si += 1
                slots[idx] = s
                fw.op(fw.pe, lambda: nc.tensor.matmul(psS[s][:, :qn], lhsT=kT[hs][:, kt * 128:(kt + 1) * 128], rhs=qT[hs][:, q0:q0 + qn], start=True, stop=True),
                      reads=[b_kT[hs], b_qT[hs]], writes=[b_pS[s]])

            def EX_PV(idx):
                nonlocal pti
                kt = ktiles[idx]
                s = slots.pop(idx)
                pt = pti % NPT; pti += 1
                fw.op(fw.act, lambda: nc.scalar.activation(out=PT[pt][:, :qn], in_=psS[s][:, :qn], func=AF.Exp), reads=[b_pS[s]], writes=[b_PT[pt]])
                fw.op(fw.pe, lambda: nc.tensor.matmul(psO[o][:M, :qn], lhsT=Vp[:, kt, pr, c0:c0 + M], rhs=PT[pt][:, :qn], start=(idx == 0), stop=(idx == nk - 1)),
                      reads=[b_V, b_PT[pt]], writes=[b_pO[o]], signal=(idx == nk - 1), partial=(idx > 0))

            LOOK = 2
            for idx in range(min(LOOK, nk)):
                S_mm(idx)
            for idx in range(nk):
                if idx + LOOK < nk:
                    S_mm(idx + LOOK)
                EX_PV(idx)
            dr = 0 if odd else 64
            r0 = 64 if odd else 0
            MB = 128 if odd else 64
            fw.op(fw.dve, lambda: nc.vector.reciprocal(out=den[o][dr:dr + 1, :qn], in_=psO[o][dr:dr + 1, :qn]), reads=[b_pO[o]], writes=[b_den[o]])
            fw.op(fw.pe, lambda: nc.tensor.matmul(psB[:MB, :qn], lhsT=k.ones_f[dr:dr + 1, 0:MB], rhs=den[o][dr:dr + 1, :qn], start=True, stop=True),
                  reads=[b_den[o], k.cbuf], writes=[b_pB])
            fw.op(fw.dve, lambda: nc.vector.tensor_copy(out=bcs[o][r0:r0 + 64, :qn], in_=psB[r0:r0 + 64, :qn]), reads=[b_pB], writes=[b_bc[o]])
            fw.op(fw.dve, lambda: nc.vector.tensor_tensor(out=ao[o][r0:r0 + 64, :qn], in0=psO[o][r0:r0 + 64, :qn], in1=bcs[o][r0:r0 + 64, :qn], op=ALU.mult),
                  reads=[b_pO[o], b_bc[o]], writes=[b_ao[o]])
            fw.dma(fw.sp, k.mixT[h * 64:(h + 1) * 64, q0:q0 + qn], ao[o][r0:r0 + 64, :qn], reads=[b_ao[o]], writes=[k.b_mixT], primary=b_ao[o], partial=True)

        NH = k.__dict__.get("dbg_nheads", 8)
        build_head(0)
        for h in range(NH):
            if h + 1 < NH:
                build_head(h + 1)
            for qb in range(k.__dict__.get("dbg_nqb", 8)):
                attend(h, NCTX + qb * 512, 512, list(range(NT0)))
        fw.barrier()


NTL = 32


def host_prep4(inputs, b, m):
    f = lambda a: np.ascontiguousarray(a, dtype=np.float32)
    m["w_out0"] = f(inputs["ev_w_out"][0])
    m["w_out1"] = f(inputs["od_w_out"][0])
    m["router_w"] = f(inputs["router_w"].reshape(8, 128, 16).transpose(1, 0, 2))
    m["router_b"] = f(inputs["router_b"].reshape(1, 16))
    m["moe_w1"] = f(inputs["moe_w1"]); m["moe_w3"] = f(inputs["moe_w3"]); m["moe_w2"] = f(inputs["moe_w2"])
    return m


def declare4(k):
    nc = k.nc
    din = lambda name, shape: nc.dram_tensor(name, list(shape), F32, kind="ExternalInput").ap()
    k.w_out = [din("w_out0", [D, D]), din("w_out1", [D, D])]
    k.router_w = din("router_w", [128, 8, 16]); k.router_b = din("router_b", [1, 16])
    k.moe_w1 = din("moe_w1", [2, 16, D, 512]); k.moe_w3 = din("moe_w3", [2, 16, D, 512]); k.moe_w2 = din("moe_w2", [2, 16, 512, D])
    k.xmid = nc.dram_tensor("xmid", [NLAT, D], F32, kind="Internal").ap(); k.b_xmid = Buf("xmid")
    k.xend = nc.dram_tensor("xend", [NLAT, D], F32, kind="Internal").ap(); k.b_xend = Buf("xend")
    k.h2T = nc.dram_tensor("h2T", [D, NLAT], BF16, kind="Internal").ap(); k.b_h2T = Buf("h2T")
    k.gates = nc.alloc_sbuf_tensor("gates", [128, NTL, 16], F32); k.b_gates = Buf("gates")
    k.rb = nc.alloc_sbuf_tensor("rb_bc", [128, 16], F32)
    k.rw = nc.alloc_sbuf_tensor("rw_bf", [128, 8, 16], BF16)
    k.g1b = nc.alloc_sbuf_tensor("g1b", [128, D], F32)
    k.g2b = nc.alloc_sbuf_tensor("g2b", [128, D], F32)
    k.b_gb = Buf("gb")
    fw = k.fw
    fw.dma(fw.sp, k.rb[:], k.router_b.to_broadcast([128, 16]), writes=[k.cbuf], partial=True)
    fw.dma(fw.pool, k.rw[:], k.router_w, writes=[k.cbuf], partial=True)


def make_bcast(k, es, layer, chunk0, j, dst, b_dst):
    nc, fw = k.nc, k.fw
    with ExitStack() as es2:
        dg = es2.enter_context(nc.sbuf_tensor("bc_dg", [128, 128], F32)); b_dg = Buf()
        ps = [es2.enter_context(nc.psum_tensor("bc_ps%d" % i, [128, 512], F32)) for i in range(2)]
        b_ps = [Buf(excl=True) for _ in range(2)]
        for c in range(8):
            fw.op(fw.dve, lambda: nc.vector.tensor_scalar(out=dg[:], in0=k.ident_f[:], scalar1=k.modT[:, layer, chunk0 + c, j:j + 1], scalar2=None, op0=ALU.mult),
                  reads=[k.cbuf], writes=[b_dg])
            hb = c // 4
            fw.op(fw.pe, lambda: nc.tensor.matmul(ps[hb][:, (c % 4) * 128:(c % 4 + 1) * 128], lhsT=k.ones_f[:], rhs=dg[:], start=True, stop=True),
                  reads=[b_dg, k.cbuf], writes=[b_ps[hb]], partial=(c % 4 > 0))
        for hb in range(2):
            fw.op(fw.dve, lambda: nc.vector.tensor_copy(out=dst[:, hb * 512:(hb + 1) * 512], in_=ps[hb][:]), reads=[b_ps[hb]], writes=[b_dst], partial=(hb > 0))
        fw.barrier()


def stage_outproj_norm2(k, layer, mix_dram, b_mix, mix_col0, x_row_ap, mix_tm=False):
    nc, fw = k.nc, k.fw
    with ExitStack() as es:
        make_bcast(k, es, layer, 16, 0, k.g1b, k.b_gb)
        sb = lambda name, shape, dt=F32: es.enter_context(nc.sbuf_tensor(name, shape, dt))
        wo = sb("o_w", [128, 8, D], BF16); b_wo = Buf()
        for c in range(8):
            fw.dma(fw.pool, wo[:, c, :], k.w_out[layer][c * 128:(c + 1) * 128, :], writes=[b_wo], partial=True)
        NS = 4
        NP = 2
        mts = [sb("o_mt%d" % i, [128, 8, 128], BF16) for i in range(NS)]; b_mt = [Buf() for _ in range(NS)]
        xts = [sb("o_xt%d" % i, [128, D]) for i in range(NS)]; b_xt = [Buf() for _ in range(NS)]
        xms = [sb("o_xm%d" % i, [128, D]) for i in range(NS)]; b_xm = [Buf() for _ in range(NS)]
        xns = [sb("o_xn%d" % i, [128, D], BF16) for i in range(NS)]; b_xn = [Buf() for _ in range(NS)]
        junks = [sb("o_j%d" % i, [128, D], BF16) for i in range(NS)]
        sss = [sb("o_s%d" % i, [128, 4]) for i in range(NS)]; b_t = [Buf() for _ in range(NS)]
        h2s = [sb("o_h2%d" % i, [128, 8, 128], BF16) for i in range(NS)]; b_h2 = [Buf() for _ in range(NS)]
        gt = [sb("o_gt%d" % i, [128, 8, 16]) for i in range(NS)]; b_gt = [Buf() for _ in range(NS)]
        _po = [[es.enter_context(nc.psum_tensor("o_po%d_%d" % (i, hb), [128, 512], F32)) for hb in range(2)] for i in range(NP)]
        _b_po = [[Buf(excl=True) for hb in range(2)] for i in range(NP)]
        _tps = [es.enter_context(nc.psum_tensor("o_tp%d" % i, [128, D], BF16)) for i in range(NP + 2)]; _b_tp = [Buf(excl=True) for _ in range(NP + 2)]
        po = [_po[i % NP] for i in range(NS)]; b_po = [_b_po[i % NP] for i in range(NS)]
        plg = [_po[i % NP][0] for i in range(NS)]; b_lg = [_b_po[i % NP][0] for i in range(NS)]
        tps = _tps; b_tp = _b_tp

        def partA(i):
            s = i % NS
            c0 = mix_col0 + i * 128
            if not mix_tm:
                fw.dma(fw.sp, mts[s][:], mix_dram[:, c0:c0 + 128].rearrange("(c p) t -> p c t", p=128), reads=[b_mix], writes=[b_mt[s]], primary=b_mt[s])
            else:
                fw.dma(fw.sp, xns[s][:], mix_dram[i * 128:(i + 1) * 128, :], reads=[b_mix], writes=[b_xn[s]], primary=b_xn[s])
                for kc in range(8):
                    fw.op(fw.pe, lambda: nc.tensor.transpose(out=tps[s][:, kc * 128:(kc + 1) * 128], in_=xns[s][:, kc * 128:(kc + 1) * 128], identity=k.ident_bf[:]),
                          reads=[b_xn[s], k.cbuf], writes=[b_tp[s]], signal=(kc == 7), partial=(kc > 0))
                fw.op(fw.dve, lambda: nc.vector.tensor_copy(out=mts[s][:].rearrange("p a b -> p (a b)"), in_=tps[s][:]), reads=[b_tp[s]], writes=[b_mt[s]])
            fw.dma(fw.sp, xts[s][:], x_row_ap(i), reads=[k.b_xend], writes=[b_xt[s]], primary=b_xt[s])
            for hb in range(2):
                for c in range(8):
                    fw.op(fw.pe, lambda: nc.tensor.matmul(po[s][hb][:, :], lhsT=mts[s][:, c, :], rhs=wo[:, c, hb * 512:(hb + 1) * 512], start=(c == 0), stop=(c == 7)),
                          reads=[b_mt[s], b_wo], writes=[b_po[s][hb]], signal=(c == 7), partial=(c > 0))
            for hb in range(2):
                sl = slice(hb * 512, (hb + 1) * 512)
                fw.op(fw.dve, lambda: nc.vector.tensor_tensor(out=xms[s][:, sl], in0=po[s][hb][:, :], in1=k.g1b[:, sl], op=ALU.mult),
                      reads=[b_po[s][hb], k.b_gb], writes=[b_xm[s]], partial=(hb > 0))
            fw.op(fw.dve, lambda: nc.vector.tensor_tensor(out=xms[s][:], in0=xms[s][:], in1=xts[s][:], op=ALU.add), reads=[b_xt[s], b_xm[s]], writes=[b_xm[s]])
            fw.dma(fw.sp, k.xmid[i * 128:(i + 1) * 128, :], xms[s][:], reads=[b_xm[s]], writes=[k.b_xmid], primary=b_xm[s], partial=True)
            norm_stats_xn(k, None, xms[s][:], b_xm[s], xns[s][:], b_xn[s], (junks[s], sss[s], b_t[s]))

        def partB(i):
            s = i % NS
            for kc in range(8):
                fw.op(fw.pe, lambda: nc.tensor.transpose(out=tps[s][:, kc * 128:(kc + 1) * 128], in_=xns[s][:, kc * 128:(kc + 1) * 128], identity=k.ident_bf[:]),
                      reads=[b_xn[s], k.cbuf], writes=[b_tp[s]], signal=(kc == 7), partial=(kc > 0))
            for kc in range(8):
                o = h2s[s][:, kc, :]
                src = tps[s][:, kc * 128:(kc + 1) * 128]
                sc_ap = k.gsT[:, layer, 1, kc, 0:1]
                bi_ap = k.modT[:, layer, 24 + kc, 0:1]
                if i % 2 == 0:
                    fw.op(fw.act, lambda: nc.scalar.activation(out=o, in_=src, func=AF.Identity, bias=bi_ap, scale=sc_ap), reads=[b_tp[s], k.cbuf], writes=[b_h2[s]], partial=(kc > 0))
                else:
                    fw.op(fw.dve, lambda: nc.vector.tensor_scalar(out=o, in0=src, scalar1=sc_ap, scalar2=bi_ap, op0=ALU.mult, op1=ALU.add), reads=[b_tp[s], k.cbuf], writes=[b_h2[s]], partial=(kc > 0))
            fw.dma(fw.sp, k.h2T[:, i * 128:(i + 1) * 128].rearrange("(c p) t -> p c t", p=128), h2s[s][:], reads=[b_h2[s]], writes=[k.b_h2T], primary=b_h2[s], partial=True)
            for kc in range(8):
                fw.op(fw.pe, lambda: nc.tensor.matmul(plg[s][:, 0:16], lhsT=h2s[s][:, kc, :], rhs=k.rw[:, kc, :], start=(kc == 0), stop=(kc == 7)),
                      reads=[b_h2[s], k.cbuf], writes=[b_lg[s]], signal=(kc == 7), partial=(kc > 0))
            G = gt[s]; bG = b_gt[s]
            sg = G[:, 0, :]; sel = G[:, 1, :]; eq = G[:, 2, :]; sel2 = G[:, 3, :]; msk = G[:, 4, :]
            m1 = G[:, 5, 0:4]; m2 = G[:, 5, 4:8]; gs = G[:, 5, 8:12]; gm = G[:, 5, 12:13]; dn = G[:, 5, 13:14]; gmask = G[:, 6, 0:4]
            v4 = lambda a: a.rearrange("p (g e) -> p g e", e=4)
            b4 = lambda a: a.unsqueeze(2).to_broadcast([128, 4, 4])
            ops = [
                (fw.act, lambda: nc.scalar.activation(out=sg, in_=plg[s][:, 0:16], func=AF.Tanh, scale=0.5), [b_lg[s]]),
                (fw.dve, lambda: nc.vector.tensor_scalar(out=sg, in0=sg, scalar1=0.5, scalar2=0.5, op0=ALU.mult, op1=ALU.add), []),
                (fw.dve, lambda: nc.vector.tensor_tensor(out=sel, in0=sg, in1=k.rb[:], op=ALU.add), [k.cbuf]),
                (fw.dve, lambda: nc.vector.tensor_reduce(out=m1, in_=v4(sel), axis=AX.X, op=ALU.max), []),
                (fw.dve, lambda: nc.vector.tensor_tensor(out=v4(eq), in0=v4(sel), in1=b4(m1), op=ALU.is_equal), []),
                (fw.dve, lambda: nc.vector.scalar_tensor_tensor(out=sel2, in0=eq, scalar=-1e9, in1=sel, op0=ALU.mult, op1=ALU.add), []),
                (fw.dve, lambda: nc.vector.tensor_reduce(out=m2, in_=v4(sel2), axis=AX.X, op=ALU.max), []),
                (fw.dve, lambda: nc.vector.tensor_tensor(out=gs, in0=m1, in1=m2, op=ALU.add), []),
                (fw.dve, lambda: nc.vector.tensor_reduce(out=gm, in_=gs, axis=AX.X, op=ALU.max), []),
                (fw.dve, lambda: nc.vector.tensor_scalar(out=gmask, in0=gs, scalar1=gm, scalar2=None, op0=ALU.is_ge), []),
                (fw.dve, lambda: nc.vector.tensor_tensor(out=v4(msk), in0=v4(sel), in1=b4(m2), op=ALU.is_ge), []),
                (fw.dve, lambda: nc.vector.tensor_tensor(out=v4(msk), in0=v4(msk), in1=b4(gmask), op=ALU.mult), []),
                (fw.dve, lambda: nc.vector.tensor_tensor(out=msk, in0=msk, in1=sg, op=ALU.mult), []),
                (fw.dve, lambda: nc.vector.tensor_reduce(out=dn, in_=msk, axis=AX.X, op=ALU.add), []),
                (fw.dve, lambda: nc.vector.reciprocal(out=dn, in_=dn), []),
            ]
            for (E, fn, rd) in ops:
                fw.op(E, fn, reads=rd + [bG], writes=[bG])
            fw.op(fw.dve, lambda: nc.vector.tensor_scalar(out=k.gates[:, i, :], in0=msk, scalar1=dn, scalar2=None, op0=ALU.mult), reads=[bG], writes=[k.b_gates], partial=True)

        for i in range(NTL):
            partA(i)
            if i > 0:
                partB(i - 1)
        partB(NTL - 1)
        fw.barrier()


def stage_moe(k, layer, final):
    nc, fw = k.nc, k.fw
    NE = k.__dict__.get("dbg_nexp", 16)
    with ExitStack() as es:
        make_bcast(k, es, layer, 40, 0, k.g2b, k.b_gb)
        sb = lambda name, shape, dt=F32: es.enter_context(nc.sbuf_tensor(name, shape, dt))
        GT = 16
        GN = GT * 128
        h2g = sb("e_h2g", [128, 8, GN], BF16); b_h2g = Buf()
        yacc = sb("e_yacc", [128, GT, D]); b_y = [Buf() for _ in range(GT)]
        w13 = [sb("e_w13_%d" % i, [128, 8, 2, 512], BF16) for i in range(2)]; b_w13 = [Buf() for _ in range(2)]
        w2 = [sb("e_w2_%d" % i, [128, 4, D], BF16) for i in range(2)]; b_w2 = [Buf() for _ in range(2)]
        s1 = [sb("e_s1_%d" % i, [128, 512], BF16) for i in range(2)]; b_s1 = [Buf() for _ in range(2)]
        actT = [sb("e_act%d" % i, [128, 4, 512], BF16) for i in range(2)]; b_act = [Buf() for _ in range(2)]
        NXT = 4
        xt = [sb("e_xt%d" % i, [128, D]) for i in range(NXT)]; b_xt = [Buf() for _ in range(NXT)]
        if final:
            fg = sb("e_fg", [128, D]); b_fg = Buf()
            fw.dma(fw.sp, fg[:], k.final_g.to_broadcast([128, D]), writes=[b_fg])
            junk = sb("e_junk", [128, D], BF16); ss = sb("e_ss", [128, 4]); b_ss = Buf()
        ph = [[es.enter_context(nc.psum_tensor("e_ph%d_%d" % (i, j), [128, 512], F32)) for j in range(2)] for i in range(2)]
        b_ph = [[Buf(excl=True) for j in range(2)] for i in range(2)]
        py = [es.enter_context(nc.psum_tensor("e_py%d" % i, [128, 512], F32)) for i in range(4)]
        b_py = [Buf(excl=True) for _ in range(4)]
        wi = 0; hi = 0; yi = 0; ai = 0
        pend = None
        yi_box = [0]

        def down_proj(e, ws, blk, a, g):
            for tt in range(4):
                tile = blk * 4 + tt
                for hb in range(2):
                    y = yi_box[0] % 4; yi_box[0] += 1
                    for fc in range(4):
                        fw.op(fw.pe, lambda: nc.tensor.matmul(py[y][:, :], lhsT=actT[a][:, fc, tt * 128:(tt + 1) * 128], rhs=w2[ws][:, fc, hb * 512:(hb + 1) * 512],
                                                              start=(fc == 0), stop=(fc == 3)),
                              reads=[b_act[a], b_w2[ws]], writes=[b_py[y]], signal=(fc == 3), partial=(fc > 0))
                    gsc = k.gates[:, g * GT + tile, e:e + 1]
                    ysl = yacc[:, tile, hb * 512:(hb + 1) * 512]
                    if e == 0:
                        fw.op(fw.dve, lambda: nc.vector.tensor_scalar(out=ysl, in0=py[y][:, :], scalar1=gsc, scalar2=None, op0=ALU.mult),
                              reads=[b_py[y], k.b_gates], writes=[b_y[tile]], partial=(hb > 0))
                    else:
                        fw.op(fw.dve, lambda: nc.vector.scalar_tensor_tensor(out=ysl, in0=py[y][:, :], scalar=gsc, in1=ysl, op0=ALU.mult, op1=ALU.add),
                              reads=[b_py[y], k.b_gates, b_y[tile]], writes=[b_y[tile]], partial=True)

        for g in range(NLAT // GN):
            g0 = g * GN
            fw.dma(fw.sp, h2g[:], k.h2T[:, g0:g0 + GN].rearrange("(c p) t -> p c t", p=128), reads=[k.b_h2T], writes=[b_h2g])
            for e in range(NE):
                ws = wi % 2; wi += 1
                for (wsrc, which) in ((k.moe_w1, 0), (k.moe_w3, 1)):
                    for half in range(2):
                        fw.dma(fw.pool, w13[ws][:, half * 4:(half + 1) * 4, which, :],
                               wsrc[layer, e, half * 512:(half + 1) * 512, :].rearrange("(c p) n -> p c n", p=128),
                               writes=[b_w13[ws]], partial=not (which == 0 and half == 0))
                fw.dma(fw.pool, w2[ws][:], k.moe_w2[layer, e].rearrange("(c p) n -> p c n", p=128), writes=[b_w2[ws]])
                for blk in range(GN // 512):
                    t0 = blk * 512
                    a = ai % 2; ai += 1
                    for fc in range(4):
                        h = hi % 2; hi += 1
                        for which in range(2):
                            for c in range(8):
                                fw.op(fw.pe, lambda: nc.tensor.matmul(ph[h][which][:, :], lhsT=w13[ws][:, c, which, fc * 128:(fc + 1) * 128], rhs=h2g[:, c, t0:t0 + 512],
                                                                      start=(c == 0), stop=(c == 7)),
                                      reads=[b_w13[ws], b_h2g], writes=[b_ph[h][which]], signal=(c == 7), partial=(c > 0))
                        fw.op(fw.act, lambda: nc.scalar.activation(out=s1[h][:], in_=ph[h][0][:], func=AF.Silu), reads=[b_ph[h][0]], writes=[b_s1[h]])
                        fw.op(fw.dve, lambda: nc.vector.tensor_tensor(out=actT[a][:, fc, :], in0=ph[h][1][:], in1=s1[h][:], op=ALU.mult),
                              reads=[b_ph[h][1], b_s1[h]], writes=[b_act[a]], partial=(fc > 0))
                    if pend is not None:
                        pend()
                    pend = (lambda e=e, ws=ws, blk=blk, a=a, g=g: down_proj(e, ws, blk, a, g))
            if pend is not None:
                pend()
                pend = None
            def xld(tile_):
                gi_ = g * GT + tile_
                fw.dma(fw.sp, xt[tile_ % NXT][:], k.xmid[gi_ * 128:(gi_ + 1) * 128, :], reads=[k.b_xmid], writes=[b_xt[tile_ % NXT]], primary=b_xt[tile_ % NXT])
            for tile in range(NXT - 1):
                xld(tile)
            for tile in range(GT):
                s = tile % NXT
                gi = g * GT + tile
                if tile + NXT - 1 < GT:
                    xld(tile + NXT - 1)
                fw.op(fw.dve, lambda: nc.vector.tensor_tensor(out=yacc[:, tile, :], in0=yacc[:, tile, :], in1=k.g2b[:], op=ALU.mult), reads=[b_y[tile], k.b_gb], writes=[b_y[tile]])
                fw.op(fw.dve, lambda: nc.vector.tensor_tensor(out=xt[s][:], in0=xt[s][:], in1=yacc[:, tile, :], op=ALU.add), reads=[b_y[tile], b_xt[s]], writes=[b_xt[s]])
                if not final:
                    fw.dma(fw.sp, k.xend[gi * 128:(gi + 1) * 128, :], xt[s][:], reads=[b_xt[s]], writes=[k.b_xend], primary=b_xt[s], partial=True)
                else:
                    fw.op(fw.act, lambda: nc.scalar.activation(out=junk[:], in_=xt[s][:], func=AF.Square, accum_out=ss[:, 0:1]), reads=[b_xt[s]], writes=[b_ss])
                    fw.op(fw.dve, lambda: nc.vector.tensor_scalar(out=ss[:, 1:2], in0=ss[:, 0:1], scalar1=1.0 / D, scalar2=EPS, op0=ALU.mult, op1=ALU.add), reads=[b_ss], writes=[b_ss])
                    fw.op(fw.pool, lambda: nc.gpsimd.tensor_tensor(out=ss[:, 2:3], in0=ss[:, 1:2], in1=k.nexp[:, 0:1], op=ALU.pow), reads=[b_ss, k.cbuf], writes=[b_ss])
                    fw.op(fw.dve, lambda: nc.vector.scalar_tensor_tensor(out=xt[s][:], in0=xt[s][:], scalar=ss[:, 2:3], in1=fg[:], op0=ALU.mult, op1=ALU.mult),
                          reads=[b_xt[s], b_ss, b_fg], writes=[b_xt[s]])
                    fw.dma(fw.sp, k.out[gi * 128:(gi + 1) * 128, :], xt[s][:], reads=[b_xt[s]], primary=b_xt[s])
        fw.barrier()


import ml_dtypes

NFFT = 8192
TWO_PI = 2.0 * math.pi
MAGIC = 12582912.0


def host_prep5(inputs, b, m):
    f = lambda a: np.ascontiguousarray(a, dtype=np.float32)
    bf = lambda a: np.ascontiguousarray(np.asarray(a, dtype=np.float32).astype(ml_dtypes.bfloat16))
    m["od_w_in"] = f(inputs["od_w_in"][0])
    m["hy_cw"] = f(inputs["hy_conv_w"][0].reshape(3, 24, 128).transpose(2, 1, 0))
    m["hy_cb"] = f(inputs["hy_conv_b"][0].reshape(24, 128).T)
    n = NLAT
    t = np.linspace(0.0, 1.0, n, dtype=np.float32)[:, None]
    w = (np.float32(2.0 * math.pi / n) * np.arange(n, dtype=np.float32))[:, None]
    fr = np.linspace(1e-4, 15, 16, dtype=np.float32)[None, :]
    z = np.concatenate([t, np.cos(fr * w), -np.sin(fr * w)], axis=-1).astype(np.float32)
    m["hy_zT"] = f(z.T)
    m["hy_tcol"] = f(np.concatenate([t[:, 0].reshape(32, 128).T, t[:, 0].reshape(32, 128)[:, ::-1].T], axis=1))
    m["hy_w1"] = f(inputs["hy_w1"][0])
    m["hy_w2"] = f(inputs["hy_w2"][0])
    m["hy_w3"] = f(inputs["hy_w3"][0])
    m["hy_vec"] = f(np.stack([inputs["hy_b1"][0], inputs["hy_freq1"][0], inputs["hy_b2"][0], inputs["hy_freq2"][0]], axis=1))
    m["hy_decay"] = f(inputs["hy_decay"][0].reshape(1, 4096))
    m["hy_skip"] = f(inputs["hy_skip"][0])
    a = np.arange(64)[:, None].astype(np.float64); ka = np.arange(64)[None, :].astype(np.float64)
    th = 2 * np.pi * a * (ka + 0.5) / 64.0
    F1 = np.concatenate([np.cos(th), -np.sin(th)], axis=1)
    m["F1"] = bf(F1)
    F1i = np.concatenate([np.cos(th).T, -np.sin(th).T], axis=0) * (2.0 / NFFT)
    m["F1i"] = bf(F1i[:, :32])
    bb = np.arange(128)[None, :, None].astype(np.float64); kb = np.arange(64)[None, None, :].astype(np.float64)
    kav = np.arange(64)[:, None, None].astype(np.float64)
    ph = 2 * np.pi * bb * (kav + 64.0 * kb + 0.5) / NFFT
    Gr = np.cos(ph); Gi = -np.sin(ph)
    pairs = [(Gr, Gi), (-Gi, Gr), (Gi, Gr), (Gr, -Gi), (Gr, Gr), (-Gi, -Gi), (-Gi, Gi), (-Gr, Gr)]
    GT = np.concatenate([np.concatenate(p, axis=2) for p in pairs], axis=2)
    m["GT"] = bf(GT)
    GrT = Gr.transpose(0, 2, 1); GiT = Gi.transpose(0, 2, 1)
    Vre = np.concatenate([GrT, GiT], axis=1)
    Vim = np.concatenate([-GiT, GrT], axis=1)
    m["GiT"] = bf(np.concatenate([Vre, Vim], axis=2))
    return m


def declare5(k):
    nc = k.nc
    din = lambda name, shape, dt=F32: nc.dram_tensor(name, list(shape), dt, kind="ExternalInput").ap()
    dscr = lambda name, shape, dt: nc.dram_tensor(name, list(shape), dt, kind="Internal").ap()
    k.od_w_in = din("od_w_in", [D, 3072]); k.hy_cw = din("hy_cw", [128, 24, 3]); k.hy_cb = din("hy_cb", [128, 24])
    k.hy_zT = din("hy_zT", [33, NLAT]); k.hy_tcol = din("hy_tcol", [128, 64])
    k.hy_w1 = din("hy_w1", [33, 64]); k.hy_w2 = din("hy_w2", [64, 64]); k.hy_w3 = din("hy_w3", [64, 4096])
    k.hy_vec = din("hy_vec", [64, 4]); k.hy_decay = din("hy_decay", [1, 4096]); k.hy_skip = din("hy_skip", [2, 1024])
    k.F1 = din("F1", [64, 128], BF16); k.F1i = din("F1i", [128, 32], BF16)
    k.GT = din("GT", [64, 128, 1024], BF16); k.GiT = din("GiT", [64, 128, 256], BF16)
    k.pv = [dscr("hy_p%d" % i, [NLAT, D], BF16) for i in range(3)]; k.b_pv = [Buf() for _ in range(3)]
    k.Tk = [dscr("hy_Tk%d" % o, [NFFT + 1, D], BF16) for o in range(2)]; k.b_Tk = [Buf() for _ in range(2)]
    k.Yd = dscr("hy_Yd", [128, 128, D], BF16); k.b_Yd = Buf()
    k.Vd = dscr("hy_Vd", [128, 128, D], BF16); k.b_Vd = Buf()
    k.Hs = [[dscr("hy_Hs%d_%d" % (o, i), [128, 64, D], BF16) for i in range(2)] for o in range(2)]; k.b_Hs = [Buf() for _ in range(2)]
    k.zd = [dscr("hy_z%d" % i, [NLAT, D], BF16) for i in range(2)]; k.b_zd = [Buf() for _ in range(2)]
    k.b_ns = Buf("nrmskip")


def alloc_hy_rows(k, es):
    nc = k.nc
    k.nrm = [es.enter_context(nc.sbuf_tensor("hy_nrm%d" % o, [128, D], F32)) for o in range(2)]
    k.skp = [es.enter_context(nc.sbuf_tensor("hy_skp%d" % o, [128, D], F32)) for o in range(2)]


def stage_hy_inproj(k):
    nc, fw = k.nc, k.fw
    with ExitStack() as es:
        sb = lambda name, shape, dt=F32: es.enter_context(nc.sbuf_tensor(name, shape, dt))
        hT = sb("hy_hT", [128, 8, NLAT], BF16); b_hT = [Buf() for _ in range(NTL)]
        with ExitStack() as es2:
            norm_transpose_pass(k, es2, 1, 0, NTL, lambda i: k.xend[i * 128:(i + 1) * 128, :], lambda i: 0, hT, b_hT, k.b_xend)
            fw.barrier()
        cw = sb("hy_cw_s", [128, 24, 3]); cb = sb("hy_cb_s", [128, 24]); b_c = Buf()
        fw.dma(fw.sp, cw[:], k.hy_cw, writes=[b_c], partial=True)
        fw.dma(fw.sp, cb[:], k.hy_cb, writes=[b_c], partial=True)
        wch = [sb("hy_wch%d" % i, [128, 8, 128], BF16) for i in range(2)]; b_wch = [Buf() for _ in range(2)]
        pb = [sb("hy_pb%d" % i, [128, NLAT + 2]) for i in range(2)]; b_pb = [Buf() for _ in range(2)]
        ub = [sb("hy_ub%d" % i, [128, NLAT], BF16) for i in range(2)]; b_ub = [Buf() for _ in range(2)]
        acc = [sb("hy_acc%d" % i, [128, NLAT]) for i in range(1)]; b_acc = [Buf()]
        ot = [sb("hy_ot%d" % i, [128, 8, 128], BF16) for i in range(2)]; b_ot = [Buf() for _ in range(2)]
        pss = [es.enter_context(nc.psum_tensor("hy_ps%d" % i, [128, 512], F32)) for i in range(3)]; b_ps = [Buf(excl=True) for _ in range(3)]
        tps = [es.enter_context(nc.psum_tensor("hy_tp%d" % i, [128, D], BF16)) for i in range(2)]; b_tp = [Buf(excl=True) for _ in range(2)]
        for s in range(2):
            fw.op(fw.pool, lambda: nc.gpsimd.memset(pb[s][:], 0.0), writes=[b_pb[s]])
        pi = 0; oi_box = [0]
        pend = None

        def store_chunk(ch, s):
            which = ch // 8; cc = ch % 8
            for grp in range(4):
                tp = (ch * 4 + grp) % 2
                o = oi_box[0] % 2; oi_box[0] += 1
                for tt in range(8):
                    ti = grp * 8 + tt
                    fw.op(fw.pe, lambda: nc.tensor.transpose(out=tps[tp][:, tt * 128:(tt + 1) * 128], in_=ub[s][:, ti * 128:(ti + 1) * 128], identity=k.ident_bf[:]),
                          reads=[b_ub[s], k.cbuf], writes=[b_tp[tp]], signal=(tt == 7), partial=(tt > 0))
                if grp % 2 == 0:
                    fw.op(fw.act, lambda: nc.scalar.copy(out=ot[o][:].rearrange("p a b -> p (a b)"), in_=tps[tp][:]), reads=[b_tp[tp]], writes=[b_ot[o]])
                else:
                    fw.op(fw.dve, lambda: nc.vector.tensor_copy(out=ot[o][:].rearrange("p a b -> p (a b)"), in_=tps[tp][:]), reads=[b_tp[tp]], writes=[b_ot[o]])
                dst = k.pv[which][grp * 1024:(grp + 1) * 1024, cc * 128:(cc + 1) * 128].rearrange("(t p) c -> p t c", p=128)
                fw.dma(fw.sp, dst, ot[o][:], reads=[b_ot[o]], writes=[k.b_pv[which]], primary=b_ot[o], partial=True)

        for ch in range(24):
            s = ch % 2
            fw.dma(fw.pool, wch[s][:], k.od_w_in[:, ch * 128:(ch + 1) * 128].rearrange("(c p) n -> p c n", p=128), writes=[b_wch[s]])
            for blk in range(8):
                p = pi % 3; pi += 1
                t0 = blk * 512
                for kc in range(8):
                    fw.op(fw.pe, lambda: nc.tensor.matmul(pss[p][:, :], lhsT=wch[s][:, kc, :], rhs=hT[:, kc, t0:t0 + 512], start=(kc == 0), stop=(kc == 7)),
                          reads=[b_wch[s]] + b_hT[blk * 4:blk * 4 + 4], writes=[b_ps[p]], signal=(kc == 7), partial=(kc > 0))
                fw.op(fw.act, lambda: nc.scalar.copy(out=pb[s][:, 1 + t0:1 + t0 + 512], in_=pss[p][:, :]), reads=[b_ps[p]], writes=[b_pb[s]], partial=True)
            fw.op(fw.act, lambda: nc.scalar.activation(out=acc[0][:], in_=pb[s][:, 0:NLAT], func=AF.Identity, bias=cb[:, ch:ch + 1], scale=cw[:, ch, 0:1]),
                  reads=[b_pb[s], b_c], writes=[b_acc[0]])
            fw.op(fw.dve, lambda: nc.vector.scalar_tensor_tensor(out=acc[0][:], in0=pb[s][:, 1:NLAT + 1], scalar=cw[:, ch, 1:2], in1=acc[0][:], op0=ALU.mult, op1=ALU.add),
                  reads=[b_pb[s], b_c, b_acc[0]], writes=[b_acc[0]])
            fw.op(fw.dve, lambda: nc.vector.scalar_tensor_tensor(out=ub[s][:], in0=pb[s][:, 2:NLAT + 2], scalar=cw[:, ch, 2:3], in1=acc[0][:], op0=ALU.mult, op1=ALU.add),
                  reads=[b_pb[s], b_c, b_acc[0]], writes=[b_ub[s]])
            if pend is not None:
                pend()
            pend = (lambda ch=ch, s=s: store_chunk(ch, s))
        pend()
        fw.barrier()


def sin_reduced(k, dst, src_ps, npart, n, f_ap, fb_ap, tmps, b_tmps, b_src, b_dst):
    nc, fw = k.nc, k.fw
    arg, kk = tmps
    b_arg, b_kk = b_tmps
    fw.op(fw.dve, lambda: nc.vector.tensor_scalar(out=arg[:npart, :n], in0=src_ps, scalar1=f_ap, scalar2=fb_ap, op0=ALU.mult, op1=ALU.add), reads=[b_src, k.b_hyp], writes=[b_arg])
    fw.op(fw.dve, lambda: nc.vector.tensor_scalar(out=kk[:npart, :n], in0=arg[:npart, :n], scalar1=1.0 / TWO_PI, scalar2=MAGIC, op0=ALU.mult, op1=ALU.add), reads=[b_arg], writes=[b_kk])
    fw.op(fw.dve, lambda: nc.vector.tensor_scalar(out=kk[:npart, :n], in0=kk[:npart, :n], scalar1=-MAGIC, scalar2=None, op0=ALU.add), reads=[b_kk], writes=[b_kk])
    fw.op(fw.dve, lambda: nc.vector.scalar_tensor_tensor(out=arg[:npart, :n], in0=kk[:npart, :n], scalar=-TWO_PI, in1=arg[:npart, :n], op0=ALU.mult, op1=ALU.add), reads=[b_kk, b_arg], writes=[b_arg])
    fw.op(fw.dve, lambda: nc.vector.tensor_scalar(out=arg[:npart, :n], in0=arg[:npart, :n], scalar1=math.pi, scalar2=-math.pi, op0=ALU.min, op1=ALU.max), reads=[b_arg], writes=[b_arg])
    fw.op(fw.act, lambda: nc.scalar.activation(out=dst, in_=arg[:npart, :n], func=AF.Sin), reads=[b_arg], writes=[b_dst], partial=True)


def stage_hy_filters(k):
    nc, fw = k.nc, k.fw
    with ExitStack() as es:
        sb = lambda name, shape, dt=F32: es.enter_context(nc.sbuf_tensor(name, shape, dt))
        zT = sb("f_zT", [33, NLAT]); w1 = sb("f_w1", [33, 64]); w2 = sb("f_w2", [64, 64]); w3 = sb("f_w3", [64, 4096])
        vec = sb("f_vec", [64, 6]); tcol = sb("f_tcol", [128, 64]); ntcol = sb("f_ntcol", [128, 64])
        dec = sb("f_dec", [128, 4096])
        k.b_hyp = Buf("hyp")
        for dst, src in ((zT[:], k.hy_zT), (w1[:], k.hy_w1), (w2[:], k.hy_w2), (w3[:], k.hy_w3), (vec[:, 0:4], k.hy_vec), (tcol[:], k.hy_tcol)):
            fw.dma(fw.sp, dst, src, writes=[k.b_hyp], partial=True)
        fw.dma(fw.sp, dec[:], k.hy_decay.to_broadcast([128, 4096]), writes=[k.b_hyp], partial=True)
        for o in range(2):
            fw.dma(fw.sp, k.skp[o][:], k.hy_skip[o:o + 1, :].to_broadcast([128, D]), writes=[k.b_ns], partial=True)
        fw.op(fw.dve, lambda: nc.vector.tensor_tensor(out=vec[:, 4:5], in0=vec[:, 0:1], in1=vec[:, 1:2], op=ALU.mult), reads=[k.b_hyp], writes=[k.b_hyp])
        fw.op(fw.dve, lambda: nc.vector.tensor_tensor(out=vec[:, 5:6], in0=vec[:, 2:3], in1=vec[:, 3:4], op=ALU.mult), reads=[k.b_hyp], writes=[k.b_hyp])
        fw.op(fw.dve, lambda: nc.vector.tensor_scalar(out=ntcol[:], in0=tcol[:], scalar1=-1.0, scalar2=None, op0=ALU.mult), reads=[k.b_hyp], writes=[k.b_hyp])
        fw.op(fw.act, lambda: nc.scalar.activation(out=dec[:], in_=dec[:], func=AF.Abs), reads=[k.b_hyp], writes=[k.b_hyp])
        a1 = sb("f_a1", [64, NLAT]); a2 = sb("f_a2", [64, NLAT]); b_a1 = Buf(); b_a2 = Buf()
        tm = [[sb("f_tm%d_%d" % (i, j), [128, 512]) for j in range(2)] for i in range(2)]; b_tm = [[Buf() for j in range(2)] for i in range(2)]
        pss = [es.enter_context(nc.psum_tensor("f_ps%d" % i, [128, 512], F32)) for i in range(3)]; b_ps = [Buf(excl=True) for _ in range(3)]
        pacc = es.enter_context(nc.psum_tensor("f_pacc", [128, 512], F32)); b_pacc = Buf(excl=True)
        pi = 0
        for blk in range(8):
            p = pi % 3; pi += 1; s = blk % 2
            t0 = blk * 512
            fw.op(fw.pe, lambda: nc.tensor.matmul(pss[p][:64, :], lhsT=w1[:, :], rhs=zT[:, t0:t0 + 512], start=True, stop=True), reads=[k.b_hyp], writes=[b_ps[p]])
            sin_reduced(k, a1[:, t0:t0 + 512], pss[p][:64, :], 64, 512, vec[:, 1:2], vec[:, 4:5], tm[s], b_tm[s], b_ps[p], b_a1)
        for blk in range(8):
            p = pi % 3; pi += 1; s = blk % 2
            t0 = blk * 512
            fw.op(fw.pe, lambda: nc.tensor.matmul(pss[p][:64, :], lhsT=w2[:, :], rhs=a1[:, t0:t0 + 512], start=True, stop=True), reads=[k.b_hyp, b_a1], writes=[b_ps[p]])
            sin_reduced(k, a2[:, t0:t0 + 512], pss[p][:64, :], 64, 512, vec[:, 3:4], vec[:, 5:6], tm[s], b_tm[s], b_ps[p], b_a2)
        a2r = sb("f_a2r", [64, NLAT], BF16); a2b = sb("f_a2b", [64, NLAT], BF16); w3b = sb("f_w3b", [64, 4096], BF16)
        fw.op(fw.dve, lambda: nc.vector.tensor_copy(out=a2r[:], in_=a2[:, ::-1]), reads=[b_a2], writes=[b_a2], partial=True)
        fw.op(fw.dve, lambda: nc.vector.tensor_copy(out=a2b[:], in_=a2[:]), reads=[b_a2], writes=[b_a2], partial=True)
        fw.op(fw.act, lambda: nc.scalar.copy(out=w3b[:], in_=w3[:]), reads=[k.b_hyp], writes=[k.b_hyp], partial=True)
        E = [sb("f_E%d" % i, [128, 512]) for i in range(2)]; b_E = [Buf() for _ in range(2)]
        hh = [sb("f_hh%d" % i, [128, 512]) for i in range(2)]; b_hh = [Buf() for _ in range(2)]
        hab = [sb("f_hab%d" % i, [128, 512], BF16) for i in range(2)]; b_hab = [Buf() for _ in range(2)]
        hb = [sb("f_hb%d" % i, [128, 512], BF16) for i in range(3)]; b_hb = [Buf() for _ in range(3)]
        row0 = sb("f_row0", [1, 512]); b_row0 = Buf()
        zrow = sb("f_zrow", [1, D], BF16); b_zrow = Buf()
        fw.op(fw.dve, lambda: nc.vector.memset(zrow[:], 0.0), writes=[b_zrow])
        it = 0; hbi = 0
        for o in range(2):
            fw.dma(fw.sp, k.Tk[o][4096:4097, :], zrow[:], reads=[b_zrow], writes=[k.b_Tk[o]], primary=b_zrow, partial=True)
            for half in range(2):
                for side in (1, 0):
                    col0 = (o * 2 + side) * 1024 + half * 512
                    for ti in range(32):
                        p = pi % 3; pi += 1
                        s = it % 2; it += 1
                        hs = hbi % 3; hbi += 1
                        lt = a2b[:, ti * 128:(ti + 1) * 128]
                        if side == 1:
                            lt = a2r[:, (31 - ti) * 128:(32 - ti) * 128]
                        tc_i = ti + (32 if side == 1 else 0)
                        fw.op(fw.pe, lambda: nc.tensor.matmul(pss[p][:, :], lhsT=lt, rhs=w3b[:, col0:col0 + 512], start=True, stop=True),
                              reads=[b_a2, k.b_hyp], writes=[b_ps[p]])
                        fw.op(fw.act, lambda: nc.scalar.activation(out=E[s][:], in_=dec[:, col0:col0 + 512], func=AF.Exp, scale=ntcol[:, tc_i:tc_i + 1]), reads=[k.b_hyp], writes=[b_E[s]])
                        fw.op(fw.dve, lambda: nc.vector.tensor_tensor(out=hh[s][:], in0=pss[p][:, :], in1=E[s][:], op=ALU.mult), reads=[b_ps[p], b_E[s]], writes=[b_hh[s]])
                        fw.op(fw.act, lambda: nc.scalar.activation(out=hab[s][:], in_=hh[s][:], func=AF.Abs), reads=[b_hh[s]], writes=[b_hab[s]])
                        first = (side == 1 and ti == 0); last = (side == 0 and ti == 31)
                        fw.op(fw.pe, lambda: nc.tensor.matmul(pacc[:, :], lhsT=k.ones_bf[:], rhs=hab[s][:], start=first, stop=last),
                              reads=[b_hab[s], k.cbuf], writes=[b_pacc], signal=last, partial=not first)
                        if side == 1:
                            if ti == 0:
                                fw.dma(fw.sp, row0[:], hh[s][127:128, :], reads=[b_hh[s]], writes=[b_row0], primary=b_row0)
                            fw.op(fw.dve, lambda: nc.vector.tensor_scalar(out=hb[hs][:], in0=hh[s][:], scalar1=-1.0, scalar2=None, op0=ALU.mult), reads=[b_hh[s]], writes=[b_hb[hs]])
                            r_lo = NFFT - 127 - ti * 128
                            dst = k.Tk[o][r_lo:r_lo + 128, half * 512:(half + 1) * 512]
                            fw.dma(fw.sp, dst, hb[hs][:], reads=[b_hb[hs]], writes=[k.b_Tk[o]], primary=b_hb[hs], partial=True)
                        else:
                            if ti == 0:
                                fw.op(fw.dve, lambda: nc.vector.tensor_tensor(out=hh[s][0:1, :], in0=hh[s][0:1, :], in1=row0[:], op=ALU.add), reads=[b_hh[s], b_row0], writes=[b_hh[s]])
                            fw.op(fw.dve, lambda: nc.vector.tensor_copy(out=hb[hs][:], in_=hh[s][:]), reads=[b_hh[s]], writes=[b_hb[hs]])
                            fw.dma(fw.sp, k.Tk[o][ti * 128:(ti + 1) * 128, half * 512:(half + 1) * 512], hb[hs][:], reads=[b_hb[hs]], writes=[k.b_Tk[o]], primary=b_hb[hs], partial=True)
                sl = slice(half * 512, (half + 1) * 512)
                fw.op(fw.dve, lambda: nc.vector.tensor_scalar(out=k.nrm[o][:, sl], in0=pacc[:, :], scalar1=1e-6, scalar2=None, op0=ALU.add), reads=[b_pacc], writes=[k.b_ns], partial=True)
                fw.op(fw.dve, lambda: nc.vector.reciprocal(out=k.nrm[o][:, sl], in_=k.nrm[o][:, sl]), reads=[k.b_ns], writes=[k.b_ns], partial=True)
        fw.barrier()


def fft_forward(k, src, b_src, a_in, pairs, post):
    nc, fw = k.nc, k.fw
    with ExitStack() as es:
        sb = lambda name, shape, dt=F32: es.enter_context(nc.sbuf_tensor(name, shape, dt))
        F1 = sb("ff_F1", [64, 128], BF16); b_F1 = Buf()
        fw.dma(fw.sp, F1[:], k.F1, writes=[b_F1])
        NX = 4
        xin = [sb("ff_x%d" % i, [64, 4096], BF16) for i in range(NX)]; b_x = [Buf() for _ in range(NX)]
        yo = [sb("ff_y%d" % i, [128, 512], BF16) for i in range(6)]; b_y = [Buf() for _ in range(6)]
        pss = [es.enter_context(nc.psum_tensor("ff_ps%d" % i, [128, 512], F32)) for i in range(3)]; b_ps = [Buf(excl=True) for _ in range(3)]
        srcv = src[0:a_in * 128, :].rearrange("(a b) c -> a (b c)", b=128)
        yv = k.Yd.rearrange("r b c -> r (b c)")
        pi = 0

        def xld(ch):
            fw.dma(fw.act, xin[ch % NX][:a_in, :], srcv[:, ch * 4096:(ch + 1) * 4096], reads=[b_src], writes=[b_x[ch % NX]], primary=b_x[ch % NX])
        for ch in range(NX - 1):
            xld(ch)
        for ch in range(32):
            s = ch % NX
            if ch + NX - 1 < 32:
                xld(ch + NX - 1)
            for j in range(8):
                p = pi % 3; y6 = pi % 6; pi += 1
                fw.op(fw.pe, lambda: nc.tensor.matmul(pss[p][:, :], lhsT=F1[:a_in, :], rhs=xin[s][:a_in, j * 512:(j + 1) * 512], start=True, stop=True),
                      reads=[b_F1, b_x[s]], writes=[b_ps[p]])
                if p % 2 == 0:
                    fw.op(fw.act, lambda: nc.scalar.copy(out=yo[y6][:], in_=pss[p][:]), reads=[b_ps[p]], writes=[b_y[y6]])
                else:
                    fw.op(fw.dve, lambda: nc.vector.tensor_copy(out=yo[y6][:], in_=pss[p][:]), reads=[b_ps[p]], writes=[b_y[y6]])
                c0 = ch * 4096 + j * 512
                fw.dma(fw.sp, yv[:, c0:c0 + 512], yo[y6][:], reads=[b_y[y6]], writes=[k.b_Yd], primary=b_y[y6], partial=True)
        fw.barrier()
    with ExitStack() as es:
        sb = lambda name, shape, dt=F32: es.enter_context(nc.sbuf_tensor(name, shape, dt))
        NR = 4
        G = [sb("ff_G%d" % i, [128, 512], BF16) for i in range(NR)]; b_G = [Buf() for _ in range(NR)]
        gp0 = min(min(p) for p in pairs)
        assert gp0 % 4 == 0 and max(max(p) for p in pairs) < gp0 + 4
        Y = [sb("ff_Y%d" % i, [128, 2, D], BF16) for i in range(NR)]; b_Y = [Buf() for _ in range(NR)]
        npair = len(pairs)
        ps = [[es.enter_context(nc.psum_tensor("ff_q%d_%d" % (i, j), [128, 512], F32)) for j in range(npair)] for i in range(2)]
        b_q = [[Buf(excl=True) for j in range(npair)] for i in range(2)]
        Ydv = k.Yd.rearrange("(ri ka) b c -> ka b ri c", ri=2)
        qi = 0
        def ld(ka):
            s = ka % NR
            fw.dma(fw.act, G[s][:], k.GT[ka][:, gp0 * 128:(gp0 + 4) * 128], writes=[b_G[s]])
            fw.dma(fw.sp, Y[s][:], Ydv[ka], reads=[k.b_Yd], writes=[b_Y[s]], primary=b_Y[s])
        for ka in range(NR - 1):
            ld(ka)
        for ka in range(64):
            s = ka % NR
            if ka + NR - 1 < 64:
                ld(ka + NR - 1)
            for hb in range(2):
                q = qi % 2; qi += 1
                for j, (pr_re, pr_im) in enumerate(pairs):
                    fw.op(fw.pe, lambda: nc.tensor.matmul(ps[q][j][:, :], lhsT=G[s][:, (pr_re - gp0) * 128:(pr_re - gp0 + 1) * 128], rhs=Y[s][:, 0, hb * 512:(hb + 1) * 512], start=True, stop=False),
                          reads=[b_G[s], b_Y[s]], writes=[b_q[q][j]], signal=False)
                    fw.op(fw.pe, lambda: nc.tensor.matmul(ps[q][j][:, :], lhsT=G[s][:, (pr_im - gp0) * 128:(pr_im - gp0 + 1) * 128], rhs=Y[s][:, 1, hb * 512:(hb + 1) * 512], start=False, stop=True),
                          reads=[b_G[s], b_Y[s]], writes=[b_q[q][j]], partial=True)
                post(ka, hb, ps[q], b_q[q])
        fw.barrier()


def stage_hy_filter_spectra(k, o):
    nc, fw = k.nc, k.fw
    with ExitStack() as es0:
        ho = [[es0.enter_context(nc.sbuf_tensor("fs_h%d_%d" % (i, j), [128, 512], BF16)) for j in range(2)] for i in range(2)]
        b_ho = [[Buf() for j in range(2)] for i in range(2)]
        tmpf = [es0.enter_context(nc.sbuf_tensor("fs_t%d" % i, [128, 512], F32)) for i in range(2)]; b_tf = [Buf() for _ in range(2)]
        cnt = [0]

        def post(ka, hb, ps, b_ps):
            s = cnt[0] % 2; cnt[0] += 1
            sl = slice(hb * 512, (hb + 1) * 512)
            fw.op(fw.dve, lambda: nc.vector.tensor_tensor(out=tmpf[s][:], in0=ps[0][:, :], in1=k.nrm[o][:, sl], op=ALU.mult), reads=[b_ps[0], k.b_ns], writes=[b_tf[s]])
            fw.op(fw.dve, lambda: nc.vector.tensor_tensor(out=ho[s][0][:], in0=tmpf[s][:], in1=k.skp[o][:, sl], op=ALU.add), reads=[b_tf[s], k.b_ns], writes=[b_ho[s][0]])
            fw.op(fw.dve, lambda: nc.vector.tensor_tensor(out=ho[s][1][:], in0=ps[1][:, :], in1=k.nrm[o][:, sl], op=ALU.mult), reads=[b_ps[1], k.b_ns], writes=[b_ho[s][1]])
            for j in range(2):
                fw.dma(fw.sp, k.Hs[o][j][:, ka, sl], ho[s][j][:], reads=[b_ho[s][j]], writes=[k.b_Hs[o]], primary=b_ho[s][j], partial=True)

        fft_forward(k, k.Tk[o], k.b_Tk[o], 64, [(4, 5), (6, 7)], post)


def stage_hy_conv(k, o, src, b_src, gate, b_gate, dst, b_dst):
    nc, fw = k.nc, k.fw
    with ExitStack() as es0:
        sb0 = lambda name, shape, dt=F32: es0.enter_context(nc.sbuf_tensor(name, shape, dt))
        Hb = [[sb0("cv_H%d_%d" % (i, j), [128, 512], BF16) for j in range(2)] for i in range(4)]; b_H = [Buf() for _ in range(4)]
        t1 = [sb0("cv_t1_%d" % i, [128, 512]) for i in range(2)]; b_t1 = [Buf() for _ in range(2)]
        t2 = [sb0("cv_t2_%d" % i, [128, 512]) for i in range(2)]; b_t2 = [Buf() for _ in range(2)]
        Pb = [sb0("cv_P%d" % i, [128, D], BF16) for i in range(2)]; b_P = [Buf() for _ in range(2)]
        Gi = [sb0("cv_Gi%d" % i, [128, 256], BF16) for i in range(2)]; b_Gi = [Buf() for _ in range(2)]
        vo = [sb0("cv_vo%d" % i, [128, 512], BF16) for i in range(4)]; b_vo = [Buf() for _ in range(4)]
        pv = [es0.enter_context(nc.psum_tensor("cv_pv%d" % i, [128, 512], F32)) for i in range(4)]; b_pv = [Buf(excl=True) for _ in range(4)]
        cnt = [0]; vi = [0]

        def post(ka, hb, ps, b_ps):
            n = cnt[0]
            hsl = n % 4
            s = n % 2; cnt[0] += 1
            sl = slice(hb * 512, (hb + 1) * 512)
            kas = ka % 2

            def hload(nn):
                ka2, hb2 = nn // 2, nn % 2
                for j in range(2):
                    fw.dma(fw.act, Hb[nn % 4][j][:], k.Hs[o][j][:, ka2, hb2 * 512:(hb2 + 1) * 512], reads=[k.b_Hs[o]], writes=[b_H[nn % 4]], primary=b_H[nn % 4], partial=(j > 0))
            if n == 0:
                for nn in range(3):
                    hload(nn)
            if n + 3 < 128:
                hload(n + 3)
            fw.op(fw.dve, lambda: nc.vector.tensor_tensor(out=t1[s][:], in0=ps[0][:, :], in1=Hb[hsl][0][:], op=ALU.mult), reads=[b_ps[0], b_H[hsl]], writes=[b_t1[s]])
            fw.op(fw.dve, lambda: nc.vector.tensor_tensor(out=t2[s][:], in0=ps[1][:, :], in1=Hb[hsl][1][:], op=ALU.mult), reads=[b_ps[1], b_H[hsl]], writes=[b_t2[s]])
            fw.op(fw.dve, lambda: nc.vector.tensor_tensor(out=Pb[kas][:, sl], in0=t1[s][:], in1=t2[s][:], op=ALU.add), reads=[b_t1[s], b_t2[s]], writes=[b_P[kas]], partial=(hb > 0))
            if hb == 0:
                fw.dma(fw.sp, Gi[kas][:], k.GiT[ka], writes=[b_Gi[kas]])
            for ri in range(2):
                v = vi[0] % 4; vi[0] += 1
                fw.op(fw.pe, lambda: nc.tensor.matmul(pv[v][:, :], lhsT=Gi[kas][:, ri * 128:(ri + 1) * 128], rhs=Pb[kas][:, sl], start=True, stop=True),
                      reads=[b_Gi[kas], b_P[kas]], writes=[b_pv[v]])
                if v % 2 == 0:
                    fw.op(fw.act, lambda: nc.scalar.copy(out=vo[v][:], in_=pv[v][:]), reads=[b_pv[v]], writes=[b_vo[v]])
                else:
                    fw.op(fw.act, lambda: nc.scalar.copy(out=vo[v][:], in_=pv[v][:]), reads=[b_pv[v]], writes=[b_vo[v]])
                fw.dma(fw.sp, k.Vd[ri * 64 + ka, :, sl], vo[v][:], reads=[b_vo[v]], writes=[k.b_Vd], primary=b_vo[v], partial=True)

        fft_forward(k, src, b_src, 32, [(0, 1), (2, 3)], post)
    with ExitStack() as es:
        sb = lambda name, shape, dt=F32: es.enter_context(nc.sbuf_tensor(name, shape, dt))
        F1i = sb("ci_F1i", [128, 32], BF16); b_F = Buf()
        fw.dma(fw.sp, F1i[:], k.F1i, writes=[b_F])
        NV = 4
        vin = [sb("ci_v%d" % i, [128, 4096], BF16) for i in range(NV)]; b_v = [Buf() for _ in range(NV)]
        gin = [sb("ci_g%d" % i, [32, 4096], BF16) for i in range(NV)]; b_g = [Buf() for _ in range(NV)]
        zo = [sb("ci_z%d" % i, [32, 4096], BF16) for i in range(2)]; b_z = [Buf() for _ in range(2)]
        pss = [es.enter_context(nc.psum_tensor("ci_ps%d" % i, [128, 512], F32)) for i in range(3)]; b_ps = [Buf(excl=True) for _ in range(3)]
        vv = k.Vd.rearrange("r b c -> r (b c)")
        gv = gate.rearrange("(a b) c -> a (b c)", b=128)
        dv = dst.rearrange("(a b) c -> a (b c)", b=128)
        pi = 0
        def vld(ch):
            s_ = ch % NV
            fw.dma(fw.sp, vin[s_][:], vv[:, ch * 4096:(ch + 1) * 4096], reads=[k.b_Vd], writes=[b_v[s_]], primary=b_v[s_])
            fw.dma(fw.act, gin[s_][:], gv[:, ch * 4096:(ch + 1) * 4096], reads=[b_gate], writes=[b_g[s_]], primary=b_g[s_])
        for ch in range(NV - 1):
            vld(ch)
        for ch in range(32):
            s = ch % NV; z = ch % 2
            if ch + NV - 1 < 32:
                vld(ch + NV - 1)
            for j in range(8):
                p = pi % 3; pi += 1
                fw.op(fw.pe, lambda: nc.tensor.matmul(pss[p][:32, :], lhsT=F1i[:, :], rhs=vin[s][:, j * 512:(j + 1) * 512], start=True, stop=True),
                      reads=[b_F, b_v[s]], writes=[b_ps[p]])
                fw.op(fw.dve, lambda: nc.vector.tensor_tensor(out=zo[z][:, j * 512:(j + 1) * 512], in0=pss[p][:32, :], in1=gin[s][:, j * 512:(j + 1) * 512], op=ALU.mult),
                      reads=[b_ps[p], b_g[s]], writes=[b_z[z]], partial=(j > 0))
            fw.dma(fw.sp, dv[:, ch * 4096:(ch + 1) * 4096], zo[z][:], reads=[b_z[z]], writes=[b_dst], primary=b_z[z], partial=True)
        fw.barrier()


def prep_shared(inputs):
    m = {}
    host_prep_shared1(inputs, m)
    host_prep2(inputs, 0, m); host_prep3(inputs, 0, m); host_prep4(inputs, 0, m); host_prep5(inputs, 0, m)
    return m


def host_prep_shared1(inputs, m):
    f = lambda a: np.ascontiguousarray(a, dtype=np.float32)
    m["ada_w"] = f(inputs["ada_w"])
    m["ada_bT"] = f(inputs["ada_b"].reshape(2, 48, 128).transpose(0, 2, 1))
    m["n1gT"] = f(inputs["norm1_g"].reshape(2, 8, 128).transpose(0, 2, 1))
    m["n2gT"] = f(inputs["norm2_g"].reshape(2, 8, 128).transpose(0, 2, 1))
    m["final_g"] = f(inputs["final_g"].reshape(1, 1024))


def prep_core(inputs, b, shared):
    f = lambda a: np.ascontiguousarray(a, dtype=np.float32)
    m = dict(shared)
    m["x"] = f(inputs["x"][b])
    m["ctx"] = f(inputs["ctx"][b])
    cc = np.stack([inputs["c"][b], inputs["c_ctx"]], axis=-1)
    m["ccT"] = f(cc.reshape(8, 128, 2).transpose(1, 0, 2))
    return m


def build_full(nc0, dbg=None):
    k = build(nc0, dbg=dbg)
    declare2(k); declare3(k); declare4(k); declare5(k)
    fw = k.fw; nc = k.nc
    stage_mod(k)
    with ExitStack() as es0:
        alloc_mla_lat(k, es0)
        with ExitStack() as es:
            stage_l0_norm1(k, es)
            w = es.enter_context(nc.sbuf_tensor("s_w_in", [128, 8, 1728], BF16))
            b_w = Buf("w_in")
            for kc in range(8):
                load_w_bf16(k, w[:, kc, :], k.w_in[kc * 128:(kc + 1) * 128, :], b_w)
            stage_l0_lru(k, es, w, b_w)
            stage_l0_mla_lat(k, w, b_w)
        stage_l0_attn(k, es0)
    stage_outproj_norm2(k, 0, k.mixT, k.b_mixT, NCTX, lambda i: k.x[i * 128:(i + 1) * 128, :])
    stage_moe(k, 0, False)
    stage_hy_inproj(k)
    with ExitStack() as esh:
        alloc_hy_rows(k, esh)
        stage_hy_filters(k)
        stage_hy_filter_spectra(k, 0)
        stage_hy_conv(k, 0, k.pv[0], k.b_pv[0], k.pv[1], k.b_pv[1], k.zd[0], k.b_zd[0])
        stage_hy_filter_spectra(k, 1)
        stage_hy_conv(k, 1, k.zd[0], k.b_zd[0], k.pv[2], k.b_pv[2], k.zd[1], k.b_zd[1])
    stage_outproj_norm2(k, 1, k.zd[1], k.b_zd[1], 0, lambda i: k.xend[i * 128:(i + 1) * 128, :], mix_tm=True)
    stage_moe(k, 1, True)
    finish(k)
    return k


def kernel(**inputs):
    inputs = {k_: np.asarray(v) for k_, v in inputs.items()}
    nc0 = bass.Bass("TRN2", target_bir_lowering=False)
    build_full(nc0)
    shared = prep_shared(inputs)
    n = 8
    maps = [prep_core(inputs, b, shared) for b in range(n)]
    res = run_bass_kernel_spmd(nc0, maps, core_ids=list(range(n)))
    out = np.stack([np.asarray(res.results[b]["out"], dtype=np.float32) for b in range(n)], axis=0)
    return out
```

```python
import numpy as np
import concourse.bass as bass
import concourse.mybir as mybir

F32 = mybir.dt.float32
BF16 = mybir.dt.bfloat16
I32 = mybir.dt.int32
ALU = mybir.AluOpType
AF = mybir.ActivationFunctionType
AX = mybir.AxisListType

SEM_ROLL = 30000


class Sem:
    def __init__(self, fw, name, is_dma):
        self.h = fw.nc.alloc_semaphore(name)
        self.total = 0
        self.is_dma = is_dma
        fw.all_sems.append(self)


class Buf:
    __slots__ = ("name", "writers", "pwriters", "readers", "dsem", "excl")

    def __init__(self, name="", excl=False):
        self.name = name
        self.excl = excl
        self.writers = {}
        self.pwriters = {}
        self.readers = {}
        self.dsem = None


class EngW:
    def __init__(self, fw, eng, name, is_pe=False):
        self.fw = fw
        self.eng = eng
        self.name = name
        self.is_pe = is_pe
        self.sem = Sem(fw, "e_" + name + "0", False)
        self.gen = 0
        self.waited = {}
        self.pending = False

    def wait_tok(self, sem, val):
        if sem.is_dma:
            val = sem.total
        if val <= 0:
            return
        if self.waited.get(sem, 0) >= val:
            return
        self.eng.wait_ge(sem.h, val)
        self.waited[sem] = val

    def roll(self):
        if self.sem.total >= SEM_ROLL and not self.pending:
            self.gen += 1
            self.sem = Sem(self.fw, "e_%s%d" % (self.name, self.gen), False)


class FW:
    def __init__(self, nc):
        self.nc = nc
        self.all_sems = []
        self.pe = EngW(self, nc.tensor, "pe", True)
        self.act = EngW(self, nc.scalar, "act")
        self.dve = EngW(self, nc.vector, "dve")
        self.pool = EngW(self, nc.gpsimd, "pool")
        self.sp = EngW(self, nc.sync, "sp")
        self.engs = [self.pe, self.act, self.dve, self.pool, self.sp]
        self.n_ins = 0
        self.free_dma = {False: [], True: []}

    def _deps(self, E, reads, writes, partial):
        def w(d, skip_same=False):
            for s, v in d.items():
                if E.is_pe and s is E.sem:
                    continue
                if skip_same and s is E.sem:
                    continue
                E.wait_tok(s, v)
        for b in reads:
            w(b.writers)
            w(b.pwriters)
            if b.excl:
                w(b.readers, skip_same=True)
        for b in writes:
            w(b.readers)
            w(b.writers)
            if not partial:
                w(b.pwriters)

    def _record(self, sem, val, reads, writes, partial):
        for b in writes:
            if not partial:
                b.writers = {sem: val}
                b.pwriters = {}
                b.readers = {}
            else:
                b.pwriters[sem] = val
        for b in reads:
            b.readers[sem] = val

    def op(self, E, fn, reads=(), writes=(), signal=True, partial=False):
        self._deps(E, reads, writes, partial)
        ins = fn()
        self.n_ins += 1
        if signal:
            ins.then_inc(E.sem.h, 1)
            E.sem.total += 1
            E.pending = False
            tokv = E.sem.total
        else:
            E.pending = True
            tokv = E.sem.total + 1
        self._record(E.sem, tokv, reads, writes, partial)
        if signal:
            E.roll()
        return ins

    def dma(self, Q, out, in_, reads=(), writes=(), primary=None, partial=False, **kw):
        self._deps(Q, reads, writes, partial)
        if primary is None:
            primary = writes[0] if writes else reads[0]
        if primary.dsem is None:
            primary.dsem = {}
        sw = Q is self.pool
        if sw not in primary.dsem or primary.dsem[sw].total >= SEM_ROLL:
            fl = self.free_dma[sw]
            while fl and fl[-1].total >= SEM_ROLL - 4000:
                fl.pop()
            if fl:
                primary.dsem[sw] = fl.pop()
            else:
                sm = Sem(self, "d%d" % len(self.all_sems), True)
                sm.sw = sw
                primary.dsem[sw] = sm
        ds = primary.dsem[sw]
        ins = Q.eng.dma_start(out=out, in_=in_, **kw)
        ins.then_inc(ds.h, 16)
        ds.total += 16
        self.n_ins += 1
        self._record(ds, ds.total, reads, writes, partial)
        return ins

    def barrier(self):
        for E in self.engs:
            for s in self.all_sems:
                if s.total > 0:
                    E.wait_tok(s, s.total)
        for s in self.all_sems:
            if s.is_dma and s.total < SEM_ROLL - 4000 and s not in self.free_dma[s.sw]:
                self.free_dma[s.sw].append(s)

    def final_wait(self, E):
        for s in self.all_sems:
            if s.total > 0:
                E.wait_tok(s, s.total)


import numpy as np
from contextlib import ExitStack
import concourse.bass as bass
import concourse.mybir as mybir
from concourse.bass_utils import run_bass_kernel_spmd

NT0 = 34
T0 = 4352
NCTX = 256
NLAT = 4096
D = 1024
EPS = 1e-6


def host_prep(inputs, b):
    f = lambda a: np.ascontiguousarray(a, dtype=np.float32)
    m = {}
    m["x"] = f(inputs["x"][b])
    m["ctx"] = f(inputs["ctx"][b])
    cc = np.stack([inputs["c"][b], inputs["c_ctx"]], axis=-1)
    m["ccT"] = f(cc.reshape(8, 128, 2).transpose(1, 0, 2))
    m["ada_w"] = f(inputs["ada_w"])
    m["ada_bT"] = f(inputs["ada_b"].reshape(2, 48, 128).transpose(0, 2, 1))
    m["n1gT"] = f(inputs["norm1_g"].reshape(2, 8, 128).transpose(0, 2, 1))
    m["n2gT"] = f(inputs["norm2_g"].reshape(2, 8, 128).transpose(0, 2, 1))
    m["final_g"] = f(inputs["final_g"].reshape(1, 1024))
    w_in = inputs["ev_w_in"][0]
    cq, ckv, kr, ux, ug = np.split(w_in, [384, 640, 672, 1184], axis=1)
    m["w_in"] = f(np.concatenate([ux, ug, cq, ckv, kr], axis=1))
    return m


class K:
    pass


class NCProxy:
    def __init__(self, nc):
        object.__setattr__(self, "_nc", nc)
        object.__setattr__(self, "_cnt", [0])

    def __getattr__(self, name):
        return getattr(self._nc, name)

    def sbuf_tensor(self, name, *a, **kw):
        self._cnt[0] += 1
        return self._nc.sbuf_tensor("%s_u%d" % (name, self._cnt[0]), *a, **kw)

    def psum_tensor(self, name, *a, **kw):
        self._cnt[0] += 1
        return self._nc.psum_tensor("%s_u%d" % (name, self._cnt[0]), *a, **kw)


def build(nc, dbg=None):
    nc = NCProxy(nc)
    fw = FW(nc)
    k = K()
    k.fw = fw
    k.nc = nc
    dbg = dbg or {}
    dram_in = lambda name, shape: nc.dram_tensor(name, list(shape), F32, kind="ExternalInput").ap()
    k.x = dram_in("x", [NLAT, D])
    k.ctx = dram_in("ctx", [NCTX, D])
    k.ccT = dram_in("ccT", [128, 8, 2])
    k.ada_w = dram_in("ada_w", [2, D, 6 * D])
    k.ada_bT = dram_in("ada_bT", [2, 128, 48])
    k.n1gT = dram_in("n1gT", [2, 128, 8])
    k.n2gT = dram_in("n2gT", [2, 128, 8])
    k.final_g = dram_in("final_g", [1, D])
    k.w_in = dram_in("w_in", [D, 1728])
    k.out = nc.dram_tensor("out", [NLAT, D], F32, kind="ExternalOutput").ap()
    k.dbg = {}
    for name, shape in dbg.items():
        k.dbg[name] = nc.dram_tensor("dbg_" + name, list(shape), F32, kind="ExternalOutput").ap()

    k.ident_bf = nc.alloc_sbuf_tensor("ident_bf", [128, 128], BF16)
    k.ident_f = nc.alloc_sbuf_tensor("ident_f", [128, 128], F32)
    k.ones_bf = nc.alloc_sbuf_tensor("ones_bf", [128, 128], BF16)
    k.ones_f = nc.alloc_sbuf_tensor("ones_f", [128, 128], F32)
    k.modT = nc.alloc_sbuf_tensor("modT", [128, 2, 48, 2], F32)
    k.gsT = nc.alloc_sbuf_tensor("gsT", [128, 2, 2, 8, 2], F32)
    k.nexp = nc.alloc_sbuf_tensor("nexp", [128, 1], F32)
    k.cbuf = Buf("consts")
    fw.op(fw.pool, lambda: nc.gpsimd.memset(k.ident_f[:], 0.0), writes=[k.cbuf])
    k.iota_p = nc.alloc_sbuf_tensor("iota_p", [128, 1], F32)
    k.iota_f = nc.alloc_sbuf_tensor("iota_f", [128, 128], F32)
    fw.op(fw.pool, lambda: nc.gpsimd.iota(k.iota_p[:], pattern=[[0, 1]], base=0, channel_multiplier=1,
                                          allow_small_or_imprecise_dtypes=True), writes=[k.cbuf], partial=True)
    fw.op(fw.pool, lambda: nc.gpsimd.iota(k.iota_f[:], pattern=[[1, 128]], base=0, channel_multiplier=0,
                                          allow_small_or_imprecise_dtypes=True), writes=[k.cbuf], partial=True)
    fw.op(fw.dve, lambda: nc.vector.tensor_scalar(out=k.ident_f[:], in0=k.iota_f[:], scalar1=k.iota_p[:, 0:1],
                                                  scalar2=None, op0=ALU.is_equal), reads=[k.cbuf], writes=[k.cbuf])
    fw.op(fw.dve, lambda: nc.vector.tensor_copy(out=k.ident_bf[:], in_=k.ident_f[:]), reads=[k.cbuf], writes=[k.cbuf], partial=True)
    fw.op(fw.dve, lambda: nc.vector.memset(k.ones_bf[:], 1.0), writes=[k.cbuf], partial=True)
    fw.op(fw.dve, lambda: nc.vector.memset(k.ones_f[:], 1.0), writes=[k.cbuf], partial=True)
    fw.op(fw.dve, lambda: nc.vector.memset(k.nexp[:], -0.5), writes=[k.cbuf], partial=True)
    return k


def stage_mod(k):
    nc, fw = k.nc, k.fw
    with ExitStack() as es:
        cc = es.enter_context(nc.sbuf_tensor("m_cc", [128, 8, 2], F32))
        sc = es.enter_context(nc.sbuf_tensor("m_sc", [128, 8, 2], F32))
        abT = es.enter_context(nc.sbuf_tensor("m_abT", [128, 2, 48], F32))
        gT = es.enter_context(nc.sbuf_tensor("m_gT", [128, 2, 2, 8], F32))
        NW = 768
        wst = [es.enter_context(nc.sbuf_tensor("m_w%d" % i, [128, 8, NW], F32)) for i in range(2)]
        ps = es.enter_context(nc.psum_tensor("m_ps", [128, 512], F32))
        b_cc, b_ab, b_g, b_ps = Buf("cc"), Buf("ab"), Buf("g"), Buf("mps", excl=True)
        b_w = [Buf("mw0"), Buf("mw1")]
        fw.dma(fw.sp, cc[:], k.ccT, writes=[b_cc])
        for l in range(2):
            fw.dma(fw.sp, abT[:, l, :], k.ada_bT[l], writes=[b_ab], partial=True)
            fw.dma(fw.sp, gT[:, l, 0, :], k.n1gT[l], writes=[b_g], partial=True)
            fw.dma(fw.sp, gT[:, l, 1, :], k.n2gT[l], writes=[b_g], partial=True)
        fw.op(fw.act, lambda: nc.scalar.activation(out=sc[:], in_=cc[:], func=AF.Silu), reads=[b_cc], writes=[b_cc])
        it = 0
        for l in range(2):
            for cg in range(8):
                slot = it % 2
                it += 1
                src = k.ada_w[l].rearrange("(kc p) n -> p kc n", p=128)[:, :, cg * NW:(cg + 1) * NW]
                fw.dma(fw.sp, wst[slot][:], src, writes=[b_w[slot]])
                for mi in range(NW // 128):
                    mc = cg * (NW // 128) + mi
                    for kc in range(8):
                        last = (kc == 7)
                        fw.op(fw.pe, lambda: nc.tensor.matmul(ps[:, mc * 2:mc * 2 + 2],
                                                              lhsT=wst[slot][:, kc, mi * 128:(mi + 1) * 128],
                                                              rhs=sc[:, kc, :], start=(kc == 0), stop=last),
                              reads=[b_w[slot], b_cc], writes=[b_ps], signal=last, partial=True)
            fw.op(fw.dve, lambda: nc.vector.tensor_tensor(out=k.modT[:, l, :, :],
                                                          in0=ps[:, 0:96].rearrange("p (c j) -> p c j", j=2),
                                                          in1=abT[:, l, :].unsqueeze(2).to_broadcast([128, 48, 2]),
                                                          op=ALU.add),
                  reads=[b_ps, b_ab], writes=[k.cbuf], partial=True)
            b_ps.readers[fw.dve.sem] = fw.dve.sem.total
            for w, c0 in ((0, 8), (1, 32)):
                fw.op(fw.dve, lambda: nc.vector.scalar_tensor_tensor(
                    out=k.gsT[:, l, w, :, :], in0=k.modT[:, l, c0:c0 + 8, :], scalar=1.0,
                    in1=gT[:, l, w, :].unsqueeze(2).to_broadcast([128, 8, 2]), op0=ALU.add, op1=ALU.mult),
                    reads=[k.cbuf, b_g], writes=[k.cbuf], partial=True)
        fw.barrier()


def norm_stats_xn(k, es_bufs, xt, b_x, xn, b_xn, tagbufs):
    nc, fw = k.nc, k.fw
    junk, ss, b_t = tagbufs
    fw.op(fw.act, lambda: nc.scalar.activation(out=junk[:], in_=xt, func=AF.Square, accum_out=ss[:, 0:1]),
          reads=[b_x], writes=[b_t])
    fw.op(fw.dve, lambda: nc.vector.tensor_scalar(out=ss[:, 1:2], in0=ss[:, 0:1], scalar1=1.0 / D, scalar2=EPS,
                                                  op0=ALU.mult, op1=ALU.add), reads=[b_t], writes=[b_t])
    fw.op(fw.pool, lambda: nc.gpsimd.tensor_tensor(out=ss[:, 2:3], in0=ss[:, 1:2], in1=k.nexp[:, 0:1], op=ALU.pow),
          reads=[b_t, k.cbuf], writes=[b_t])
    fw.op(fw.dve, lambda: nc.vector.tensor_scalar(out=xn, in0=xt, scalar1=ss[:, 2:3], scalar2=None, op0=ALU.mult),
          reads=[b_x, b_t], writes=[b_xn])


TBLK = [(0, 256)] + [(256 + 512 * i, 512) for i in range(8)]


def stage_l0_norm1(k, es):
    nc, fw = k.nc, k.fw
    k.hT = es.enter_context(nc.sbuf_tensor("hT", [128, 8, T0], BF16))
    k.b_hT = [Buf("hT%d" % i) for i in range(NT0)]
    with ExitStack() as es2:
        norm_transpose_pass(k, es2, 0, 0, NT0,
                            lambda i: (k.ctx[i * 128:(i + 1) * 128, :] if i < 2 else k.x[(i - 2) * 128:(i - 1) * 128, :]),
                            lambda i: 1 if i < 2 else 0, k.hT, k.b_hT, None)
        fw.barrier()


def norm_transpose_pass(k, es, layer, which, ntiles, src_fn, j_fn, hT, b_hT, src_buf):
    nc, fw = k.nc, k.fw
    NS = 4
    N2 = 4
    xts = [es.enter_context(nc.sbuf_tensor("nt_x%d" % i, [128, D], F32)) for i in range(NS)]
    xns = [es.enter_context(nc.sbuf_tensor("nt_xn%d" % i, [128, D], BF16)) for i in range(N2)]
    junks = [es.enter_context(nc.sbuf_tensor("nt_j%d" % i, [128, D], BF16)) for i in range(N2)]
    sss = [es.enter_context(nc.sbuf_tensor("nt_s%d" % i, [128, 4], F32)) for i in range(N2)]
    tps = [es.enter_context(nc.psum_tensor("nt_tp%d" % i, [128, D], BF16)) for i in range(N2)]
    b_x = [Buf() for _ in range(NS)]
    b_xn = [Buf() for _ in range(N2)]
    b_t = [Buf() for _ in range(N2)]
    b_tp = [Buf(excl=True) for _ in range(N2)]
    sh_c0 = 0 if which == 0 else 24
    for i in range(ntiles):
        s3, s2 = i % NS, i % N2
        j = j_fn(i)
        rd = [src_buf] if src_buf is not None else []
        fw.dma(fw.sp, xts[s3][:], src_fn(i), reads=rd, writes=[b_x[s3]], primary=b_x[s3])
        norm_stats_xn(k, None, xts[s3][:], b_x[s3], xns[s2][:], b_xn[s2], (junks[s2], sss[s2], b_t[s2]))
        for kc in range(8):
            fw.op(fw.pe, lambda: nc.tensor.transpose(out=tps[s2][:, kc * 128:(kc + 1) * 128],
                                                     in_=xns[s2][:, kc * 128:(kc + 1) * 128], identity=k.ident_bf[:]),
                  reads=[b_xn[s2], k.cbuf], writes=[b_tp[s2]], signal=(kc == 7), partial=(kc > 0))
        for kc in range(8):
            o = hT[:, kc, i * 128:(i + 1) * 128]
            src = tps[s2][:, kc * 128:(kc + 1) * 128]
            sc_ap = k.gsT[:, layer, which, kc, j:j + 1]
            bi_ap = k.modT[:, layer, sh_c0 + kc, j:j + 1]
            if i % 2 == 0:
                fw.op(fw.act, lambda: nc.scalar.activation(out=o, in_=src, func=AF.Identity, bias=bi_ap, scale=sc_ap),
                      reads=[b_tp[s2], k.cbuf], writes=[b_hT[i]], partial=(kc > 0))
            else:
                fw.op(fw.dve, lambda: nc.vector.tensor_scalar(out=o, in0=src, scalar1=sc_ap, scalar2=bi_ap,
                                                              op0=ALU.mult, op1=ALU.add),
                      reads=[b_tp[s2], k.cbuf], writes=[b_hT[i]], partial=(kc > 0))


def load_w_bf16(k, dst, src_ap, buf, nparts=1):
    k.fw.dma(k.fw.pool, dst, src_ap, writes=[buf], partial=True)


def stage_l0_inproj_test(k, es):
    nc, fw = k.nc, k.fw
    w = es.enter_context(nc.sbuf_tensor("s_w_in", [128, 8, 1696], BF16))
    b_w = Buf("w_in")
    for kc in range(8):
        load_w_bf16(k, w[:, kc, :], k.w_in[kc * 128:(kc + 1) * 128, :], b_w)
    pss = [es.enter_context(nc.psum_tensor("ip_ps%d" % i, [128, 512], F32)) for i in range(2)]
    b_ps = [Buf(excl=True), Buf(excl=True)]
    obs = [es.enter_context(nc.sbuf_tensor("ip_o%d" % i, [128, 512], F32)) for i in range(2)]
    b_o = [Buf(), Buf()]
    chunks = [(c * 128, 128) for c in range(13)] + [(1664, 32)]
    it = 0
    for (f0, fs) in chunks:
        for (t0, ts) in TBLK:
            s = it % 2
            it += 1
            tiles = range(t0 // 128, (t0 + ts) // 128)
            for kc in range(8):
                fw.op(fw.pe, lambda: nc.tensor.matmul(pss[s][:fs, :ts], lhsT=w[:, kc, f0:f0 + fs], rhs=k.hT[:, kc, t0:t0 + ts],
                                                      start=(kc == 0), stop=(kc == 7)),
                      reads=[b_w] + [k.b_hT[i] for i in tiles], writes=[b_ps[s]], signal=(kc == 7), partial=(kc > 0))
            fw.op(fw.act, lambda: nc.scalar.copy(out=obs[s][:fs, :ts], in_=pss[s][:fs, :ts]), reads=[b_ps[s]], writes=[b_o[s]])
            fw.dma(fw.sp, k.dbg["projT"][f0:f0 + fs, t0:t0 + ts], obs[s][:fs, :ts], reads=[b_o[s]])


def finish(k):
    k.fw.final_wait(k.fw.sp)


C1 = 0.7978845608028654
C2 = C1 * 0.044715


def host_prep2(inputs, b, m):
    f = lambda a: np.ascontiguousarray(a, dtype=np.float32)
    w_in = inputs["ev_w_in"][0]
    cq, ckv, kr, ux, ug = np.split(w_in, [384, 640, 672, 1184], axis=1)
    perm = np.arange(32) ^ 8
    m["w_in"] = f(np.concatenate([ux, ug, cq, ckv, kr, kr[:, perm]], axis=1))
    cw = inputs["lru_conv_w"][0]
    m["lru_cw"] = f(cw.reshape(4, 4, 128).transpose(2, 1, 0))
    m["lru_cb"] = f(inputs["lru_conv_b"][0].reshape(4, 128).T)
    wbd = np.zeros((2, 2, 4, 128, 128), np.float32)
    for wi, key in enumerate(("lru_w_a", "lru_w_x")):
        w = inputs[key][0]
        for d in range(2):
            for j in range(4):
                for hh in range(2):
                    wbd[wi, d, j, hh * 64:(hh + 1) * 64, hh * 64:(hh + 1) * 64] = w[d, 2 * j + hh]
    m["lru_wbd"] = f(wbd.transpose(3, 0, 1, 2, 4).reshape(128, 16, 128))
    vec = lambda a: f(a.reshape(2, 4, 128).transpose(2, 0, 1))
    m["lru_ba"] = vec(inputs["lru_b_a"][0])
    m["lru_bx"] = vec(inputs["lru_b_x"][0])
    m["lru_lam"] = vec(inputs["lru_lambda"][0])
    return m


def declare2(k):
    nc = k.nc
    din = lambda name, shape: nc.dram_tensor(name, list(shape), F32, kind="ExternalInput").ap()
    k.lru_cw = din("lru_cw", [128, 4, 4])
    k.lru_cb = din("lru_cb", [128, 4])
    k.lru_wbd = din("lru_wbd", [128, 16, 128])
    k.lru_ba = din("lru_ba", [128, 2, 4])
    k.lru_bx = din("lru_bx", [128, 2, 4])
    k.lru_lam = din("lru_lam", [128, 2, 4])
    k.mixT = nc.dram_tensor("mixT", [1024, T0], BF16, kind="Internal").ap()
    k.b_mixT = Buf("mixT")


def inproj_block(k, w, b_w, f0, fs, t0, ts, ps, b_ps):
    nc, fw = k.nc, k.fw
    tiles = range(t0 // 128, (t0 + ts) // 128)
    for kc in range(8):
        fw.op(fw.pe, lambda: nc.tensor.matmul(ps[:fs, :ts], lhsT=w[:, kc, f0:f0 + fs], rhs=k.hT[:, kc, t0:t0 + ts],
                                              start=(kc == 0), stop=(kc == 7)),
              reads=[b_w] + [k.b_hT[i] for i in tiles], writes=[b_ps], signal=(kc == 7), partial=(kc > 0))


def rev(t, t0, ts):
    return t[:, t0:t0 + ts][:, ::-1]


def ucol(t0):
    return 2 + t0 if t0 < NCTX else 261 + (t0 - NCTX)


def stage_l0_lru(k, es, w, b_w):
    nc, fw = k.nc, k.fw
    with ExitStack() as es2:
        sb = lambda name, shape, dt=F32: es2.enter_context(nc.sbuf_tensor(name, shape, dt))
        cw = sb("l_cw", [128, 4, 4]); cb = sb("l_cb", [128, 4])
        ba = sb("l_ba", [128, 2, 4]); bx = sb("l_bx", [128, 2, 4]); lam = sb("l_lam", [128, 2, 4])
        sp4 = sb("l_sp4", [128, 2, 4])
        wbd = sb("l_wbd", [128, 16, 128], BF16)
        phalf = sb("l_phalf", [128, 1])
        b_p = Buf("lru_params")
        for dst, src in ((cw, k.lru_cw), (cb, k.lru_cb), (ba, k.lru_ba), (bx, k.lru_bx), (lam, k.lru_lam)):
            fw.dma(fw.sp, dst[:], src, writes=[b_p], partial=True)
        fw.dma(fw.pool, wbd[:], k.lru_wbd, writes=[b_p], partial=True)
        fw.op(fw.dve, lambda: nc.vector.memset(phalf[:], 0.5), writes=[b_p], partial=True)
        fw.op(fw.dve, lambda: nc.vector.tensor_scalar(out=ba[:], in0=ba[:], scalar1=0.5, scalar2=None, op0=ALU.mult), reads=[b_p], writes=[b_p])
        fw.op(fw.dve, lambda: nc.vector.tensor_scalar(out=bx[:], in0=bx[:], scalar1=0.5, scalar2=None, op0=ALU.mult), reads=[b_p], writes=[b_p])
        fw.op(fw.act, lambda: nc.scalar.activation(out=sp4[:], in_=lam[:], func=AF.Exp, scale=-1.0), reads=[b_p], writes=[b_p])
        fw.op(fw.act, lambda: nc.scalar.activation(out=sp4[:], in_=sp4[:], func=AF.Ln, bias=1.0, scale=1.0), reads=[b_p], writes=[b_p])
        fw.op(fw.dve, lambda: nc.vector.tensor_scalar(out=sp4[:], in0=sp4[:], scalar1=-4.0, scalar2=None, op0=ALU.mult), reads=[b_p], writes=[b_p])
        UXW = 4358
        uxp = sb("l_uxp", [128, UXW]); b_uxp = Buf("uxp")
        u = sb("l_u", [128, T0]); b_u = Buf("u")
        ubf = sb("l_ubf", [128, T0], BF16); b_ubf = Buf("ubf")
        gg = sb("l_gg", [128, T0], BF16); b_gg = Buf("gg")
        NB = 2
        tmp = [[sb("l_t%d_%d" % (i, s), [128, 512]) for i in range(6)] for s in range(NB)]
        b_tmp = [[Buf() for i in range(6)] for s in range(NB)]
        lo = [sb("l_lo%d" % s, [128, 512], BF16) for s in range(NB)]
        b_lo = [Buf() for s in range(NB)]
        pss = [es2.enter_context(nc.psum_tensor("l_ps%d" % i, [128, 512], F32)) for i in range(4)]
        b_ps = [Buf(excl=True) for _ in range(4)]
        fw.op(fw.pool, lambda: nc.gpsimd.memset(uxp[:], 0.0), writes=[b_uxp])
        pi = 0
        it = 0
        for j in range(4):
            for (t0, ts) in TBLK:
                p = pi % 4; pi += 1
                inproj_block(k, w, b_w, j * 128, 128, t0, ts, pss[p], b_ps[p])
                c0 = ucol(t0)
                fw.op(fw.act, lambda: nc.scalar.copy(out=uxp[:, c0:c0 + ts], in_=pss[p][:, :ts]), reads=[b_ps[p]], writes=[b_uxp], partial=True)
            for (t0, ts) in TBLK:
                p = pi % 4; pi += 1
                s = it % NB; it += 1
                inproj_block(k, w, b_w, 512 + j * 128, 128, t0, ts, pss[p], b_ps[p])
                xg, sq, th = tmp[s][0], tmp[s][1], tmp[s][2]
                fw.op(fw.act, lambda: nc.scalar.copy(out=xg[:, :ts], in_=pss[p][:, :ts]), reads=[b_ps[p]], writes=[b_tmp[s][0]])
                fw.op(fw.dve, lambda: nc.vector.tensor_tensor(out=sq[:, :ts], in0=xg[:, :ts], in1=xg[:, :ts], op=ALU.mult), reads=[b_tmp[s][0]], writes=[b_tmp[s][1]])
                fw.op(fw.dve, lambda: nc.vector.tensor_scalar(out=sq[:, :ts], in0=sq[:, :ts], scalar1=C2, scalar2=C1, op0=ALU.mult, op1=ALU.add), reads=[b_tmp[s][1]], writes=[b_tmp[s][1]])
                fw.op(fw.dve, lambda: nc.vector.tensor_tensor(out=sq[:, :ts], in0=sq[:, :ts], in1=xg[:, :ts], op=ALU.mult), reads=[b_tmp[s][0], b_tmp[s][1]], writes=[b_tmp[s][1]])
                fw.op(fw.act, lambda: nc.scalar.activation(out=th[:, :ts], in_=sq[:, :ts], func=AF.Tanh), reads=[b_tmp[s][1]], writes=[b_tmp[s][2]])
                fw.op(fw.dve, lambda: nc.vector.scalar_tensor_tensor(out=gg[:, t0:t0 + ts], in0=th[:, :ts], scalar=1.0, in1=xg[:, :ts], op0=ALU.add, op1=ALU.mult),
                      reads=[b_tmp[s][2], b_tmp[s][0]], writes=[b_gg], partial=True)
            for (o0, n, base) in ((0, NCTX, 0), (NCTX, NLAT, 259)):
                fw.op(fw.act, lambda: nc.scalar.activation(out=u[:, o0:o0 + n], in_=uxp[:, base:base + n], func=AF.Identity,
                                                           bias=cb[:, j:j + 1], scale=cw[:, j, 0:1]),
                      reads=[b_uxp, b_p], writes=[b_u], partial=True)
                for tap in range(1, 4):
                    E = fw.dve if tap != 2 else fw.pool
                    eng = nc.vector if tap != 2 else nc.gpsimd
                    if tap != 2:
                        fw.op(fw.dve, lambda: nc.vector.scalar_tensor_tensor(out=u[:, o0:o0 + n], in0=uxp[:, base + tap:base + tap + n], scalar=cw[:, j, tap:tap + 1],
                                                                             in1=u[:, o0:o0 + n], op0=ALU.mult, op1=ALU.add),
                              reads=[b_uxp, b_p, b_u], writes=[b_u], partial=True)
                    else:
                        fw.op(fw.dve, lambda: nc.vector.scalar_tensor_tensor(out=u[:, o0:o0 + n], in0=uxp[:, base + tap:base + tap + n], scalar=cw[:, j, tap:tap + 1],
                                                                             in1=u[:, o0:o0 + n], op0=ALU.mult, op1=ALU.add),
                              reads=[b_uxp, b_p, b_u], writes=[b_u], partial=True)
            fw.op(fw.act, lambda: nc.scalar.copy(out=ubf[:], in_=u[:]), reads=[b_u], writes=[b_ubf])
            yb = uxp
            b_yb = b_uxp
            for d in (1, 0):
                order = [TBLK[0]] + (TBLK[1:] if d == 0 else TBLK[:0:-1])
                prev = None
                for bi, (t0, ts) in enumerate(order):
                    s = it % NB; it += 1
                    pa = pi % 4; pi += 1
                    px = pi % 4; pi += 1
                    ia = (0 * 2 + d) * 4 + j
                    ix = (1 * 2 + d) * 4 + j
                    fw.op(fw.pe, lambda: nc.tensor.matmul(pss[pa][:, :ts], lhsT=wbd[:, ia, :], rhs=ubf[:, t0:t0 + ts], start=True, stop=True),
                          reads=[b_p, b_ubf], writes=[b_ps[pa]])
                    fw.op(fw.pe, lambda: nc.tensor.matmul(pss[px][:, :ts], lhsT=wbd[:, ix, :], rhs=ubf[:, t0:t0 + ts], start=True, stop=True),
                          reads=[b_p, b_ubf], writes=[b_ps[px]])
                    tr, a, ti, om, iu, bb = tmp[s]
                    btr, bA, bti, bom, biu, bbb = b_tmp[s]
                    fw.op(fw.act, lambda: nc.scalar.activation(out=tr[:, :ts], in_=pss[pa][:, :ts], func=AF.Tanh, bias=ba[:, d, j:j + 1], scale=0.5), reads=[b_ps[pa], b_p], writes=[btr])
                    fw.op(fw.act, lambda: nc.scalar.activation(out=ti[:, :ts], in_=pss[px][:, :ts], func=AF.Tanh, bias=bx[:, d, j:j + 1], scale=0.5), reads=[b_ps[px], b_p], writes=[bti])
                    fw.op(fw.act, lambda: nc.scalar.activation(out=a[:, :ts], in_=tr[:, :ts], func=AF.Exp, bias=sp4[:, d, j:j + 1], scale=sp4[:, d, j:j + 1]), reads=[btr, b_p], writes=[bA])
                    fw.op(fw.dve, lambda: nc.vector.tensor_tensor(out=om[:, :ts], in0=a[:, :ts], in1=a[:, :ts], op=ALU.mult), reads=[bA], writes=[bom])
                    fw.op(fw.dve, lambda: nc.vector.tensor_scalar(out=om[:, :ts], in0=om[:, :ts], scalar1=-1.0, scalar2=1.0, op0=ALU.mult, op1=ALU.add), reads=[bom], writes=[bom])
                    fw.op(fw.dve, lambda: nc.vector.tensor_scalar_max(out=om[:, :ts], in0=om[:, :ts], scalar1=1e-30), reads=[bom], writes=[bom])
                    fw.op(fw.act, lambda: nc.scalar.activation(out=om[:, :ts], in_=om[:, :ts], func=AF.Sqrt), reads=[bom], writes=[bom])
                    fw.op(fw.dve, lambda: nc.vector.scalar_tensor_tensor(out=iu[:, :ts], in0=ti[:, :ts], scalar=1.0, in1=u[:, t0:t0 + ts], op0=ALU.add, op1=ALU.mult), reads=[bti, b_u], writes=[biu])
                    fw.op(fw.dve, lambda: nc.vector.scalar_tensor_tensor(out=bb[:, :ts], in0=om[:, :ts], scalar=0.5, in1=iu[:, :ts], op0=ALU.mult, op1=ALU.mult), reads=[bom, biu], writes=[bbb])
                    if d == 1:
                        c0 = t0
                        init = 0.0 if bi == 0 else yb[:, prev:prev + 1]
                        fw.op(fw.dve, lambda: nc.vector.tensor_tensor_scan(out=rev(yb, t0, ts), data0=rev(a, 0, ts), data1=rev(bb, 0, ts),
                                                                           initial=init, op0=ALU.mult, op1=ALU.add),
                              reads=[bA, bbb, b_yb], writes=[b_yb], partial=True)
                        prev = t0
                    else:
                        yf = tmp[s][0]
                        rd = [bA, bbb] + ([b_tmp[1 - s][0]] if bi > 0 else [])
                        init = 0.0 if bi == 0 else k._yf_last
                        fw.op(fw.dve, lambda: nc.vector.tensor_tensor_scan(out=yf[:, :ts], data0=a[:, :ts], data1=bb[:, :ts], initial=init, op0=ALU.mult, op1=ALU.add),
                              reads=rd, writes=[btr])
                        k._yf_last = yf[:, ts - 1:ts]
                        fw.op(fw.dve, lambda: nc.vector.tensor_tensor(out=om[:, :ts], in0=yf[:, :ts], in1=yb[:, t0:t0 + ts], op=ALU.add), reads=[btr, b_yb], writes=[bom])
                        fw.op(fw.dve, lambda: nc.vector.scalar_tensor_tensor(out=lo[s][:, :ts], in0=om[:, :ts], scalar=0.5, in1=gg[:, t0:t0 + ts], op0=ALU.mult, op1=ALU.mult),
                              reads=[bom, b_gg], writes=[b_lo[s]])
                        fw.dma(fw.sp, k.mixT[512 + j * 128:512 + (j + 1) * 128, t0:t0 + ts], lo[s][:, :ts], reads=[b_lo[s]], writes=[k.b_mixT], primary=b_lo[s], partial=True)
            if j < 3:
                fw.op(fw.pool, lambda: nc.gpsimd.memset(uxp[:], 0.0), writes=[b_uxp])
        fw.barrier()


import math

SM_SCALE = 96.0 ** -0.5


def host_prep3(inputs, b, m):
    f = lambda a: np.ascontiguousarray(a, dtype=np.float32)
    perm = np.arange(32) ^ 8
    m["gq"] = f(inputs["mla_q_norm_g"][0].reshape(3, 128).T)
    m["gkv"] = f(inputs["mla_kv_norm_g"][0].reshape(2, 128).T)
    wq = inputs["mla_w_uq"][0].reshape(384, 8, 96)
    A = wq
    Bm = np.concatenate([np.zeros((384, 8, 64), np.float32), wq[:, :, 64:96][:, :, perm]], axis=2)
    wqAB = np.concatenate([A.reshape(384, 768), Bm.reshape(384, 768)], axis=1)
    m["w_uq"] = f(wqAB.reshape(3, 128, 1536).transpose(1, 0, 2))
    wkv = inputs["mla_w_ukv"][0].reshape(256, 8, 128)
    wk = wkv[:, :, 0:64]
    wv = wkv[:, :, 64:128]
    wkv2 = np.concatenate([wk.reshape(256, 512), wv.reshape(256, 512)], axis=1)
    m["w_ukv"] = f(wkv2.reshape(2, 128, 1024).transpose(1, 0, 2))
    t = np.arange(NLAT)
    pos = np.stack([t // 64, t % 64], axis=0).astype(np.float32)
    inv = (10000.0 ** (-np.arange(8, dtype=np.float32) / 8)).astype(np.float32)
    C = np.ones((32, T0), np.float32); S = np.zeros((32, T0), np.float32)
    for axis in range(2):
        for ab in range(2):
            for p in range(8):
                r = axis * 16 + ab * 8 + p
                ang = (pos[axis] * inv[p]).astype(np.float32)
                C[r, NCTX:] = np.cos(ang)
                S[r, NCTX:] = np.sin(ang) * (-1.0 if ab == 0 else 1.0)
    m["ropeC"] = f(C); m["ropeS"] = f(S)
    return m


def declare3(k):
    nc = k.nc
    din = lambda name, shape: nc.dram_tensor(name, list(shape), F32, kind="ExternalInput").ap()
    k.gq = din("gq", [128, 3]); k.gkv = din("gkv", [128, 2])
    k.w_uq = din("w_uq", [128, 3, 1536]); k.w_ukv = din("w_ukv", [128, 2, 1024])
    k.ropeC = din("ropeC", [32, T0]); k.ropeS = din("ropeS", [32, T0])


def alloc_mla_lat(k, es):
    nc = k.nc
    k.cqTd = nc.dram_tensor("cqTd", [384, T0], BF16, kind="Internal").ap()
    k.ckvTd = nc.dram_tensor("ckvTd", [256, T0], BF16, kind="Internal").ap()
    k.krrd = nc.dram_tensor("krrd", [32, T0], BF16, kind="Internal").ap()
    k.b_latd = Buf("latd")


def load_rope(k, es):
    nc, fw = k.nc, k.fw
    sbp = lambda name, shape, dt=F32: es.enter_context(nc.sbuf_tensor(name, shape, dt))
    k.rC = sbp("ropeC_s", [96, T0], BF16); k.rS = sbp("ropeS_s", [96, T0], BF16); k.b_rope = Buf("rope")
    fw.dma(fw.pool, k.rC[64:96, :], k.ropeC, writes=[k.b_rope], partial=True)
    fw.dma(fw.pool, k.rS[64:96, :], k.ropeS, writes=[k.b_rope], partial=True)


def stage_l0_mla_lat(k, w, b_w):
    nc, fw = k.nc, k.fw
    with ExitStack() as es2:
        sb = lambda name, shape, dt=F32: es2.enter_context(nc.sbuf_tensor(name, shape, dt))
        load_rope(k, es2)
        cqb = [sb("p3_cq%d" % i, [128, 3, 512], BF16) for i in range(2)]; b_cqb = [Buf() for _ in range(2)]
        ckvb = [sb("p3_ckv%d" % i, [128, 2, 512], BF16) for i in range(2)]; b_ckvb = [Buf() for _ in range(2)]
        krb = [sb("p3_kr%d" % i, [96, 512], BF16) for i in range(2)]; b_krb = [Buf() for _ in range(2)]
        pss = [es2.enter_context(nc.psum_tensor("p3_ps%d" % i, [128, 512], F32)) for i in range(4)]
        b_ps = [Buf(excl=True) for _ in range(4)]
        pq = [es2.enter_context(nc.psum_tensor("p3_pq%d" % i, [128, 512], F32)) for i in range(2)]
        b_pq = [Buf(excl=True) for _ in range(2)]
        sqt = [sb("p3_sq%d" % i, [128, 512], BF16) for i in range(3)]; b_sq = [Buf() for _ in range(3)]
        rr = [sb("p3_rr%d" % i, [128, 512]) for i in range(2)]; b_rr = [Buf() for _ in range(2)]
        t1 = [sb("p3_t1_%d" % i, [96, 512]) for i in range(2)]; t2 = [sb("p3_t2_%d" % i, [96, 512]) for i in range(2)]
        b_t1 = [Buf() for _ in range(2)]; b_t2 = [Buf() for _ in range(2)]
        pi = 0; si = 0; qi = 0
        for bi, (t0, ts) in enumerate(TBLK):
            bs = bi % 2
            for (dstt, b_dst, f_base, nch, width, dd) in ((cqb[bs], b_cqb[bs], 1024, 3, 384.0, k.cqTd), (ckvb[bs], b_ckvb[bs], 1408, 2, 256.0, k.ckvTd)):
                q = qi % 2; qi += 1
                for c in range(nch):
                    p = pi % 4; pi += 1
                    s = si % 3; si += 1
                    inproj_block(k, w, b_w, f_base + c * 128, 128, t0, ts, pss[p], b_ps[p])
                    fw.op(fw.act, lambda: nc.scalar.copy(out=dstt[:, c, :ts], in_=pss[p][:, :ts]), reads=[b_ps[p]], writes=[b_dst], partial=(c > 0))
                    fw.op(fw.act, lambda: nc.scalar.activation(out=sqt[s][:, :ts], in_=pss[p][:, :ts], func=AF.Square), reads=[b_ps[p]], writes=[b_sq[s]])
                    fw.op(fw.pe, lambda: nc.tensor.matmul(pq[q][:, :ts], lhsT=k.ones_bf[:], rhs=sqt[s][:, :ts], start=(c == 0), stop=(c == nch - 1)),
                          reads=[b_sq[s], k.cbuf], writes=[b_pq[q]], signal=(c == nch - 1), partial=(c > 0))
                fw.op(fw.dve, lambda: nc.vector.tensor_scalar(out=rr[q][:, :ts], in0=pq[q][:, :ts], scalar1=1.0 / width, scalar2=EPS, op0=ALU.mult, op1=ALU.add),
                      reads=[b_pq[q]], writes=[b_rr[q]])
                fw.op(fw.act, lambda: nc.scalar.activation(out=rr[q][:, :ts], in_=rr[q][:, :ts], func=AF.Sqrt), reads=[b_rr[q]], writes=[b_rr[q]])
                fw.op(fw.dve, lambda: nc.vector.reciprocal(out=rr[q][:, :ts], in_=rr[q][:, :ts]), reads=[b_rr[q]], writes=[b_rr[q]])
                for c in range(nch):
                    fw.op(fw.dve, lambda: nc.vector.tensor_tensor(out=dstt[:, c, :ts], in0=dstt[:, c, :ts], in1=rr[q][:, :ts], op=ALU.mult),
                          reads=[b_rr[q], b_dst], writes=[b_dst], partial=True)
                fw.dma(fw.sp, dd[:, t0:t0 + ts].rearrange("(c p) t -> p c t", p=128), dstt[:, :, :ts], reads=[b_dst], writes=[k.b_latd], primary=b_dst, partial=True)
            pa = pi % 4; pi += 1
            pb = pi % 4; pi += 1
            q = qi % 2
            inproj_block(k, w, b_w, 1600, 96, t0, ts, pss[pa], b_ps[pa])
            inproj_block(k, w, b_w, 1632, 96, t0, ts, pss[pb], b_ps[pb])
            fw.op(fw.dve, lambda: nc.vector.tensor_tensor(out=t1[q][64:96, :ts], in0=pss[pa][64:96, :ts], in1=k.rC[64:96, t0:t0 + ts], op=ALU.mult), reads=[b_ps[pa], k.b_rope], writes=[b_t1[q]])
            fw.op(fw.dve, lambda: nc.vector.tensor_tensor(out=t2[q][64:96, :ts], in0=pss[pb][64:96, :ts], in1=k.rS[64:96, t0:t0 + ts], op=ALU.mult), reads=[b_ps[pb], k.b_rope], writes=[b_t2[q]])
            fw.op(fw.dve, lambda: nc.vector.tensor_tensor(out=krb[bs][64:96, :ts], in0=t1[q][64:96, :ts], in1=t2[q][64:96, :ts], op=ALU.add), reads=[b_t1[q], b_t2[q]], writes=[b_krb[bs]])
            fw.dma(fw.sp, k.krrd[:, t0:t0 + ts], krb[bs][64:96, :ts], reads=[b_krb[bs]], writes=[k.b_latd], primary=b_krb[bs], partial=True)
        fw.barrier()


def stage_l0_attn(k, es):
    nc, fw = k.nc, k.fw
    with ExitStack() as es2:
        sb = lambda name, shape, dt=F32: es2.enter_context(nc.sbuf_tensor(name, shape, dt))
        k.cqT = sb("cqT", [128, 3, T0], BF16); k.ckvT = sb("ckvT", [128, 2, T0], BF16); k.krr = sb("krr", [96, T0], BF16)
        b_lat = Buf("lat")
        k.b_cq = [b_lat for _ in TBLK]; k.b_ckv = [b_lat for _ in TBLK]; k.b_krr = b_lat
        fw.dma(fw.sp, k.cqT[:], k.cqTd.rearrange("(c p) t -> p c t", p=128), reads=[k.b_latd], writes=[b_lat], partial=True)
        fw.dma(fw.sp, k.ckvT[:], k.ckvTd.rearrange("(c p) t -> p c t", p=128), reads=[k.b_latd], writes=[b_lat], partial=True)
        fw.dma(fw.sp, k.krr[64:96, :], k.krrd, reads=[k.b_latd], writes=[b_lat], partial=True)
        load_rope(k, es2)
        gq = sb("a_gq", [128, 3]); gkv = sb("a_gkv", [128, 2])
        wq = sb("a_wq", [128, 3, 1536], BF16)
        wkv = sb("a_wkv", [128, 2, 1024], BF16)
        b_g, b_wst, b_wq, b_wkv = Buf(), Buf(), Buf(), Buf()
        fw.dma(fw.sp, gq[:], k.gq, writes=[b_g], partial=True)
        fw.dma(fw.sp, gkv[:], k.gkv, writes=[b_g], partial=True)
        with ExitStack() as es3:
            wst = es3.enter_context(nc.sbuf_tensor("a_wst", [128, 3, 1536], F32))
            fw.dma(fw.sp, wst[:, :, :], k.w_uq, writes=[b_wst])
            for c in range(3):
                fw.op(fw.dve, lambda: nc.vector.tensor_scalar(out=wq[:, c, :], in0=wst[:, c, :], scalar1=gq[:, c:c + 1], scalar2=SM_SCALE, op0=ALU.mult, op1=ALU.mult),
                      reads=[b_wst, b_g], writes=[b_wq], partial=True)
            fw.dma(fw.sp, wst[:, 0:2, 0:1024], k.w_ukv, reads=[], writes=[b_wst])
            for c in range(2):
                fw.op(fw.dve, lambda: nc.vector.tensor_scalar(out=wkv[:, c, :], in0=wst[:, c, 0:1024], scalar1=gkv[:, c:c + 1], scalar2=None, op0=ALU.mult),
                      reads=[b_wst, b_g], writes=[b_wkv], partial=True)
            fw.barrier()
        Vp = sb("a_Vp", [128, NT0, 4, 192], BF16); b_V = Buf("Vp")
        fw.op(fw.pool, lambda: nc.gpsimd.memset(Vp[:], 0.0), writes=[b_V])
        fw.op(fw.pool, lambda: nc.gpsimd.memset(Vp[:, :, :, 64:65], 1.0), writes=[b_V], partial=True)
        NPS = 3
        psS = [es2.enter_context(nc.psum_tensor("a_pS%d" % i, [128, 512], F32)) for i in range(NPS)]; b_pS = [Buf(excl=True) for _ in range(NPS)]
        psO = [es2.enter_context(nc.psum_tensor("a_pO%d" % i, [128, 512], F32)) for i in range(2)]; b_pO = [Buf(excl=True) for _ in range(2)]
        psP = [es2.enter_context(nc.psum_tensor("a_pP%d" % i, [128, 512], F32)) for i in range(2)]; b_pP = [Buf(excl=True) for _ in range(2)]
        psB = es2.enter_context(nc.psum_tensor("a_pB", [128, 512], F32)); b_pB = Buf(excl=True)
        ppi = 0
        for i in range(NT0):
            p = ppi % 2; ppi += 1
            blk = 0 if i < 2 else 1 + (i - 2) // 4
            for c in range(2):
                fw.op(fw.pe, lambda: nc.tensor.matmul(psP[p][:, :], lhsT=k.ckvT[:, c, i * 128:(i + 1) * 128], rhs=wkv[:, c, 512:1024], start=(c == 0), stop=(c == 1)),
                      reads=[k.b_ckv[blk], b_wkv], writes=[b_pP[p]], signal=(c == 1), partial=(c > 0))
            src = psP[p][:, :].rearrange("p (pr two d) -> p pr two d", two=2, d=64)
            fw.op(fw.dve, lambda: nc.vector.tensor_copy(out=Vp[:, i, :, 0:64], in_=src[:, :, 0, :]), reads=[b_pP[p]], writes=[b_V], partial=True)
            fw.op(fw.dve, lambda: nc.vector.tensor_copy(out=Vp[:, i, :, 128:192], in_=src[:, :, 1, :]), reads=[b_pP[p]], writes=[b_V], partial=True)
        qT = [sb("a_qT%d" % i, [96, T0], BF16) for i in range(2)]; b_qT = [Buf() for _ in range(2)]
        kT = [sb("a_kT%d" % i, [96, T0], BF16) for i in range(2)]; b_kT = [Buf() for _ in range(2)]
        NPT = 3
        PT = [sb("a_PT%d" % i, [128, 512], BF16) for i in range(NPT)]; b_PT = [Buf() for _ in range(NPT)]
        t1 = [sb("a_t1_0", [96, 512])] * 2; t2 = [sb("a_t2_0", [96, 512])] * 2
        b_t1 = [Buf()] * 2; b_t2 = [Buf()] * 2
        den = [sb("a_den0", [128, 512])] * 2; b_den = [Buf()] * 2
        bcs = [sb("a_bc0", [128, 512])] * 2; b_bc = [Buf()] * 2
        ao = [sb("a_ao%d" % i, [128, 512], BF16) for i in range(2)]; b_ao = [Buf() for _ in range(2)]
        ti = 0; si = 0; pti = 0; oi = 0

        def build_head(h):
            nonlocal ppi, ti
            hs = h % 2
            fw.op(fw.dve, lambda: nc.vector.tensor_copy(out=kT[hs][64:96, :], in_=k.krr[64:96, :]), reads=[k.b_krr], writes=[b_kT[hs]])
            for bi, (t0, ts) in enumerate(TBLK):
                p = ppi % 2; ppi += 1
                for c in range(2):
                    fw.op(fw.pe, lambda: nc.tensor.matmul(psP[p][:64, :ts], lhsT=wkv[:, c, h * 64:(h + 1) * 64], rhs=k.ckvT[:, c, t0:t0 + ts], start=(c == 0), stop=(c == 1)),
                          reads=[k.b_ckv[bi], b_wkv], writes=[b_pP[p]], signal=(c == 1), partial=(c > 0))
                fw.op(fw.dve, lambda: nc.vector.tensor_copy(out=kT[hs][0:64, t0:t0 + ts], in_=psP[p][0:64, :ts]), reads=[b_pP[p]], writes=[b_kT[hs]], partial=True)
            first = True
            for bi, (t0, ts) in enumerate(TBLK):
                pa = ppi % 2; ppi += 1
                t = ti % 2; ti += 1
                for c in range(3):
                    fw.op(fw.pe, lambda: nc.tensor.matmul(psP[pa][:96, :ts], lhsT=wq[:, c, h * 96:(h + 1) * 96], rhs=k.cqT[:, c, t0:t0 + ts], start=(c == 0), stop=(c == 2)),
                          reads=[k.b_cq[bi], b_wq], writes=[b_pP[pa]], signal=(c == 2), partial=(c > 0))
                for c in range(3):
                    fw.op(fw.pe, lambda: nc.tensor.matmul(psB[:96, :ts], lhsT=wq[:, c, 768 + h * 96:768 + (h + 1) * 96], rhs=k.cqT[:, c, t0:t0 + ts], start=(c == 0), stop=(c == 2)),
                          reads=[k.b_cq[bi], b_wq], writes=[b_pB], signal=(c == 2), partial=(c > 0))
                fw.op(fw.dve, lambda: nc.vector.tensor_copy(out=qT[hs][0:64, t0:t0 + ts], in_=psP[pa][0:64, :ts]), reads=[b_pP[pa]], writes=[b_qT[hs]], partial=not first)
                first = False
                fw.op(fw.dve, lambda: nc.vector.tensor_tensor(out=t1[t][64:96, :ts], in0=psP[pa][64:96, :ts], in1=k.rC[64:96, t0:t0 + ts], op=ALU.mult), reads=[b_pP[pa], k.b_rope], writes=[b_t1[t]])
                fw.op(fw.dve, lambda: nc.vector.tensor_tensor(out=t2[t][64:96, :ts], in0=psB[64:96, :ts], in1=k.rS[64:96, t0:t0 + ts], op=ALU.mult), reads=[b_pB, k.b_rope], writes=[b_t2[t]])
                fw.op(fw.dve, lambda: nc.vector.tensor_tensor(out=qT[hs][64:96, t0:t0 + ts], in0=t1[t][64:96, :ts], in1=t2[t][64:96, :ts], op=ALU.add), reads=[b_t1[t], b_t2[t]], writes=[b_qT[hs]], partial=True)

        def attend(h, q0, qn, ktiles):
            nonlocal si, pti, oi
            hs = h % 2; pr = h // 2; odd = h % 2
            o = oi % 2; oi += 1
            M = 128 if odd else 65
            c0 = 64 if odd else 0
            nk = len(ktiles)
            slots = {}

            def S_mm(idx):
                nonlocal si
                kt = ktiles[idx]
                s = si % NPS; si += 1
                slots[idx] = s
                fw.op(fw.pe, lambda: nc.tensor.matmul(psS[s][:, :qn], lhsT=kT[hs][:, kt * 128:(kt + 1) * 128], rhs=qT[hs][:, q0:q0 + qn], start=True, stop=True),
                      reads=[b_kT[hs], b_qT[hs]], writes=[b_pS[s]])

            def EX_PV(idx):
                nonlocal pti
                kt = ktiles[idx]
                s = slots.pop(idx)
                pt = pti % NPT; pti += 1
                fw.op(fw.act, lambda: nc.scalar.activation(out=PT[pt][:, :qn], in_=psS[s][:, :qn], func=AF.Exp), reads=[b_pS[s]], writes=[b_PT[pt]])
                fw.op(fw.pe, lambda: nc.tensor.matmul(psO[o][:M, :qn], lhsT=Vp[:, kt, pr, c0:c0 + M], rhs=PT[pt][:, :qn], start=(idx == 0), stop=(idx == nk - 1)),
                      reads=[b_V, b_PT[pt]], writes=[b_pO[o]], signal=(idx == nk - 1), partial=(idx > 0))

            LOOK = 2
            for idx in range(min(LOOK, nk)):
                S_mm(idx)
            for idx in range(nk):
                if idx + LOOK < nk:
                    S_mm(idx + LOOK)
                EX_PV(idx)
            dr = 0 if odd else 64
            r0 = 64 if odd else 0
            MB = 128 if odd else 64
            fw.op(fw.dve, lambda: nc.vector.reciprocal(out=den[o][dr:dr + 1, :qn], in_=psO[o][dr:dr + 1, :qn]), reads=[b_pO[o]], writes=[b_den[o]])
            fw.op(fw.pe, lambda: nc.tensor.matmul(psB[:MB, :qn], lhsT=k.ones_f[dr:dr + 1, 0:MB], rhs=den[o][dr:dr + 1, :qn], start=True, stop=True),
                  reads=[b_den[o], k.cbuf], writes=[b_pB])
            fw.op(fw.dve, lambda: nc.vector.tensor_copy(out=bcs[o][r0:r0 + 64, :qn], in_=psB[r0:r0 + 64, :qn]), reads=[b_pB], writes=[b_bc[o]])
            fw.op(fw.dve, lambda: nc.vector.tensor_tensor(out=ao[o][r0:r0 + 64, :qn], in0=psO[o][r0:r0 + 64, :qn], in1=bcs[o][r0:r0 + 64, :qn], op=ALU.mult),
                  reads=[b_pO[o], b_bc[o]], writes=[b_ao[o]])
            fw.dma(fw.sp, k.mixT[h * 64:(h + 1) * 64, q0:q0 + qn], ao[o][r0:r0 + 64, :qn], reads=[b_ao[o]], writes=[k.b_mixT], primary=b_ao[o], partial=True)

        NH = k.__dict__.get("dbg_nheads", 8)
        build_head(0)
        for h in range(NH):
            if h + 1 < NH:
                build_head(h + 1)
            for qb in range(k.__dict__.get("dbg_nqb", 8)):
                attend(h, NCTX + qb * 512, 512, list(range(NT0)))
        fw.barrier()


NTL = 32


def host_prep4(inputs, b, m):
    f = lambda a: np.ascontiguousarray(a, dtype=np.float32)
    m["w_out0"] = f(inputs["ev_w_out"][0])
    m["w_out1"] = f(inputs["od_w_out"][0])
    m["router_w"] = f(inputs["router_w"].reshape(8, 128, 16).transpose(1, 0, 2))
    m["router_b"] = f(inputs["router_b"].reshape(1, 16))
    m["moe_w1"] = f(inputs["moe_w1"]); m["moe_w3"] = f(inputs["moe_w3"]); m["moe_w2"] = f(inputs["moe_w2"])
    return m


def declare4(k):
    nc = k.nc
    din = lambda name, shape: nc.dram_tensor(name, list(shape), F32, kind="ExternalInput").ap()
    k.w_out = [din("w_out0", [D, D]), din("w_out1", [D, D])]
    k.router_w = din("router_w", [128, 8, 16]); k.router_b = din("router_b", [1, 16])
    k.moe_w1 = din("moe_w1", [2, 16, D, 512]); k.moe_w3 = din("moe_w3", [2, 16, D, 512]); k.moe_w2 = din("moe_w2", [2, 16, 512, D])
    k.xmid = nc.dram_tensor("xmid", [NLAT, D], F32, kind="Internal").ap(); k.b_xmid = Buf("xmid")
    k.xend = nc.dram_tensor("xend", [NLAT, D], F32, kind="Internal").ap(); k.b_xend = Buf("xend")
    k.h2T = nc.dram_tensor("h2T", [D, NLAT], BF16, kind="Internal").ap(); k.b_h2T = Buf("h2T")
    k.gates = nc.alloc_sbuf_tensor("gates", [128, NTL, 16], F32); k.b_gates = Buf("gates")
    k.rb = nc.alloc_sbuf_tensor("rb_bc", [128, 16], F32)
    k.rw = nc.alloc_sbuf_tensor("rw_bf", [128, 8, 16], BF16)
    k.g1b = nc.alloc_sbuf_tensor("g1b", [128, D], F32)
    k.g2b = nc.alloc_sbuf_tensor("g2b", [128, D], F32)
    k.b_gb = Buf("gb")
    fw = k.fw
    fw.dma(fw.sp, k.rb[:], k.router_b.to_broadcast([128, 16]), writes=[k.cbuf], partial=True)
    fw.dma(fw.pool, k.rw[:], k.router_w, writes=[k.cbuf], partial=True)


def make_bcast(k, es, layer, chunk0, j, dst, b_dst):
    nc, fw = k.nc, k.fw
    with ExitStack() as es2:
        dg = es2.enter_context(nc.sbuf_tensor("bc_dg", [128, 128], F32)); b_dg = Buf()
        ps = [es2.enter_context(nc.psum_tensor("bc_ps%d" % i, [128, 512], F32)) for i in range(2)]
        b_ps = [Buf(excl=True) for _ in range(2)]
        for c in range(8):
            fw.op(fw.dve, lambda: nc.vector.tensor_scalar(out=dg[:], in0=k.ident_f[:], scalar1=k.modT[:, layer, chunk0 + c, j:j + 1], scalar2=None, op0=ALU.mult),
                  reads=[k.cbuf], writes=[b_dg])
            hb = c // 4
            fw.op(fw.pe, lambda: nc.tensor.matmul(ps[hb][:, (c % 4) * 128:(c % 4 + 1) * 128], lhsT=k.ones_f[:], rhs=dg[:], start=True, stop=True),
                  reads=[b_dg, k.cbuf], writes=[b_ps[hb]], partial=(c % 4 > 0))
        for hb in range(2):
            fw.op(fw.dve, lambda: nc.vector.tensor_copy(out=dst[:, hb * 512:(hb + 1) * 512], in_=ps[hb][:]), reads=[b_ps[hb]], writes=[b_dst], partial=(hb > 0))
        fw.barrier()


def stage_outproj_norm2(k, layer, mix_dram, b_mix, mix_col0, x_row_ap, mix_tm=False):
    nc, fw = k.nc, k.fw
    with ExitStack() as es:
        make_bcast(k, es, layer, 16, 0, k.g1b, k.b_gb)
        sb = lambda name, shape, dt=F32: es.enter_context(nc.sbuf_tensor(name, shape, dt))
        wo = sb("o_w", [128, 8, D], BF16); b_wo = Buf()
        for c in range(8):
            fw.dma(fw.pool, wo[:, c, :], k.w_out[layer][c * 128:(c + 1) * 128, :], writes=[b_wo], partial=True)
        NS = 4
        NP = 2
        mts = [sb("o_mt%d" % i, [128, 8, 128], BF16) for i in range(NS)]; b_mt = [Buf() for _ in range(NS)]
        xts = [sb("o_xt%d" % i, [128, D]) for i in range(NS)]; b_xt = [Buf() for _ in range(NS)]
        xms = [sb("o_xm%d" % i, [128, D]) for i in range(NS)]; b_xm = [Buf() for _ in range(NS)]
        xns = [sb("o_xn%d" % i, [128, D], BF16) for i in range(NS)]; b_xn = [Buf() for _ in range(NS)]
        junks = [sb("o_j%d" % i, [128, D], BF16) for i in range(NS)]
        sss = [sb("o_s%d" % i, [128, 4]) for i in range(NS)]; b_t = [Buf() for _ in range(NS)]
        h2s = [sb("o_h2%d" % i, [128, 8, 128], BF16) for i in range(NS)]; b_h2 = [Buf() for _ in range(NS)]
        gt = [sb("o_gt%d" % i, [128, 8, 16]) for i in range(NS)]; b_gt = [Buf() for _ in range(NS)]
        _po = [[es.enter_context(nc.psum_tensor("o_po%d_%d" % (i, hb), [128, 512], F32)) for hb in range(2)] for i in range(NP)]
        _b_po = [[Buf(excl=True) for hb in range(2)] for i in range(NP)]
        _tps = [es.enter_context(nc.psum_tensor("o_tp%d" % i, [128, D], BF16)) for i in range(NP + 2)]; _b_tp = [Buf(excl=True) for _ in range(NP + 2)]
        po = [_po[i % NP] for i in range(NS)]; b_po = [_b_po[i % NP] for i in range(NS)]
        plg = [_po[i % NP][0] for i in range(NS)]; b_lg = [_b_po[i % NP][0] for i in range(NS)]
        tps = _tps; b_tp = _b_tp

        def partA(i):
            s = i % NS
            c0 = mix_col0 + i * 128
            if not mix_tm:
                fw.dma(fw.sp, mts[s][:], mix_dram[:, c0:c0 + 128].rearrange("(c p) t -> p c t", p=128), reads=[b_mix], writes=[b_mt[s]], primary=b_mt[s])
            else:
                fw.dma(fw.sp, xns[s][:], mix_dram[i * 128:(i + 1) * 128, :], reads=[b_mix], writes=[b_xn[s]], primary=b_xn[s])
                for kc in range(8):
                    fw.op(fw.pe, lambda: nc.tensor.transpose(out=tps[s][:, kc * 128:(kc + 1) * 128], in_=xns[s][:, kc * 128:(kc + 1) * 128], identity=k.ident_bf[:]),
                          reads=[b_xn[s], k.cbuf], writes=[b_tp[s]], signal=(kc == 7), partial=(kc > 0))
                fw.op(fw.dve, lambda: nc.vector.tensor_copy(out=mts[s][:].rearrange("p a b -> p (a b)"), in_=tps[s][:]), reads=[b_tp[s]], writes=[b_mt[s]])
            fw.dma(fw.sp, xts[s][:], x_row_ap(i), reads=[k.b_xend], writes=[b_xt[s]], primary=b_xt[s])
            for hb in range(2):
                for c in range(8):
                    fw.op(fw.pe, lambda: nc.tensor.matmul(po[s][hb][:, :], lhsT=mts[s][:, c, :], rhs=wo[:, c, hb * 512:(hb + 1) * 512], start=(c == 0), stop=(c == 7)),
                          reads=[b_mt[s], b_wo], writes=[b_po[s][hb]], signal=(c == 7), partial=(c > 0))
            for hb in range(2):
                sl = slice(hb * 512, (hb + 1) * 512)
                fw.op(fw.dve, lambda: nc.vector.tensor_tensor(out=xms[s][:, sl], in0=po[s][hb][:, :], in1=k.g1b[:, sl], op=ALU.mult),
                      reads=[b_po[s][hb], k.b_gb], writes=[b_xm[s]], partial=(hb > 0))
            fw.op(fw.dve, lambda: nc.vector.tensor_tensor(out=xms[s][:], in0=xms[s][:], in1=xts[s][:], op=ALU.add), reads=[b_xt[s], b_xm[s]], writes=[b_xm[s]])
            fw.dma(fw.sp, k.xmid[i * 128:(i + 1) * 128, :], xms[s][:], reads=[b_xm[s]], writes=[k.b_xmid], primary=b_xm[s], partial=True)
            norm_stats_xn(k, None, xms[s][:], b_xm[s], xns[s][:], b_xn[s], (junks[s], sss[s], b_t[s]))

        def partB(i):
            s = i % NS
            for kc in range(8):
                fw.op(fw.pe, lambda: nc.tensor.transpose(out=tps[s][:, kc * 128:(kc + 1) * 128], in_=xns[s][:, kc * 128:(kc + 1) * 128], identity=k.ident_bf[:]),
                      reads=[b_xn[s], k.cbuf], writes=[b_tp[s]], signal=(kc == 7), partial=(kc > 0))
            for kc in range(8):
                o = h2s[s][:, kc, :]
                src = tps[s][:, kc * 128:(kc + 1) * 128]
                sc_ap = k.gsT[:, layer, 1, kc, 0:1]
                bi_ap = k.modT[:, layer, 24 + kc, 0:1]
                if i % 2 == 0:
                    fw.op(fw.act, lambda: nc.scalar.activation(out=o, in_=src, func=AF.Identity, bias=bi_ap, scale=sc_ap), reads=[b_tp[s], k.cbuf], writes=[b_h2[s]], partial=(kc > 0))
                else:
                    fw.op(fw.dve, lambda: nc.vector.tensor_scalar(out=o, in0=src, scalar1=sc_ap, scalar2=bi_ap, op0=ALU.mult, op1=ALU.add), reads=[b_tp[s], k.cbuf], writes=[b_h2[s]], partial=(kc > 0))
            fw.dma(fw.sp, k.h2T[:, i * 128:(i + 1) * 128].rearrange("(c p) t -> p c t", p=128), h2s[s][:], reads=[b_h2[s]], writes=[k.b_h2T], primary=b_h2[s], partial=True)
            for kc in range(8):
                fw.op(fw.pe, lambda: nc.tensor.matmul(plg[s][:, 0:16], lhsT=h2s[s][:, kc, :], rhs=k.rw[:, kc, :], start=(kc == 0), stop=(kc == 7)),
                      reads=[b_h2[s], k.cbuf], writes=[b_lg[s]], signal=(kc == 7), partial=(kc > 0))
            G = gt[s]; bG = b_gt[s]
            sg = G[:, 0, :]; sel = G[:, 1, :]; eq = G[:, 2, :]; sel2 = G[:, 3, :]; msk = G[:, 4, :]
            m1 = G[:, 5, 0:4]; m2 = G[:, 5, 4:8]; gs = G[:, 5, 8:12]; gm = G[:, 5, 12:13]; dn = G[:, 5, 13:14]; gmask = G[:, 6, 0:4]
            v4 = lambda a: a.rearrange("p (g e) -> p g e", e=4)
            b4 = lambda a: a.unsqueeze(2).to_broadcast([128, 4, 4])
            ops = [
                (fw.act, lambda: nc.scalar.activation(out=sg, in_=plg[s][:, 0:16], func=AF.Tanh, scale=0.5), [b_lg[s]]),
                (fw.dve, lambda: nc.vector.tensor_scalar(out=sg, in0=sg, scalar1=0.5, scalar2=0.5, op0=ALU.mult, op1=ALU.add), []),
                (fw.dve, lambda: nc.vector.tensor_tensor(out=sel, in0=sg, in1=k.rb[:], op=ALU.add), [k.cbuf]),
                (fw.dve, lambda: nc.vector.tensor_reduce(out=m1, in_=v4(sel), axis=AX.X, op=ALU.max), []),
                (fw.dve, lambda: nc.vector.tensor_tensor(out=v4(eq), in0=v4(sel), in1=b4(m1), op=ALU.is_equal), []),
                (fw.dve, lambda: nc.vector.scalar_tensor_tensor(out=sel2, in0=eq, scalar=-1e9, in1=sel, op0=ALU.mult, op1=ALU.add), []),
                (fw.dve, lambda: nc.vector.tensor_reduce(out=m2, in_=v4(sel2), axis=AX.X, op=ALU.max), []),
                (fw.dve, lambda: nc.vector.tensor_tensor(out=gs, in0=m1, in1=m2, op=ALU.add), []),
                (fw.dve, lambda: nc.vector.tensor_reduce(out=gm, in_=gs, axis=AX.X, op=ALU.max), []),
                (fw.dve, lambda: nc.vector.tensor_scalar(out=gmask, in0=gs, scalar1=gm, scalar2=None, op0=ALU.is_ge), []),
                (fw.dve, lambda: nc.vector.tensor_tensor(out=v4(msk), in0=v4(sel), in1=b4(m2), op=ALU.is_ge), []),
                (fw.dve, lambda: nc.vector.tensor_tensor(out=v4(msk), in0=v4(msk), in1=b4(gmask), op=ALU.mult), []),
                (fw.dve, lambda: nc.vector.tensor_tensor(out=msk, in0=msk, in1=sg, op=ALU.mult), []),
                (fw.dve, lambda: nc.vector.tensor_reduce(out=dn, in_=msk, axis=AX.X, op=ALU.add), []),
                (fw.dve, lambda: nc.vector.reciprocal(out=dn, in_=dn), []),
            ]
            for (E, fn, rd) in ops:
                fw.op(E, fn, reads=rd + [bG], writes=[bG])
            fw.op(fw.dve, lambda: nc.vector.tensor_scalar(out=k.gates[:, i, :], in0=msk, scalar1=dn, scalar2=None, op0=ALU.mult), reads=[bG], writes=[k.b_gates], partial=True)

        for i in range(NTL):
            partA(i)
            if i > 0:
                partB(i - 1)
        partB(NTL - 1)
        fw.barrier()


def stage_moe(k, layer, final):
    nc, fw = k.nc, k.fw
    NE = k.__dict__.get("dbg_nexp", 16)
    with ExitStack() as es:
        make_bcast(k, es, layer, 40, 0, k.g2b, k.b_gb)
        sb = lambda name, shape, dt=F32: es.enter_context(nc.sbuf_tensor(name, shape, dt))
        GT = 16
        GN = GT * 128
        h2g = sb("e_h2g", [128, 8, GN], BF16); b_h2g = Buf()
        yacc = sb("e_yacc", [128, GT, D]); b_y = [Buf() for _ in range(GT)]
        w13 = [sb("e_w13_%d" % i, [128, 8, 2, 512], BF16) for i in range(2)]; b_w13 = [Buf() for _ in range(2)]
        w2 = [sb("e_w2_%d" % i, [128, 4, D], BF16) for i in range(2)]; b_w2 = [Buf() for _ in range(2)]
        s1 = [sb("e_s1_%d" % i, [128, 512], BF16) for i in range(2)]; b_s1 = [Buf() for _ in range(2)]
        actT = [sb("e_act%d" % i, [128, 4, 512], BF16) for i in range(2)]; b_act = [Buf() for _ in range(2)]
        NXT = 4
        xt = [sb("e_xt%d" % i, [128, D]) for i in range(NXT)]; b_xt = [Buf() for _ in range(NXT)]
        if final:
            fg = sb("e_fg", [128, D]); b_fg = Buf()
            fw.dma(fw.sp, fg[:], k.final_g.to_broadcast([128, D]), writes=[b_fg])
            junk = sb("e_junk", [128, D], BF16); ss = sb("e_ss", [128, 4]); b_ss = Buf()
        ph = [[es.enter_context(nc.psum_tensor("e_ph%d_%d" % (i, j), [128, 512], F32)) for j in range(2)] for i in range(2)]
        b_ph = [[Buf(excl=True) for j in range(2)] for i in range(2)]
        py = [es.enter_context(nc.psum_tensor("e_py%d" % i, [128, 512], F32)) for i in range(4)]
        b_py = [Buf(excl=True) for _ in range(4)]
        wi = 0; hi = 0; yi = 0; ai = 0
        pend = None
        yi_box = [0]

        def down_proj(e, ws, blk, a, g):
            for tt in range(4):
                tile = blk * 4 + tt
                for hb in range(2):
                    y = yi_box[0] % 4; yi_box[0] += 1
                    for fc in range(4):
                        fw.op(fw.pe, lambda: nc.tensor.matmul(py[y][:, :], lhsT=actT[a][:, fc, tt * 128:(tt + 1) * 128], rhs=w2[ws][:, fc, hb * 512:(hb + 1) * 512],
                                                              start=(fc == 0), stop=(fc == 3)),
                              reads=[b_act[a], b_w2[ws]], writes=[b_py[y]], signal=(fc == 3), partial=(fc > 0))
                    gsc = k.gates[:, g * GT + tile, e:e + 1]
                    ysl = yacc[:, tile, hb * 512:(hb + 1) * 512]
                    if e == 0:
                        fw.op(fw.dve, lambda: nc.vector.tensor_scalar(out=ysl, in0=py[y][:, :], scalar1=gsc, scalar2=None, op0=ALU.mult),
                              reads=[b_py[y], k.b_gates], writes=[b_y[tile]], partial=(hb > 0))
                    else:
                        fw.op(fw.dve, lambda: nc.vector.scalar_tensor_tensor(out=ysl, in0=py[y][:, :], scalar=gsc, in1=ysl, op0=ALU.mult, op1=ALU.add),
                              reads=[b_py[y], k.b_gates, b_y[tile]], writes=[b_y[tile]], partial=True)

        for g in range(NLAT // GN):
            g0 = g * GN
            fw.dma(fw.sp, h2g[:], k.h2T[:, g0:g0 + GN].rearrange("(c p) t -> p c t", p=128), reads=[k.b_h2T], writes=[b_h2g])
            for e in range(NE):
                ws = wi % 2; wi += 1
                for (wsrc, which) in ((k.moe_w1, 0), (k.moe_w3, 1)):
                    for half in range(2):
                        fw.dma(fw.pool, w13[ws][:, half * 4:(half + 1) * 4, which, :],
                               wsrc[layer, e, half * 512:(half + 1) * 512, :].rearrange("(c p) n -> p c n", p=128),
                               writes=[b_w13[ws]], partial=not (which == 0 and half == 0))
                fw.dma(fw.pool, w2[ws][:], k.moe_w2[layer, e].rearrange("(c p) n -> p c n", p=128), writes=[b_w2[ws]])
                for blk in range(GN // 512):
                    t0 = blk * 512
                    a = ai % 2; ai += 1
                    for fc in range(4):
                        h = hi % 2; hi += 1
                        for which in range(2):
                            for c in range(8):
                                fw.op(fw.pe, lambda: nc.tensor.matmul(ph[h][which][:, :], lhsT=w13[ws][:, c, which, fc * 128:(fc + 1) * 128], rhs=h2g[:, c, t0:t0 + 512],
                                                                      start=(c == 0), stop=(c == 7)),
                                      reads=[b_w13[ws], b_h2g], writes=[b_ph[h][which]], signal=(c == 7), partial=(c > 0))
                        fw.op(fw.act, lambda: nc.scalar.activation(out=s1[h][:], in_=ph[h][0][:], func=AF.Silu), reads=[b_ph[h][0]], writes=[b_s1[h]])
                        fw.op(fw.dve, lambda: nc.vector.tensor_tensor(out=actT[a][:, fc, :], in0=ph[h][1][:], in1=s1[h][:], op=ALU.mult),
                              reads=[b_ph[h][1], b_s1[h]], writes=[b_act[a]], partial=(fc > 0))
                    if pend is not None:
                        pend()
                    pend = (lambda e=e, ws=ws, blk=blk, a=a, g=g: down_proj(e, ws, blk, a, g))
            if pend is not None:
                pend()
                pend = None
            def xld(tile_):
                gi_ = g * GT + tile_
                fw.dma(fw.sp, xt[tile_ % NXT][:], k.xmid[gi_ * 128:(gi_ + 1) * 128, :], reads=[k.b_xmid], writes=[b_xt[tile_ % NXT]], primary=b_xt[tile_ % NXT])
            for tile in range(NXT - 1):
                xld(tile)
            for tile in range(GT):
                s = tile % NXT
                gi = g * GT + tile
                if tile + NXT - 1 < GT:
                    xld(tile + NXT - 1)
                fw.op(fw.dve, lambda: nc.vector.tensor_tensor(out=yacc[:, tile, :], in0=yacc[:, tile, :], in1=k.g2b[:], op=ALU.mult), reads=[b_y[tile], k.b_gb], writes=[b_y[tile]])
                fw.op(fw.dve, lambda: nc.vector.tensor_tensor(out=xt[s][:], in0=xt[s][:], in1=yacc[:, tile, :], op=ALU.add), reads=[b_y[tile], b_xt[s]], writes=[b_xt[s]])
                if not final:
                    fw.dma(fw.sp, k.xend[gi * 128:(gi + 1) * 128, :], xt[s][:], reads=[b_xt[s]], writes=[k.b_xend], primary=b_xt[s], partial=True)
                else:
                    fw.op(fw.act, lambda: nc.scalar.activation(out=junk[:], in_=xt[s][:], func=AF.Square, accum_out=ss[:, 0:1]), reads=[b_xt[s]], writes=[b_ss])
                    fw.op(fw.dve, lambda: nc.vector.tensor_scalar(out=ss[:, 1:2], in0=ss[:, 0:1], scalar1=1.0 / D, scalar2=EPS, op0=ALU.mult, op1=ALU.add), reads=[b_ss], writes=[b_ss])
                    fw.op(fw.pool, lambda: nc.gpsimd.tensor_tensor(out=ss[:, 2:3], in0=ss[:, 1:2], in1=k.nexp[:, 0:1], op=ALU.pow), reads=[b_ss, k.cbuf], writes=[b_ss])
                    fw.op(fw.dve, lambda: nc.vector.scalar_tensor_tensor(out=xt[s][:], in0=xt[s][:], scalar=ss[:, 2:3], in1=fg[:], op0=ALU.mult, op1=ALU.mult),
                          reads=[b_xt[s], b_ss, b_fg], writes=[b_xt[s]])
                    fw.dma(fw.sp, k.out[gi * 128:(gi + 1) * 128, :], xt[s][:], reads=[b_xt[s]], primary=b_xt[s])
        fw.barrier()


import ml_dtypes

NFFT = 8192
TWO_PI = 2.0 * math.pi
MAGIC = 12582912.0


def host_prep5(inputs, b, m):
    f = lambda a: np.ascontiguousarray(a, dtype=np.float32)
    bf = lambda a: np.ascontiguousarray(np.asarray(a, dtype=np.float32).astype(ml_dtypes.bfloat16))
    m["od_w_in"] = f(inputs["od_w_in"][0])
    m["hy_cw"] = f(inputs["hy_conv_w"][0].reshape(3, 24, 128).transpose(2, 1, 0))
    m["hy_cb"] = f(inputs["hy_conv_b"][0].reshape(24, 128).T)
    n = NLAT
    t = np.linspace(0.0, 1.0, n, dtype=np.float32)[:, None]
    w = (np.float32(2.0 * math.pi / n) * np.arange(n, dtype=np.float32))[:, None]
    fr = np.linspace(1e-4, 15, 16, dtype=np.float32)[None, :]
    z = np.concatenate([t, np.cos(fr * w), -np.sin(fr * w)], axis=-1).astype(np.float32)
    m["hy_zT"] = f(z.T)
    m["hy_tcol"] = f(np.concatenate([t[:, 0].reshape(32, 128).T, t[:, 0].reshape(32, 128)[:, ::-1].T], axis=1))
    m["hy_w1"] = f(inputs["hy_w1"][0])
    m["hy_w2"] = f(inputs["hy_w2"][0])
    m["hy_w3"] = f(inputs["hy_w3"][0])
    m["hy_vec"] = f(np.stack([inputs["hy_b1"][0], inputs["hy_freq1"][0], inputs["hy_b2"][0], inputs["hy_freq2"][0]], axis=1))
    m["hy_decay"] = f(inputs["hy_decay"][0].reshape(1, 4096))
    m["hy_skip"] = f(inputs["hy_skip"][0])
    a = np.arange(64)[:, None].astype(np.float64); ka = np.arange(64)[None, :].astype(np.float64)
    th = 2 * np.pi * a * (ka + 0.5) / 64.0
    F1 = np.concatenate([np.cos(th), -np.sin(th)], axis=1)
    m["F1"] = bf(F1)
    F1i = np.concatenate([np.cos(th).T, -np.sin(th).T], axis=0) * (2.0 / NFFT)
    m["F1i"] = bf(F1i[:, :32])
    bb = np.arange(128)[None, :, None].astype(np.float64); kb = np.arange(64)[None, None, :].astype(np.float64)
    kav = np.arange(64)[:, None, None].astype(np.float64)
    ph = 2 * np.pi * bb * (kav + 64.0 * kb + 0.5) / NFFT
    Gr = np.cos(ph); Gi = -np.sin(ph)
    pairs = [(Gr, Gi), (-Gi, Gr), (Gi, Gr), (Gr, -Gi), (Gr, Gr), (-Gi, -Gi), (-Gi, Gi), (-Gr, Gr)]
    GT = np.concatenate([np.concatenate(p, axis=2) for p in pairs], axis=2)
    m["GT"] = bf(GT)
    GrT = Gr.transpose(0, 2, 1); GiT = Gi.transpose(0, 2, 1)
    Vre = np.concatenate([GrT, GiT], axis=1)
    Vim = np.concatenate([-GiT, GrT], axis=1)
    m["GiT"] = bf(np.concatenate([Vre, Vim], axis=2))
    return m


def declare5(k):
    nc = k.nc
    din = lambda name, shape, dt=F32: nc.dram_tensor(name, list(shape), dt, kind="ExternalInput").ap()
    dscr = lambda name, shape, dt: nc.dram_tensor(name, list(shape), dt, kind="Internal").ap()
    k.od_w_in = din("od_w_in", [D, 3072]); k.hy_cw = din("hy_cw", [128, 24, 3]); k.hy_cb = din("hy_cb", [128, 24])
    k.hy_zT = din("hy_zT", [33, NLAT]); k.hy_tcol = din("hy_tcol", [128, 64])
    k.hy_w1 = din("hy_w1", [33, 64]); k.hy_w2 = din("hy_w2", [64, 64]); k.hy_w3 = din("hy_w3", [64, 4096])
    k.hy_vec = din("hy_vec", [64, 4]); k.hy_decay = din("hy_decay", [1, 4096]); k.hy_skip = din("hy_skip", [2, 1024])
    k.F1 = din("F1", [64, 128], BF16); k.F1i = din("F1i", [128, 32], BF16)
    k.GT = din("GT", [64, 128, 1024], BF16); k.GiT = din("GiT", [64, 128, 256], BF16)
    k.pv = [dscr("hy_p%d" % i, [NLAT, D], BF16) for i in range(3)]; k.b_pv = [Buf() for _ in range(3)]
    k.Tk = [dscr("hy_Tk%d" % o, [NFFT + 1, D], BF16) for o in range(2)]; k.b_Tk = [Buf() for _ in range(2)]
    k.Yd = dscr("hy_Yd", [128, 128, D], BF16); k.b_Yd = Buf()
    k.Vd = dscr("hy_Vd", [128, 128, D], BF16); k.b_Vd = Buf()
    k.Hs = [[dscr("hy_Hs%d_%d" % (o, i), [128, 64, D], BF16) for i in range(2)] for o in range(2)]; k.b_Hs = [Buf() for _ in range(2)]
    k.zd = [dscr("hy_z%d" % i, [NLAT, D], BF16) for i in range(2)]; k.b_zd = [Buf() for _ in range(2)]
    k.b_ns = Buf("nrmskip")


def alloc_hy_rows(k, es):
    nc = k.nc
    k.nrm = [es.enter_context(nc.sbuf_tensor("hy_nrm%d" % o, [128, D], F32)) for o in range(2)]
    k.skp = [es.enter_context(nc.sbuf_tensor("hy_skp%d" % o, [128, D], F32)) for o in range(2)]


def stage_hy_inproj(k):
    nc, fw = k.nc, k.fw
    with ExitStack() as es:
        sb = lambda name, shape, dt=F32: es.enter_context(nc.sbuf_tensor(name, shape, dt))
        hT = sb("hy_hT", [128, 8, NLAT], BF16); b_hT = [Buf() for _ in range(NTL)]
        with ExitStack() as es2:
            norm_transpose_pass(k, es2, 1, 0, NTL, lambda i: k.xend[i * 128:(i + 1) * 128, :], lambda i: 0, hT, b_hT, k.b_xend)
            fw.barrier()
        cw = sb("hy_cw_s", [128, 24, 3]); cb = sb("hy_cb_s", [128, 24]); b_c = Buf()
        fw.dma(fw.sp, cw[:], k.hy_cw, writes=[b_c], partial=True)
        fw.dma(fw.sp, cb[:], k.hy_cb, writes=[b_c], partial=True)
        wch = [sb("hy_wch%d" % i, [128, 8, 128], BF16) for i in range(2)]; b_wch = [Buf() for _ in range(2)]
        pb = [sb("hy_pb%d" % i, [128, NLAT + 2]) for i in range(2)]; b_pb = [Buf() for _ in range(2)]
        ub = [sb("hy_ub%d" % i, [128, NLAT], BF16) for i in range(2)]; b_ub = [Buf() for _ in range(2)]
        acc = [sb("hy_acc%d" % i, [128, NLAT]) for i in range(1)]; b_acc = [Buf()]
        ot = [sb("hy_ot%d" % i, [128, 8, 128], BF16) for i in range(2)]; b_ot = [Buf() for _ in range(2)]
        pss = [es.enter_context(nc.psum_tensor("hy_ps%d" % i, [128, 512], F32)) for i in range(3)]; b_ps = [Buf(excl=True) for _ in range(3)]
        tps = [es.enter_context(nc.psum_tensor("hy_tp%d" % i, [128, D], BF16)) for i in range(2)]; b_tp = [Buf(excl=True) for _ in range(2)]
        for s in range(2):
            fw.op(fw.pool, lambda: nc.gpsimd.memset(pb[s][:], 0.0), writes=[b_pb[s]])
        pi = 0; oi_box = [0]
        pend = None

        def store_chunk(ch, s):
            which = ch // 8; cc = ch % 8
            for grp in range(4):
                tp = (ch * 4 + grp) % 2
                o = oi_box[0] % 2; oi_box[0] += 1
                for tt in range(8):
                    ti = grp * 8 + tt
                    fw.op(fw.pe, lambda: nc.tensor.transpose(out=tps[tp][:, tt * 128:(tt + 1) * 128], in_=ub[s][:, ti * 128:(ti + 1) * 128], identity=k.ident_bf[:]),
                          reads=[b_ub[s], k.cbuf], writes=[b_tp[tp]], signal=(tt == 7), partial=(tt > 0))
                if grp % 2 == 0:
                    fw.op(fw.act, lambda: nc.scalar.copy(out=ot[o][:].rearrange("p a b -> p (a b)"), in_=tps[tp][:]), reads=[b_tp[tp]], writes=[b_ot[o]])
                else:
                    fw.op(fw.dve, lambda: nc.vector.tensor_copy(out=ot[o][:].rearrange("p a b -> p (a b)"), in_=tps[tp][:]), reads=[b_tp[tp]], writes=[b_ot[o]])
                dst = k.pv[which][grp * 1024:(grp + 1) * 1024, cc * 128:(cc + 1) * 128].rearrange("(t p) c -> p t c", p=128)
                fw.dma(fw.sp, dst, ot[o][:], reads=[b_ot[o]], writes=[k.b_pv[which]], primary=b_ot[o], partial=True)

        for ch in range(24):
            s = ch % 2
            fw.dma(fw.pool, wch[s][:], k.od_w_in[:, ch * 128:(ch + 1) * 128].rearrange("(c p) n -> p c n", p=128), writes=[b_wch[s]])
            for blk in range(8):
                p = pi % 3; pi += 1
                t0 = blk * 512
                for kc in range(8):
                    fw.op(fw.pe, lambda: nc.tensor.matmul(pss[p][:, :], lhsT=wch[s][:, kc, :], rhs=hT[:, kc, t0:t0 + 512], start=(kc == 0), stop=(kc == 7)),
                          reads=[b_wch[s]] + b_hT[blk * 4:blk * 4 + 4], writes=[b_ps[p]], signal=(kc == 7), partial=(kc > 0))
                fw.op(fw.act, lambda: nc.scalar.copy(out=pb[s][:, 1 + t0:1 + t0 + 512], in_=pss[p][:, :]), reads=[b_ps[p]], writes=[b_pb[s]], partial=True)
            fw.op(fw.act, lambda: nc.scalar.activation(out=acc[0][:], in_=pb[s][:, 0:NLAT], func=AF.Identity, bias=cb[:, ch:ch + 1], scale=cw[:, ch, 0:1]),
                  reads=[b_pb[s], b_c], writes=[b_acc[0]])
            fw.op(fw.dve, lambda: nc.vector.scalar_tensor_tensor(out=acc[0][:], in0=pb[s][:, 1:NLAT + 1], scalar=cw[:, ch, 1:2], in1=acc[0][:], op0=ALU.mult, op1=ALU.add),
                  reads=[b_pb[s], b_c, b_acc[0]], writes=[b_acc[0]])
            fw.op(fw.dve, lambda: nc.vector.scalar_tensor_tensor(out=ub[s][:], in0=pb[s][:, 2:NLAT + 2], scalar=cw[:, ch, 2:3], in1=acc[0][:], op0=ALU.mult, op1=ALU.add),
                  reads=[b_pb[s], b_c, b_acc[0]], writes=[b_ub[s]])
            if pend is not None:
                pend()
            pend = (lambda ch=ch, s=s: store_chunk(ch, s))
        pend()
        fw.barrier()


def sin_reduced(k, dst, src_ps, npart, n, f_ap, fb_ap, tmps, b_tmps, b_src, b_dst):
    nc, fw = k.nc, k.fw
    arg, kk = tmps
    b_arg, b_kk = b_tmps
    fw.op(fw.dve, lambda: nc.vector.tensor_scalar(out=arg[:npart, :n], in0=src_ps, scalar1=f_ap, scalar2=fb_ap, op0=ALU.mult, op1=ALU.add), reads=[b_src, k.b_hyp], writes=[b_arg])
    fw.op(fw.dve, lambda: nc.vector.tensor_scalar(out=kk[:npart, :n], in0=arg[:npart, :n], scalar1=1.0 / TWO_PI, scalar2=MAGIC, op0=ALU.mult, op1=ALU.add), reads=[b_arg], writes=[b_kk])
    fw.op(fw.dve, lambda: nc.vector.tensor_scalar(out=kk[:npart, :n], in0=kk[:npart, :n], scalar1=-MAGIC, scalar2=None, op0=ALU.add), reads=[b_kk], writes=[b_kk])
    fw.op(fw.dve, lambda: nc.vector.scalar_tensor_tensor(out=arg[:npart, :n], in0=kk[:npart, :n], scalar=-TWO_PI, in1=arg[:npart, :n], op0=ALU.mult, op1=ALU.add), reads=[b_kk, b_arg], writes=[b_arg])
    fw.op(fw.dve, lambda: nc.vector.tensor_scalar(out=arg[:npart, :n], in0=arg[:npart, :n], scalar1=math.pi, scalar2=-math.pi, op0=ALU.min, op1=ALU.max), reads=[b_arg], writes=[b_arg])
    fw.op(fw.act, lambda: nc.scalar.activation(out=dst, in_=arg[:npart, :n], func=AF.Sin), reads=[b_arg], writes=[b_dst], partial=True)


def stage_hy_filters(k):
    nc, fw = k.nc, k.fw
    with ExitStack() as es:
        sb = lambda name, shape, dt=F32: es.enter_context(nc.sbuf_tensor(name, shape, dt))
        zT = sb("f_zT", [33, NLAT]); w1 = sb("f_w1", [33, 64]); w2 = sb("f_w2", [64, 64]); w3 = sb("f_w3", [64, 4096])
        vec = sb("f_vec", [64, 6]); tcol = sb("f_tcol", [128, 64]); ntcol = sb("f_ntcol", [128, 64])
        dec = sb("f_dec", [128, 4096])
        k.b_hyp = Buf("hyp")
        for dst, src in ((zT[:], k.hy_zT), (w1[:], k.hy_w1), (w2[:], k.hy_w2), (w3[:], k.hy_w3), (vec[:, 0:4], k.hy_vec), (tcol[:], k.hy_tcol)):
            fw.dma(fw.sp, dst, src, writes=[k.b_hyp], partial=True)
        fw.dma(fw.sp, dec[:], k.hy_decay.to_broadcast([128, 4096]), writes=[k.b_hyp], partial=True)
        for o in range(2):
            fw.dma(fw.sp, k.skp[o][:], k.hy_skip[o:o + 1, :].to_broadcast([128, D]), writes=[k.b_ns], partial=True)
        fw.op(fw.dve, lambda: nc.vector.tensor_tensor(out=vec[:, 4:5], in0=vec[:, 0:1], in1=vec[:, 1:2], op=ALU.mult), reads=[k.b_hyp], writes=[k.b_hyp])
        fw.op(fw.dve, lambda: nc.vector.tensor_tensor(out=vec[:, 5:6], in0=vec[:, 2:3], in1=vec[:, 3:4], op=ALU.mult), reads=[k.b_hyp], writes=[k.b_hyp])
        fw.op(fw.dve, lambda: nc.vector.tensor_scalar(out=ntcol[:], in0=tcol[:], scalar1=-1.0, scalar2=None, op0=ALU.mult), reads=[k.b_hyp], writes=[k.b_hyp])
        fw.op(fw.act, lambda: nc.scalar.activation(out=dec[:], in_=dec[:], func=AF.Abs), reads=[k.b_hyp], writes=[k.b_hyp])
        a1 = sb("f_a1", [64, NLAT]); a2 = sb("f_a2", [64, NLAT]); b_a1 = Buf(); b_a2 = Buf()
        tm = [[sb("f_tm%d_%d" % (i, j), [128, 512]) for j in range(2)] for i in range(2)]; b_tm = [[Buf() for j in range(2)] for i in range(2)]
        pss = [es.enter_context(nc.psum_tensor("f_ps%d" % i, [128, 512], F32)) for i in range(3)]; b_ps = [Buf(excl=True) for _ in range(3)]
        pacc = es.enter_context(nc.psum_tensor("f_pacc", [128, 512], F32)); b_pacc = Buf(excl=True)
        pi = 0
        for blk in range(8):
            p = pi % 3; pi += 1; s = blk % 2
            t0 = blk * 512
            fw.op(fw.pe, lambda: nc.tensor.matmul(pss[p][:64, :], lhsT=w1[:, :], rhs=zT[:, t0:t0 + 512], start=True, stop=True), reads=[k.b_hyp], writes=[b_ps[p]])
            sin_reduced(k, a1[:, t0:t0 + 512], pss[p][:64, :], 64, 512, vec[:, 1:2], vec[:, 4:5], tm[s], b_tm[s], b_ps[p], b_a1)
        for blk in range(8):
            p = pi % 3; pi += 1; s = blk % 2
            t0 = blk * 512
            fw.op(fw.pe, lambda: nc.tensor.matmul(pss[p][:64, :], lhsT=w2[:, :], rhs=a1[:, t0:t0 + 512], start=True, stop=True), reads=[k.b_hyp, b_a1], writes=[b_ps[p]])
            sin_reduced(k, a2[:, t0:t0 + 512], pss[p][:64, :], 64, 512, vec[:, 3:4], vec[:, 5:6], tm[s], b_tm[s], b_ps[p], b_a2)
        a2r = sb("f_a2r", [64, NLAT], BF16); a2b = sb("f_a2b", [64, NLAT], BF16); w3b = sb("f_w3b", [64, 4096], BF16)
        fw.op(fw.dve, lambda: nc.vector.tensor_copy(out=a2r[:], in_=a2[:, ::-1]), reads=[b_a2], writes=[b_a2], partial=True)
        fw.op(fw.dve, lambda: nc.vector.tensor_copy(out=a2b[:], in_=a2[:]), reads=[b_a2], writes=[b_a2], partial=True)
        fw.op(fw.act, lambda: nc.scalar.copy(out=w3b[:], in_=w3[:]), reads=[k.b_hyp], writes=[k.b_hyp], partial=True)
        E = [sb("f_E%d" % i, [128, 512]) for i in range(2)]; b_E = [Buf() for _ in range(2)]
        hh = [sb("f_hh%d" % i, [128, 512]) for i in range(2)]; b_hh = [Buf() for _ in range(2)]
        hab = [sb("f_hab%d" % i, [128, 512], BF16) for i in range(2)]; b_hab = [Buf() for _ in range(2)]
        hb = [sb("f_hb%d" % i, [128, 512], BF16) for i in range(3)]; b_hb = [Buf() for _ in range(3)]
        row0 = sb("f_row0", [1, 512]); b_row0 = Buf()
        zrow = sb("f_zrow", [1, D], BF16); b_zrow = Buf()
        fw.op(fw.dve, lambda: nc.vector.memset(zrow[:], 0.0), writes=[b_zrow])
        it = 0; hbi = 0
        for o in range(2):
            fw.dma(fw.sp, k.Tk[o][4096:4097, :], zrow[:], reads=[b_zrow], writes=[k.b_Tk[o]], primary=b_zrow, partial=True)
            for half in range(2):
                for side in (1, 0):
                    col0 = (o * 2 + side) * 1024 + half * 512
                    for ti in range(32):
                        p = pi % 3; pi += 1
                        s = it % 2; it += 1
                        hs = hbi % 3; hbi += 1
                        lt = a2b[:, ti * 128:(ti + 1) * 128]
                        if side == 1:
                            lt = a2r[:, (31 - ti) * 128:(32 - ti) * 128]
                        tc_i = ti + (32 if side == 1 else 0)
                        fw.op(fw.pe, lambda: nc.tensor.matmul(pss[p][:, :], lhsT=lt, rhs=w3b[:, col0:col0 + 512], start=True, stop=True),
                              reads=[b_a2, k.b_hyp], writes=[b_ps[p]])
                        fw.op(fw.act, lambda: nc.scalar.activation(out=E[s][:], in_=dec[:, col0:col0 + 512], func=AF.Exp, scale=ntcol[:, tc_i:tc_i + 1]), reads=[k.b_hyp], writes=[b_E[s]])
                        fw.op(fw.dve, lambda: nc.vector.tensor_tensor(out=hh[s][:], in0=pss[p][:, :], in1=E[s][:], op=ALU.mult), reads=[b_ps[p], b_E[s]], writes=[b_hh[s]])
                        fw.op(fw.act, lambda: nc.scalar.activation(out=hab[s][:], in_=hh[s][:], func=AF.Abs), reads=[b_hh[s]], writes=[b_hab[s]])
                        first = (side == 1 and ti == 0); last = (side == 0 and ti == 31)
                        fw.op(fw.pe, lambda: nc.tensor.matmul(pacc[:, :], lhsT=k.ones_bf[:], rhs=hab[s][:], start=first, stop=last),
                              reads=[b_hab[s], k.cbuf], writes=[b_pacc], signal=last, partial=not first)
                        if side == 1:
                            if ti == 0:
                                fw.dma(fw.sp, row0[:], hh[s][127:128, :], reads=[b_hh[s]], writes=[b_row0], primary=b_row0)
                            fw.op(fw.dve, lambda: nc.vector.tensor_scalar(out=hb[hs][:], in0=hh[s][:], scalar1=-1.0, scalar2=None, op0=ALU.mult), reads=[b_hh[s]], writes=[b_hb[hs]])
                            r_lo = NFFT - 127 - ti * 128
                            dst = k.Tk[o][r_lo:r_lo + 128, half * 512:(half + 1) * 512]
                            fw.dma(fw.sp, dst, hb[hs][:], reads=[b_hb[hs]], writes=[k.b_Tk[o]], primary=b_hb[hs], partial=True)
                        else:
                            if ti == 0:
                                fw.op(fw.dve, lambda: nc.vector.tensor_tensor(out=hh[s][0:1, :], in0=hh[s][0:1, :], in1=row0[:], op=ALU.add), reads=[b_hh[s], b_row0], writes=[b_hh[s]])
                            fw.op(fw.dve, lambda: nc.vector.tensor_copy(out=hb[hs][:], in_=hh[s][:]), reads=[b_hh[s]], writes=[b_hb[hs]])
                            fw.dma(fw.sp, k.Tk[o][ti * 128:(ti + 1) * 128, half * 512:(half + 1) * 512], hb[hs][:], reads=[b_hb[hs]], writes=[k.b_Tk[o]], primary=b_hb[hs], partial=True)
                sl = slice(half * 512, (half + 1) * 512)
                fw.op(fw.dve, lambda: nc.vector.tensor_scalar(out=k.nrm[o][:, sl], in0=pacc[:, :], scalar1=1e-6, scalar2=None, op0=ALU.add), reads=[b_pacc], writes=[k.b_ns], partial=True)
                fw.op(fw.dve, lambda: nc.vector.reciprocal(out=k.nrm[o][:, sl], in_=k.nrm[o][:, sl]), reads=[k.b_ns], writes=[k.b_ns], partial=True)
        fw.barrier()


class _NullCtx:
    def __init__(self, es):
        self.es = es

    def __enter__(self):
        return self.es

    def __exit__(self, *a):
        return False


def fft_forward(k, src, b_src, a_in, pairs, post, ext_es=None):
    nc, fw = k.nc, k.fw
    with (ExitStack() if ext_es is None else _NullCtx(ext_es)) as es:
        sb = lambda name, shape, dt=F32: es.enter_context(nc.sbuf_tensor(name, shape, dt))
        F1 = sb("ff_F1", [64, 128], BF16); b_F1 = Buf()
        fw.dma(fw.sp, F1[:], k.F1, writes=[b_F1])
        NX = 4
        xin = [sb("ff_x%d" % i, [64, 4096], BF16) for i in range(NX)]; b_x = [Buf() for _ in range(NX)]
        yo = [sb("ff_y%d" % i, [128, 512], BF16) for i in range(6)]; b_y = [Buf() for _ in range(6)]
        pss = [es.enter_context(nc.psum_tensor("ff_ps%d" % i, [128, 512], F32)) for i in range(2)]; b_ps = [Buf(excl=True) for _ in range(2)]
        srcv = src[0:a_in * 128, :].rearrange("(a b) c -> a (b c)", b=128)
        yv = k.Yd.rearrange("r b c -> r (b c)")
        pi = 0

        def xld(ch):
            fw.dma(fw.act, xin[ch % NX][:a_in, :], srcv[:, ch * 4096:(ch + 1) * 4096], reads=[b_src], writes=[b_x[ch % NX]], primary=b_x[ch % NX])
        for ch in range(NX - 1):
            xld(ch)
        for ch in range(32):
            s = ch % NX
            if ch + NX - 1 < 32:
                xld(ch + NX - 1)
            for j in range(8):
                p = pi % 2; y6 = pi % 6; pi += 1
                fw.op(fw.pe, lambda: nc.tensor.matmul(pss[p][:, :], lhsT=F1[:a_in, :], rhs=xin[s][:a_in, j * 512:(j + 1) * 512], start=True, stop=True),
                      reads=[b_F1, b_x[s]], writes=[b_ps[p]])
                if p % 2 == 0:
                    fw.op(fw.act, lambda: nc.scalar.copy(out=yo[y6][:], in_=pss[p][:]), reads=[b_ps[p]], writes=[b_y[y6]])
                else:
                    fw.op(fw.dve, lambda: nc.vector.tensor_copy(out=yo[y6][:], in_=pss[p][:]), reads=[b_ps[p]], writes=[b_y[y6]])
                c0 = ch * 4096 + j * 512
                fw.dma(fw.sp, yv[:, c0:c0 + 512], yo[y6][:], reads=[b_y[y6]], writes=[k.b_Yd], primary=b_y[y6], partial=True)
        NR = 4
        G = [sb("ff_G%d" % i, [128, 512], BF16) for i in range(NR)]; b_G = [Buf() for _ in range(NR)]
        gp0 = min(min(p) for p in pairs)
        assert gp0 % 4 == 0 and max(max(p) for p in pairs) < gp0 + 4
        Y = [sb("ff_Y%d" % i, [128, 2, D], BF16) for i in range(NR)]; b_Y = [Buf() for _ in range(NR)]
        npair = len(pairs)
        ps = [[es.enter_context(nc.psum_tensor("ff_q%d_%d" % (i, j), [128, 512], F32)) for j in range(npair)] for i in range(2)]
        b_q = [[Buf(excl=True) for j in range(npair)] for i in range(2)]
        Ydv = k.Yd.rearrange("(ri ka) b c -> ka b ri c", ri=2)
        qi = 0
        def ld(ka):
            s = ka % NR
            fw.dma(fw.act, G[s][:], k.GT[ka][:, gp0 * 128:(gp0 + 4) * 128], writes=[b_G[s]])
            fw.dma(fw.sp, Y[s][:], Ydv[ka], reads=[k.b_Yd], writes=[b_Y[s]], primary=b_Y[s])
        for ka in range(NR - 1):
            ld(ka)
        for ka in range(64):
            s = ka % NR
            if ka + NR - 1 < 64:
                ld(ka + NR - 1)
            for hb in range(2):
                q = qi % 2; qi += 1
                for j, (pr_re, pr_im) in enumerate(pairs):
                    fw.op(fw.pe, lambda: nc.tensor.matmul(ps[q][j][:, :], lhsT=G[s][:, (pr_re - gp0) * 128:(pr_re - gp0 + 1) * 128], rhs=Y[s][:, 0, hb * 512:(hb + 1) * 512], start=True, stop=False),
                          reads=[b_G[s], b_Y[s]], writes=[b_q[q][j]], signal=False)
                    fw.op(fw.pe, lambda: nc.tensor.matmul(ps[q][j][:, :], lhsT=G[s][:, (pr_im - gp0) * 128:(pr_im - gp0 + 1) * 128], rhs=Y[s][:, 1, hb * 512:(hb + 1) * 512], start=False, stop=True),
                          reads=[b_G[s], b_Y[s]], writes=[b_q[q][j]], partial=True)
                post(ka, hb, ps[q], b_q[q])
        if ext_es is None:
            fw.barrier()


def stage_hy_filter_spectra(k, o):
    nc, fw = k.nc, k.fw
    with ExitStack() as es0:
        ho = [[es0.enter_context(nc.sbuf_tensor("fs_h%d_%d" % (i, j), [128, 512], BF16)) for j in range(2)] for i in range(2)]
        b_ho = [[Buf() for j in range(2)] for i in range(2)]
        tmpf = [es0.enter_context(nc.sbuf_tensor("fs_t%d" % i, [128, 512], F32)) for i in range(2)]; b_tf = [Buf() for _ in range(2)]
        cnt = [0]

        def post(ka, hb, ps, b_ps):
            s = cnt[0] % 2; cnt[0] += 1
            sl = slice(hb * 512, (hb + 1) * 512)
            fw.op(fw.dve, lambda: nc.vector.tensor_tensor(out=tmpf[s][:], in0=ps[0][:, :], in1=k.nrm[o][:, sl], op=ALU.mult), reads=[b_ps[0], k.b_ns], writes=[b_tf[s]])
            fw.op(fw.dve, lambda: nc.vector.tensor_tensor(out=ho[s][0][:], in0=tmpf[s][:], in1=k.skp[o][:, sl], op=ALU.add), reads=[b_tf[s], k.b_ns], writes=[b_ho[s][0]])
            fw.op(fw.dve, lambda: nc.vector.tensor_tensor(out=ho[s][1][:], in0=ps[1][:, :], in1=k.nrm[o][:, sl], op=ALU.mult), reads=[b_ps[1], k.b_ns], writes=[b_ho[s][1]])
            for j in range(2):
                fw.dma(fw.sp, k.Hs[o][j][:, ka, sl], ho[s][j][:], reads=[b_ho[s][j]], writes=[k.b_Hs[o]], primary=b_ho[s][j], partial=True)

        fft_forward(k, k.Tk[o], k.b_Tk[o], 64, [(4, 5), (6, 7)], post)


def stage_hy_conv(k, o, src, b_src, gate, b_gate, dst, b_dst):
    nc, fw = k.nc, k.fw
    with ExitStack() as es0:
        sb0 = lambda name, shape, dt=F32: es0.enter_context(nc.sbuf_tensor(name, shape, dt))
        Hb = [[sb0("cv_H%d_%d" % (i, j), [128, 512], BF16) for j in range(2)] for i in range(4)]; b_H = [Buf() for _ in range(4)]
        t1 = [sb0("cv_t1_%d" % i, [128, 512]) for i in range(2)]; b_t1 = [Buf() for _ in range(2)]
        t2 = [sb0("cv_t2_%d" % i, [128, 512]) for i in range(2)]; b_t2 = [Buf() for _ in range(2)]
        Pb = [sb0("cv_P%d" % i, [128, D], BF16) for i in range(2)]; b_P = [Buf() for _ in range(2)]
        Gi = [sb0("cv_Gi%d" % i, [128, 256], BF16) for i in range(2)]; b_Gi = [Buf() for _ in range(2)]
        vo = [sb0("cv_vo%d" % i, [128, 512], BF16) for i in range(4)]; b_vo = [Buf() for _ in range(4)]
        pv = [es0.enter_context(nc.psum_tensor("cv_pv%d" % i, [128, 512], F32)) for i in range(2)]; b_pv = [Buf(excl=True) for _ in range(2)]
        cnt = [0]; vi = [0]

        def post(ka, hb, ps, b_ps):
            n = cnt[0]
            hsl = n % 4
            s = n % 2; cnt[0] += 1
            sl = slice(hb * 512, (hb + 1) * 512)
            kas = ka % 2

            def hload(nn):
                ka2, hb2 = nn // 2, nn % 2
                for j in range(2):
                    fw.dma(fw.act, Hb[nn % 4][j][:], k.Hs[o][j][:, ka2, hb2 * 512:(hb2 + 1) * 512], reads=[k.b_Hs[o]], writes=[b_H[nn % 4]], primary=b_H[nn % 4], partial=(j > 0))
            if n == 0:
                for nn in range(3):
                    hload(nn)
            if n + 3 < 128:
                hload(n + 3)
            fw.op(fw.dve, lambda: nc.vector.tensor_tensor(out=t1[s][:], in0=ps[0][:, :], in1=Hb[hsl][0][:], op=ALU.mult), reads=[b_ps[0], b_H[hsl]], writes=[b_t1[s]])
            fw.op(fw.dve, lambda: nc.vector.tensor_tensor(out=t2[s][:], in0=ps[1][:, :], in1=Hb[hsl][1][:], op=ALU.mult), reads=[b_ps[1], b_H[hsl]], writes=[b_t2[s]])
            fw.op(fw.dve, lambda: nc.vector.tensor_tensor(out=Pb[kas][:, sl], in0=t1[s][:], in1=t2[s][:], op=ALU.add), reads=[b_t1[s], b_t2[s]], writes=[b_P[kas]], partial=(hb > 0))
            if hb == 0:
                fw.dma(fw.sp, Gi[kas][:], k.GiT[ka], writes=[b_Gi[kas]])
            for ri in range(2):
                v4 = vi[0] % 4; v = vi[0] % 2; vi[0] += 1
                fw.op(fw.pe, lambda: nc.tensor.matmul(pv[v][:, :], lhsT=Gi[kas][:, ri * 128:(ri + 1) * 128], rhs=Pb[kas][:, sl], start=True, stop=True),
                      reads=[b_Gi[kas], b_P[kas]], writes=[b_pv[v]])
                fw.op(fw.act, lambda: nc.scalar.copy(out=vo[v4][:], in_=pv[v][:]), reads=[b_pv[v]], writes=[b_vo[v4]])
                fw.dma(fw.sp, k.Vd[ri * 64 + ka, :, sl], vo[v4][:], reads=[b_vo[v4]], writes=[k.b_Vd], primary=b_vo[v4], partial=True)

        fft_forward(k, src, b_src, 32, [(0, 1), (2, 3)], post, ext_es=es0)
        es = es0
        sb = sb0
        F1i = sb("ci_F1i", [128, 32], BF16); b_F = Buf()
        fw.dma(fw.sp, F1i[:], k.F1i, writes=[b_F])
        NV = 4
        vin = [sb("ci_v%d" % i, [128, 4096], BF16) for i in range(NV)]; b_v = [Buf() for _ in range(NV)]
        gin = [sb("ci_g%d" % i, [32, 4096], BF16) for i in range(NV)]; b_g = [Buf() for _ in range(NV)]
        zo = [sb("ci_z%d" % i, [32, 4096], BF16) for i in range(2)]; b_z = [Buf() for _ in range(2)]
        pss = pv; b_ps = b_pv
        vv = k.Vd.rearrange("r b c -> r (b c)")
        gv = gate.rearrange("(a b) c -> a (b c)", b=128)
        dv = dst.rearrange("(a b) c -> a (b c)", b=128)
        pi = 0
        def vld(ch):
            s_ = ch % NV
            fw.dma(fw.sp, vin[s_][:], vv[:, ch * 4096:(ch + 1) * 4096], reads=[k.b_Vd], writes=[b_v[s_]], primary=b_v[s_])
            fw.dma(fw.act, gin[s_][:], gv[:, ch * 4096:(ch + 1) * 4096], reads=[b_gate], writes=[b_g[s_]], primary=b_g[s_])
        for ch in range(NV - 1):
            vld(ch)
        for ch in range(32):
            s = ch % NV; z = ch % 2
            if ch + NV - 1 < 32:
                vld(ch + NV - 1)
            for j in range(8):
                p = pi % 2; pi += 1
                fw.op(fw.pe, lambda: nc.tensor.matmul(pss[p][:32, :], lhsT=F1i[:, :], rhs=vin[s][:, j * 512:(j + 1) * 512], start=True, stop=True),
                      reads=[b_F, b_v[s]], writes=[b_ps[p]])
                fw.op(fw.dve, lambda: nc.vector.tensor_tensor(out=zo[z][:, j * 512:(j + 1) * 512], in0=pss[p][:32, :], in1=gin[s][:, j * 512:(j + 1) * 512], op=ALU.mult),
                      reads=[b_ps[p], b_g[s]], writes=[b_z[z]], partial=(j > 0))
            fw.dma(fw.sp, dv[:, ch * 4096:(ch + 1) * 4096], zo[z][:], reads=[b_z[z]], writes=[b_dst], primary=b_z[z], partial=True)
        fw.barrier()


def prep_shared(inputs):
    m = {}
    host_prep_shared1(inputs, m)
    host_prep2(inputs, 0, m); host_prep3(inputs, 0, m); host_prep4(inputs, 0, m); host_prep5(inputs, 0, m)
    return m


def host_prep_shared1(inputs, m):
    f = lambda a: np.ascontiguousarray(a, dtype=np.float32)
    m["ada_w"] = f(inputs["ada_w"])
    m["ada_bT"] = f(inputs["ada_b"].reshape(2, 48, 128).transpose(0, 2, 1))
    m["n1gT"] = f(inputs["norm1_g"].reshape(2, 8, 128).transpose(0, 2, 1))
    m["n2gT"] = f(inputs["norm2_g"].reshape(2, 8, 128).transpose(0, 2, 1))
    m["final_g"] = f(inputs["final_g"].reshape(1, 1024))


def prep_core(inputs, b, shared):
    f = lambda a: np.ascontiguousarray(a, dtype=np.float32)
    m = dict(shared)
    m["x"] = f(inputs["x"][b])
    m["ctx"] = f(inputs["ctx"][b])
    cc = np.stack([inputs["c"][b], inputs["c_ctx"]], axis=-1)
    m["ccT"] = f(cc.reshape(8, 128, 2).transpose(1, 0, 2))
    return m


def build_full(nc0, dbg=None):
    k = build(nc0, dbg=dbg)
    declare2(k); declare3(k); declare4(k); declare5(k)
    fw = k.fw; nc = k.nc
    stage_mod(k)
    with ExitStack() as es0:
        alloc_mla_lat(k, es0)
        with ExitStack() as es:
            stage_l0_norm1(k, es)
            w = es.enter_context(nc.sbuf_tensor("s_w_in", [128, 8, 1728], BF16))
            b_w = Buf("w_in")
            for kc in range(8):
                load_w_bf16(k, w[:, kc, :], k.w_in[kc * 128:(kc + 1) * 128, :], b_w)
            stage_l0_lru(k, es, w, b_w)
            stage_l0_mla_lat(k, w, b_w)
        stage_l0_attn(k, es0)
    stage_outproj_norm2(k, 0, k.mixT, k.b_mixT, NCTX, lambda i: k.x[i * 128:(i + 1) * 128, :])
    stage_moe(k, 0, False)
    stage_hy_inproj(k)
    with ExitStack() as esh:
        alloc_hy_rows(k, esh)
        stage_hy_filters(k)
        stage_hy_filter_spectra(k, 0)
        stage_hy_conv(k, 0, k.pv[0], k.b_pv[0], k.pv[1], k.b_pv[1], k.zd[0], k.b_zd[0])
        stage_hy_filter_spectra(k, 1)
        stage_hy_conv(k, 1, k.zd[0], k.b_zd[0], k.pv[2], k.b_pv[2], k.zd[1], k.b_zd[1])
    stage_outproj_norm2(k, 1, k.zd[1], k.b_zd[1], 0, lambda i: k.xend[i * 128:(i + 1) * 128, :], mix_tm=True)
    stage_moe(k, 1, True)
    finish(k)
    return k


def kernel(**inputs):
    inputs = {k_: np.asarray(v) for k_, v in inputs.items()}
    nc0 = bass.Bass("TRN2", target_bir_lowering=False)
    build_full(nc0)
    shared = prep_shared(inputs)
    n = 8
    maps = [prep_core(inputs, b, shared) for b in range(n)]
    res = run_bass_kernel_spmd(nc0, maps, core_ids=list(range(n)))
    out = np.stack([np.asarray(res.results[b]["out"], dtype=np.float32) for b in range(n)], axis=0)
    return out
```
